# Optimizing a Trainium2 kernel written in Bass

```python
import math
import jax, jax.numpy as jnp
from jax import lax
import numpy as np


D_MODEL = 2048
BATCH = 4
SEQ = 2048
DEPTH = 2

PLE_DIM = 256
HEAD_DIM = 64
D_ATTN = D_MODEL // 2
N_Q_HEADS = D_ATTN // HEAD_DIM
N_KV_HEADS = max(1, N_Q_HEADS // 8)
KV_GROUP = N_Q_HEADS // N_KV_HEADS
WINDOW = 128
ROT_DIM = HEAD_DIM // 4
ROPE_THETA = 500000.0
D_GMLP = D_MODEL - D_ATTN
GMLP_CHUNK = 128
GMLP_HEAD = 128
N_GMLP_HEADS = D_GMLP // GMLP_HEAD
D_Q = N_Q_HEADS * HEAD_DIM
D_KV = N_KV_HEADS * HEAD_DIM
D_IN = D_Q + 2 * D_KV + 2 * D_GMLP
N_EXPERTS = 32
TOP_K = 4
D_EXPERT = D_MODEL
SWIGLU_LIMIT = 7.0
SWIGLU_ALPHA = 1.702
MOE_BLOCK = 128
DEEPNORM_ALPHA = (2.0 * DEPTH) ** 0.25
DEEPNORM_BETA = (8.0 * DEPTH) ** -0.25
LN_EPS = 1e-5

kernel_name = 'hymba_swa_sink_gmlp_moe_deepnorm'


def layer_norm(x, g, b):
    xf = x.astype(jnp.float32)
    mu = jnp.mean(xf, axis=-1, keepdims=True)
    var = jnp.mean(jnp.square(xf - mu), axis=-1, keepdims=True)
    return ((xf - mu) * lax.rsqrt(var + LN_EPS) * g.astype(jnp.float32) + b.astype(jnp.float32)).astype(x.dtype)


def rms_norm(x, g):
    xf = x.astype(jnp.float32)
    ms = jnp.mean(jnp.square(xf), axis=-1, keepdims=True)
    return (xf * lax.rsqrt(ms + LN_EPS) * g.astype(jnp.float32)).astype(x.dtype)


def partial_rotary(x, positions):
    half = ROT_DIM // 2
    inv_freq = jnp.exp(-math.log(ROPE_THETA) * jnp.arange(half, dtype=jnp.float32) * (2.0 / ROT_DIM))
    ang = positions.astype(jnp.float32)[..., None] * inv_freq
    cos = jnp.cos(ang)[:, :, None, :]
    sin = jnp.sin(ang)[:, :, None, :]
    xr = x[..., :ROT_DIM].astype(jnp.float32)
    x1, x2 = xr[..., :half], xr[..., half:]
    rot = jnp.concatenate([x1 * cos - x2 * sin, x2 * cos + x1 * sin], axis=-1)
    return jnp.concatenate([rot.astype(x.dtype), x[..., ROT_DIM:]], axis=-1)


def sliding_window_attention(q, k, v, sinks):
    b_, s_ = q.shape[0], q.shape[1]
    nb = s_ // WINDOW
    qb = q.reshape(b_, nb, WINDOW, N_KV_HEADS, KV_GROUP, HEAD_DIM)

    def band(t):
        tb = t.reshape(b_, nb, WINDOW, N_KV_HEADS, HEAD_DIM)
        prev = jnp.pad(tb[:, :-1], ((0, 0), (1, 0), (0, 0), (0, 0), (0, 0)))
        return jnp.concatenate([prev, tb], axis=2)

    kb, vb = band(k), band(v)
    scores = jnp.einsum('bnqhgd,bnkhd->bnhgqk', qb, kb,
                        preferred_element_type=jnp.float32) * (HEAD_DIM ** -0.5)
    qi = jnp.arange(WINDOW)[:, None] + WINDOW
    ki = jnp.arange(2 * WINDOW)[None, :]
    rel = qi - ki
    in_window = (rel >= 0) & (rel < WINDOW)
    not_pad = (jnp.arange(nb)[:, None] > 0) | (jnp.arange(2 * WINDOW)[None, :] >= WINDOW)
    mask = in_window[None] & not_pad[:, None, :]
    scores = jnp.where(mask[None, :, None, None], scores, -jnp.inf)
    sink = sinks.astype(jnp.float32).reshape(1, 1, N_KV_HEADS, KV_GROUP, 1, 1)
    m = jnp.maximum(jnp.max(scores, axis=-1, keepdims=True), sink)
    e = jnp.exp(scores - m)
    probs = e / (jnp.sum(e, axis=-1, keepdims=True) + jnp.exp(sink - m))
    out = jnp.einsum('bnhgqk,bnkhd->bnqhgd', probs.astype(v.dtype), vb)
    return out.reshape(b_, s_, D_Q)


def chunked_spatial_gating(u, v, w_s, b_s, vn_g, vn_b):
    b_, s_ = u.shape[0], u.shape[1]
    nc = s_ // GMLP_CHUNK
    vh = layer_norm(v.reshape(b_, nc, GMLP_CHUNK, N_GMLP_HEADS, GMLP_HEAD), vn_g, vn_b)
    causal = jnp.tril(jnp.ones((GMLP_CHUNK, GMLP_CHUNK), dtype=bool))
    w = jnp.where(causal[None], w_s, 0)
    mixed = jnp.einsum('hts,bnshc->bnthc', w, vh) + jnp.transpose(b_s)[None, None, :, :, None]
    return u * mixed.reshape(b_, s_, D_GMLP)


def hybrid_mixer(h, positions, w_in, sinks, w_s, b_s, vn_g, vn_b, gn_attn, gn_gmlp, w_o):
    b_, s_ = h.shape[0], h.shape[1]
    z = h @ w_in
    q, k, v, zu, zv = jnp.split(z, [D_Q, D_Q + D_KV, D_Q + 2 * D_KV, D_Q + 2 * D_KV + D_GMLP], axis=-1)
    q = partial_rotary(q.reshape(b_, s_, N_Q_HEADS, HEAD_DIM), positions)
    k = partial_rotary(k.reshape(b_, s_, N_KV_HEADS, HEAD_DIM), positions)
    v = v.reshape(b_, s_, N_KV_HEADS, HEAD_DIM)
    y_attn = sliding_window_attention(q, k, v, sinks)
    y_gmlp = chunked_spatial_gating(jax.nn.gelu(zu), jax.nn.gelu(zv), w_s, b_s, vn_g, vn_b)
    y = jnp.concatenate([rms_norm(y_attn, gn_attn), rms_norm(y_gmlp, gn_gmlp)], axis=-1)
    return y @ w_o


def clamped_swiglu(gu):
    gate, up = gu[..., :D_EXPERT], gu[..., D_EXPERT:]
    gate = jnp.minimum(gate, SWIGLU_LIMIT)
    up = jnp.clip(up, -SWIGLU_LIMIT, SWIGLU_LIMIT)
    return (up + 1) * (gate * jax.nn.sigmoid(SWIGLU_ALPHA * gate))


def routed_moe(h, w_router, b_router, w_gu, b_gu, w_down, b_down):
    b_, s_, d = h.shape
    n_tok = b_ * s_
    xt = h.reshape(n_tok, d)
    logits = jnp.dot(xt, w_router, preferred_element_type=jnp.float32) + b_router.astype(jnp.float32)
    top_val, top_idx = lax.top_k(logits, TOP_K)
    gates = jax.nn.softmax(top_val, axis=-1)
    n_assign = n_tok * TOP_K
    flat_e = top_idx.reshape(-1)
    order = jnp.argsort(flat_e)
    sorted_e = flat_e[order]
    tok = order // TOP_K
    counts = jnp.bincount(flat_e, length=N_EXPERTS)
    padded = (counts + MOE_BLOCK - 1) // MOE_BLOCK * MOE_BLOCK
    start = jnp.cumsum(counts) - counts
    pad_end = jnp.cumsum(padded)
    pad_start = pad_end - padded
    slot = pad_start[sorted_e] + jnp.arange(n_assign, dtype=pad_start.dtype) - start[sorted_e]
    n_blocks = -(-n_assign // MOE_BLOCK) + N_EXPERTS
    n_slots = n_blocks * MOE_BLOCK
    slot_tok = jnp.full((n_slots,), n_tok, dtype=jnp.int32).at[slot].set(tok.astype(jnp.int32))
    x_pad = jnp.concatenate([xt, jnp.zeros((1, d), xt.dtype)], axis=0)
    xs = x_pad[slot_tok].reshape(n_blocks, MOE_BLOCK, d)
    block_e = jnp.minimum(jnp.searchsorted(pad_end, jnp.arange(n_blocks) * MOE_BLOCK, side='right'),
                          N_EXPERTS - 1)

    def expert_block(args):
        xb, e = args
        gu = xb @ w_gu[e] + b_gu[e]
        return clamped_swiglu(gu) @ w_down[e] + b_down[e]

    ys = lax.map(expert_block, (xs, block_e)).reshape(n_slots, d)
    w_assign = gates.reshape(-1)[order].astype(h.dtype)
    contrib = ys[slot] * w_assign[:, None]
    y = jax.ops.segment_sum(contrib, tok, num_segments=n_tok)
    return y.reshape(b_, s_, d)


def per_layer_embedding(h, p_i, w_gate, b_gate, w_proj):
    return jax.nn.sigmoid(h @ w_gate + b_gate) * (p_i @ w_proj)


def setup_inputs(seed: int = 0) -> dict:
    key = jax.random.key(seed)
    ks = jax.random.split(key, 32)
    f32 = jnp.float32
    L = DEPTH

    def nrm(k, shape, scale):
        return jax.random.normal(k, shape, f32) * scale

    def gain(k, shape):
        return 1.0 + 0.01 * jax.random.normal(k, shape, f32)

    x = nrm(ks[0], (BATCH, SEQ, D_MODEL), 1.0)
    p = nrm(ks[1], (DEPTH, BATCH, SEQ, PLE_DIM), 1.0)
    positions = (jax.random.randint(ks[2], (BATCH, 1), 0, 4096, dtype=jnp.int32)
                 + jnp.arange(SEQ, dtype=jnp.int32)[None, :])
    return {
        'x': x,
        'p': p,
        'positions': positions,
        'ln_in_g': gain(ks[3], (D_MODEL,)),
        'ln_in_b': nrm(ks[4], (D_MODEL,), 0.01),
        'w_in': nrm(ks[5], (L, D_MODEL, D_IN), D_MODEL ** -0.5),
        'sinks': nrm(ks[6], (L, N_Q_HEADS), 0.5),
        'w_s': nrm(ks[7], (L, N_GMLP_HEADS, GMLP_CHUNK, GMLP_CHUNK), GMLP_CHUNK ** -0.5),
        'b_s': 1.0 + nrm(ks[8], (L, N_GMLP_HEADS, GMLP_CHUNK), 0.01),
        'vnorm_g': gain(ks[9], (L, N_GMLP_HEADS, GMLP_HEAD)),
        'vnorm_b': nrm(ks[10], (L, N_GMLP_HEADS, GMLP_HEAD), 0.01),
        'gnorm_attn': gain(ks[11], (L, D_ATTN)),
        'gnorm_gmlp': gain(ks[12], (L, D_GMLP)),
        'w_o': nrm(ks[13], (L, D_MODEL, D_MODEL), DEEPNORM_BETA * D_MODEL ** -0.5),
        'ln1_g': gain(ks[14], (L, D_MODEL)),
        'ln1_b': nrm(ks[15], (L, D_MODEL), 0.01),
        'w_router': nrm(ks[16], (L, D_MODEL, N_EXPERTS), D_MODEL ** -0.5),
        'b_router': nrm(ks[17], (L, N_EXPERTS), 0.01),
        'w_gu': nrm(ks[18], (L, N_EXPERTS, D_MODEL, 2 * D_EXPERT), D_MODEL ** -0.5),
        'b_gu': nrm(ks[19], (L, N_EXPERTS, 2 * D_EXPERT), 0.01),
        'w_down': nrm(ks[20], (L, N_EXPERTS, D_EXPERT, D_MODEL), DEEPNORM_BETA * D_EXPERT ** -0.5),
        'b_down': nrm(ks[21], (L, N_EXPERTS, D_MODEL), 0.01),
        'ln2_g': gain(ks[22], (L, D_MODEL)),
        'ln2_b': nrm(ks[23], (L, D_MODEL), 0.01),
        'w_ple_gate': nrm(ks[24], (L, D_MODEL, D_MODEL), D_MODEL ** -0.5),
        'b_ple_gate': nrm(ks[25], (L, D_MODEL), 0.01),
        'w_ple_proj': nrm(ks[26], (L, PLE_DIM, D_MODEL), DEEPNORM_BETA * PLE_DIM ** -0.5),
        'ln3_g': gain(ks[27], (L, D_MODEL)),
        'ln3_b': nrm(ks[28], (L, D_MODEL), 0.01),
    }


def reference(x, p, positions, ln_in_g, ln_in_b, w_in, sinks, w_s, b_s, vnorm_g, vnorm_b,
              gnorm_attn, gnorm_gmlp, w_o, ln1_g, ln1_b, w_router, b_router, w_gu, b_gu,
              w_down, b_down, ln2_g, ln2_b, w_ple_gate, b_ple_gate, w_ple_proj, ln3_g, ln3_b):
    h = layer_norm(x, ln_in_g, ln_in_b)
    for i in range(DEPTH):
        mix = hybrid_mixer(h, positions, w_in[i], sinks[i], w_s[i], b_s[i], vnorm_g[i], vnorm_b[i],
                           gnorm_attn[i], gnorm_gmlp[i], w_o[i])
        h = layer_norm(DEEPNORM_ALPHA * h + mix, ln1_g[i], ln1_b[i])
        ffn = routed_moe(h, w_router[i], b_router[i], w_gu[i], b_gu[i], w_down[i], b_down[i])
        h = layer_norm(DEEPNORM_ALPHA * h + ffn, ln2_g[i], ln2_b[i])
        ple = per_layer_embedding(h, p[i], w_ple_gate[i], b_ple_gate[i], w_ple_proj[i])
        h = layer_norm(DEEPNORM_ALPHA * h + ple, ln3_g[i], ln3_b[i])
    return h
```

```python
import numpy as np
import ml_dtypes
from contextlib import ExitStack
import concourse.bass as bass
import concourse.mybir as mybir
from concourse.bass_utils import run_bass_kernel_spmd

F32 = mybir.dt.float32
BF16 = mybir.dt.bfloat16
I32 = mybir.dt.int32
ALU = mybir.AluOpType
AF = mybir.ActivationFunctionType
AX = mybir.AxisListType

NCORES = 8
D = 2048
DEPTH = 2
SEQ = 2048
BATCH = 4
NTOK = BATCH * SEQ
TPC = NTOK // NCORES
D_ATTN = 1024
D_GMLP = 1024
D_IN = 3328
NE = 32
EPC = NE // NCORES
PLE = 256
ALPHA = (2.0 * DEPTH) ** 0.25
EPS = 1e-5
PI = float(np.pi)
NEG = -30000.0


class Res:
    __slots__ = ("w", "r")

    def __init__(self):
        self.w = None
        self.r = []


class KB:
    def __init__(self, nc, es):
        self.nc = nc
        self.es = es
        self.eng = {"pe": nc.tensor, "act": nc.scalar, "dve": nc.vector, "pool": nc.gpsimd, "sp": nc.sync}
        self.ops = {k: [] for k in self.eng}
        self.sem = {k: es.enter_context(nc.semaphore("s_" + k)) for k in self.eng}
        self.cnt = {k: 0 for k in self.eng}
        self.waited = {k: {} for k in self.eng}
        self.dsem = {}
        self.out_toks = []

    def sb(self, name, shape, dt):
        return self.es.enter_context(self.nc.sbuf_tensor(name, list(shape), dt))

    def ps(self, name, shape, dt):
        return self.es.enter_context(self.nc.psum_tensor(name, list(shape), dt))

    def _deps(self, e, reads, writes):
        toks = []
        for r in reads:
            if r.w is not None:
                toks.append(r.w)
        for w in writes:
            if w.w is not None:
                toks.append(w.w)
            toks.extend(w.r)
        waits = []
        wd = self.waited[e]
        for (skey, sem, val, teng) in toks:
            if teng == e and e == "pe":
                continue
            if wd.get(skey, 0) >= val:
                continue
            wd[skey] = val
            waits.append((sem, val))
        return waits

    def op(self, e, fn, reads=(), writes=(), sig=True):
        waits = self._deps(e, reads, writes)
        if sig:
            self.cnt[e] += 1
            val = self.cnt[e]
        else:
            val = self.cnt[e] + 1
        tok = (e, self.sem[e], val, e)
        for r in reads:
            r.r.append(tok)
        for w in writes:
            w.w = tok
            w.r = []
        self.ops[e].append((waits, fn, (self.sem[e], 1) if sig else None))
        return tok

    def dma(self, e, out, in_, reads=(), writes=(), key="misc", is_out=False):
        waits = self._deps(e, reads, writes)
        if key not in self.dsem:
            self.dsem[key] = [self.es.enter_context(self.nc.semaphore("d_" + str(key))), 0]
        ds = self.dsem[key]
        ds[1] += 16
        tok = ("d_" + str(key), ds[0], ds[1], "dma")
        for r in reads:
            r.r.append(tok)
        for w in writes:
            w.w = tok
            w.r = []
        self.ops[e].append((waits, (lambda eng, o=out, i=in_: eng.dma_start(out=o, in_=i)), (ds[0], 16)))
        if is_out:
            self.out_toks.append(tok)
        return tok

    def finish(self):
        waits = []
        seen = {}
        for (skey, sem, val, teng) in self.out_toks:
            if seen.get(skey, (None, 0))[1] < val:
                seen[skey] = (sem, val)
        for skey, (sem, val) in seen.items():
            waits.append((sem, val))
        self.ops["sp"].append((waits, None, None))
        with self.nc.Block() as block:
            def mk(name):
                def body(eng):
                    for waits, fn, inc in self.ops[name]:
                        for sem, val in waits:
                            eng.wait_ge(sem, val)
                        if fn is None:
                            continue
                        ins = fn(eng)
                        if inc is not None:
                            ins.then_inc(inc[0], inc[1])
                return body
            block.tensor(mk("pe"))
            block.scalar(mk("act"))
            block.vector(mk("dve"))
            block.gpsimd(mk("pool"))
            block.sync(mk("sp"))


def bcast_rows(ap, n=128):
    return ap.partition_broadcast(n)


def emit_layernorm(k, X, rX, G, rG, B, rB, tmp):
    st, rst, mv, rmv, rs, rrs = tmp
    for i in range(4):
        k.op("dve", lambda e, i=i: e.bn_stats(out=st[:, i, :], in_=X[:, i * 512:(i + 1) * 512]),
             reads=[rX], writes=[rst])
    k.op("dve", lambda e: e.bn_aggr(out=mv[:], in_=st[:].rearrange("p a b -> p (a b)")), reads=[rst], writes=[rmv])
    k.op("act", lambda e: e.activation(out=rs[:], in_=mv[:, 1:2], func=AF.Sqrt, bias=EPS, scale=1.0),
         reads=[rmv], writes=[rrs])
    k.op("dve", lambda e: e.reciprocal(out=rs[:], in_=rs[:]), reads=[rrs], writes=[rrs])
    k.op("dve", lambda e: e.tensor_scalar(out=X, in0=X, scalar1=mv[:, 0:1], scalar2=rs[:, 0:1],
                                          op0=ALU.subtract, op1=ALU.mult), reads=[rX, rmv, rrs], writes=[rX])
    k.op("pool", lambda e: e.tensor_tensor(out=X, in0=X, in1=G[:], op=ALU.mult), reads=[rX, rG], writes=[rX])
    k.op("pool", lambda e: e.tensor_tensor(out=X, in0=X, in1=B[:], op=ALU.add), reads=[rX, rB], writes=[rX])


def ln_tmp(k, name):
    return (k.sb(name + "_st", [128, 4, 6], F32), Res(), k.sb(name + "_mv", [128, 2], F32), Res(),
            k.sb(name + "_rs", [128, 1], F32), Res())


class WRing:
    def __init__(self, k, name, nslots, kc=16, ncol=256):
        self.k = k
        self.n = nslots
        self.buf = [k.sb("%s%d" % (name, i), [128, kc, ncol], BF16) for i in range(nslots)]
        self.res = [Res() for _ in range(nslots)]
        self.i = 0
        self.name = name

    def load(self, parts):
        s = self.i % self.n
        self.i += 1
        for (c0, c1, src) in parts:
            self.k.dma("pool", self.buf[s][:, :, c0:c1], src.rearrange("(kc p) n -> p kc n", p=128),
                       writes=[self.res[s]], key="%s%d" % (self.name, s))
        return self.buf[s], self.res[s]


NCH = 9
GROUPS = [[0, 1, 2, 3, 4], [5, 6, 7, 8]]


def build_mixer(layer0, stop=None):
    nc = bass.Bass("TRN2", target_bir_lowering=False)
    dt_in = lambda name, shape, dt=F32: nc.dram_tensor(name, list(shape), dt, kind="ExternalInput").ap()
    dt_out = lambda name, shape, dt=F32: nc.dram_tensor(name, list(shape), dt, kind="ExternalOutput").ap()
    xin = dt_in("xin", [NCH * 128, D])
    posi = dt_in("posi", [128, NCH], I32)
    maskd = dt_in("maskd", [128, 2, 256])
    identb = dt_in("identb", [128, 128], BF16)
    identf = dt_in("identf", [128, 128])
    trild = dt_in("trild", [128, 128])
    invfd = dt_in("invfd", [128, 8])
    if layer0:
        lning = dt_in("ln_in_g", [D])
        lninb = dt_in("ln_in_b", [D])
    w_in = dt_in("w_in", [D, D_IN])
    sinksd = dt_in("sinks", [16])
    w_s = dt_in("w_s", [8, 128, 128])
    bsT = dt_in("bsT", [128, 8])
    vng = dt_in("vnorm_g", [1024])
    vnb = dt_in("vnorm_b", [1024])
    gnT = dt_in("gnT", [128, 16])
    w_o = dt_in("w_o", [D, D])
    ln1g = dt_in("ln1_g", [D])
    ln1b = dt_in("ln1_b", [D])
    w_r = dt_in("w_router", [D, NE])
    b_r = dt_in("b_router", [NE])
    h1_out = dt_out("h1", [TPC, D])
    hT_out = dt_out("hT", [16, 128, TPC], BF16)
    hbt_out = dt_out("hbt", [TPC, D], BF16)
    g_out = dt_out("gates", [TPC, NE])

    with ExitStack() as es:
        k = KB(nc, es)
        ID = k.sb("ID", [128, 128], BF16); rID = Res()
        MASK = k.sb("MASK", [128, 2, 256], F32); rMASK = Res()
        TRIL = k.sb("TRIL", [128, 128], F32); rTRIL = Res()
        INVF = k.sb("INVF", [128, 8], F32); rINVF = Res()
        POSI = k.sb("POSI", [128, NCH], I32); rPOSI = Res()
        LG = k.sb("LG", [128, D], F32); rLG = Res()
        LB = k.sb("LB", [128, D], F32); rLB = Res()
        SINK = k.sb("SINK", [128, 16], F32); rSINK = Res()
        VNG = k.sb("VNG", [128, 1024], F32); rVNG = Res()
        VNB = k.sb("VNB", [128, 1024], F32); rVNB = Res()
        BST = k.sb("BST", [128, 8], F32); rBST = Res()
        GNT = k.sb("GNT", [128, 16], F32); rGNT = Res()
        WR = k.sb("WR", [128, 16, NE], F32); rWR = Res()
        BR = k.sb("BR", [128, NE], F32); rBR = Res()
        WST = k.sb("WST", [128, 8, 128], BF16); rWST = Res()
        H = k.sb("H", [128, NCH, D], F32); rH = [Res() for _ in range(NCH)]
        COS = k.sb("COS", [128, NCH, 4, 8], F32); rCOS = Res()
        SIN = k.sb("SIN", [128, NCH, 4, 8], F32); rSIN = Res()
        KT = k.sb("KT", [128, 2, NCH * 128], BF16); rKT = [Res() for _ in range(NCH)]
        V = k.sb("V", [128, NCH, 128], BF16); rV = [Res() for _ in range(NCH)]
        HT = k.sb("HT", [128, 16, 640], BF16); rHT = [Res() for _ in range(5)]
        YT = k.sb("YT", [128, 16, 512], BF16); rYT = [Res() for _ in range(4)]
        SSQ = k.sb("SSQ", [128, NCH, 12], F32); rSSQ = [Res() for _ in range(NCH)]
        ring = WRing(k, "wr", 3)
        lt = ln_tmp(k, "lt")
        hb = k.sb("hb", [128, D], BF16); rhb = Res()
        t8 = [k.sb("t8_%d" % i, [128, 4, 8], F32) for i in range(4)]; rt8 = [Res() for _ in range(4)]
        qb = k.sb("qb", [128, 4, 64], BF16); rqb = Res()
        kd = k.sb("kd", [128, 2, 64], BF16); rkd = Res()
        qT = k.sb("qT", [128, 4, 128], BF16); rqT = Res()
        sm = k.sb("sm", [128, 4, 256], F32); rsm = Res()
        eb = k.sb("eb", [128, 4, 256], BF16); reb = Res()
        eT = k.sb("eT", [128, 2, 4, 128], BF16); reT = Res()
        mx = k.sb("mx", [128, 4], F32); rmx = Res()
        sx = k.sb("sx", [128, 4], F32); rsx = Res()
        es_ = k.sb("es", [128, 4], F32); res_ = Res()
        o32 = k.sb("o32", [128, 256], F32); ro32 = Res()
        yb = k.sb("yb", [128, 256], BF16); ryb = Res()
        g1 = k.sb("g1", [128, 256], F32); rg1 = Res()
        junk, rjunk = g1, rg1
        g2 = k.sb("g2", [128, 256], F32); rg2 = Res()
        g3 = k.sb("g3", [128, 128], F32); rg3 = Res()
        vst = k.sb("vst", [128, 6], F32); rvst = Res()
        vmv = k.sb("vmv", [128, 2], F32); rvmv = Res()
        vrs = k.sb("vrs", [128, 1], F32); rvrs = Res()
        vh = k.sb("vh", [128, 128], BF16); rvh = Res()
        wtmp = k.sb("wtmp", [128, 128], F32); rwtmp = Res()
        wtb = k.sb("wtb", [128, 128], BF16); rwtb = Res()
        rsa = k.sb("rsa", [128, 2], F32); rrsa = Res()
        hloT = k.sb("hloT", [128, 16, 128], BF16); rhloT = Res()
        hlo = k.sb("hlo", [128, D], BF16); rhlo = Res()
        WRH = k.sb("WRH", [128, 16, NE], BF16); rWRH = Res()
        WRL = k.sb("WRL", [128, 16, NE], BF16); rWRL = Res()
        hbT = k.sb("hbT", [128, 16, 128], BF16); rhbT = Res()
        lg = k.sb("lg", [128, NE], F32); rlg = Res()
        m8 = k.sb("m8", [128, 8], F32); rm8 = Res()
        msk = k.sb("msk", [128, NE], F32); rmsk = Res()
        nm = k.sb("nm", [128, 1], F32); rnm = Res()
        gs = k.sb("gs", [128, 1], F32); rgs = Res()
        posf = k.sb("posf", [128, NCH], F32); rposf = Res()
        ang = k.sb("ang", [128, NCH, 8], F32); rang = Res()
        qf = k.sb("qf", [128, NCH, 8], F32); rqf = Res()
        qi = k.sb("qi", [128, NCH, 8], I32); rqi = Res()
        PT = [k.ps("PT%d" % i, [128, 1024], BF16) for i in range(2)]; rPT = [Res(), Res()]
        PP = [k.ps("PP%d" % i, [128, 512], F32) for i in range(2)]; rPP = [Res(), Res()]
        PS = k.ps("PS", [128, 4, 256], F32); rPS = Res()
        PV = k.ps("PV", [128, 512], F32); rPV = Res()
        PR = k.ps("PR", [128, 512], F32); rPR = Res()
        ptc = [0]
        ppc = [0]

        def nextPT():
            i = ptc[0] % 2; ptc[0] += 1
            return PT[i], rPT[i]

        def nextPP():
            i = ppc[0] % 2; ppc[0] += 1
            return PP[i], rPP[i]

        k.op("pool", lambda e: e.memset(SSQ[:], 0.0), writes=rSSQ)
        ld = lambda dst, src, r, key: k.dma("sp", dst, src, writes=[r], key=key)
        ld(ID[:], identb, rID, "c0"); ld(MASK[:], maskd, rMASK, "c2")
        ld(TRIL[:], trild, rTRIL, "c3"); ld(INVF[:], invfd, rINVF, "c4"); ld(POSI[:], posi, rPOSI, "c5")
        ld(SINK[:], sinksd.partition_broadcast(128), rSINK, "c6")
        ld(VNG[:], vng.partition_broadcast(128), rVNG, "c7"); ld(VNB[:], vnb.partition_broadcast(128), rVNB, "c8")
        ld(BST[:], bsT, rBST, "c9"); ld(GNT[:], gnT, rGNT, "c10")
        ld(WR[:], w_r.rearrange("(kc p) n -> p kc n", p=128), rWR, "c11")
        ld(BR[:], b_r.partition_broadcast(128), rBR, "c12")
        k.op("dve", lambda e: e.tensor_copy(out=WRH[:], in_=WR[:]), reads=[rWR], writes=[rWRH])
        k.op("dve", lambda e: e.tensor_tensor(out=WRL[:], in0=WR[:], in1=WRH[:], op=ALU.subtract), reads=[rWR, rWRH], writes=[rWRL])
        if layer0:
            ld(LG[:], lning.partition_broadcast(128), rLG, "c13"); ld(LB[:], lninb.partition_broadcast(128), rLB, "c14")
        for j in range(NCH):
            k.dma("sp", H[:, j, :], xin[j * 128:(j + 1) * 128, :], writes=[rH[j]], key="x%d" % j)
            if layer0:
                emit_layernorm(k, H[:, j, :], rH[j], LG, rLG, LB, rLB, lt)
        if stop == 'ln':
            k.finish()
            return nc
        ld(LG[:], ln1g.partition_broadcast(128), rLG, "c13"); ld(LB[:], ln1b.partition_broadcast(128), rLB, "c14")
        k.op("dve", lambda e: e.tensor_copy(out=posf[:], in_=POSI[:]), reads=[rPOSI], writes=[rposf])
        for which, TAB, rTAB in ((0, SIN, rSIN), (1, COS, rCOS)):
            for j in range(NCH):
                k.op("dve", lambda e, j=j, which=which: e.tensor_scalar(out=ang[:, j, :], in0=INVF[:], scalar1=posf[:, j:j + 1],
                                                           scalar2=(PI / 2 if which else 0.0), op0=ALU.mult, op1=ALU.add),
                     reads=[rINVF, rposf], writes=[rang])
            k.op("dve", lambda e: e.tensor_scalar(out=qf[:], in0=ang[:], scalar1=1.0 / (2 * PI), scalar2=None, op0=ALU.mult),
                 reads=[rang], writes=[rqf])
            k.op("dve", lambda e: e.tensor_copy(out=qi[:], in_=qf[:]), reads=[rqf], writes=[rqi])
            k.op("dve", lambda e: e.tensor_copy(out=qf[:], in_=qi[:]), reads=[rqi], writes=[rqf])
            k.op("dve", lambda e: e.scalar_tensor_tensor(out=ang[:], in0=qf[:], scalar=-2 * PI, in1=ang[:], op0=ALU.mult, op1=ALU.add),
                 reads=[rqf, rang], writes=[rang])
            k.op("dve", lambda e: e.tensor_scalar(out=qf[:], in0=ang[:], scalar1=PI, scalar2=None, op0=ALU.is_gt), reads=[rang], writes=[rqf])
            k.op("dve", lambda e: e.scalar_tensor_tensor(out=ang[:], in0=qf[:], scalar=-2 * PI, in1=ang[:], op0=ALU.mult, op1=ALU.add),
                 reads=[rqf, rang], writes=[rang])
            k.op("dve", lambda e: e.tensor_scalar(out=qf[:], in0=ang[:], scalar1=-PI, scalar2=None, op0=ALU.is_lt), reads=[rang], writes=[rqf])
            k.op("dve", lambda e: e.scalar_tensor_tensor(out=ang[:], in0=qf[:], scalar=2 * PI, in1=ang[:], op0=ALU.mult, op1=ALU.add),
                 reads=[rqf, rang], writes=[rang])
            for hh in range(4):
                k.op("act", lambda e, hh=hh, TAB=TAB: e.activation(out=TAB[:, :, hh, :], in_=ang[:], func=AF.Sin),
                     reads=[rang], writes=[rTAB])
        if stop == 'rope':
            k.finish()
            return nc
        for h in range(8):
            k.dma("sp", wtmp[:], w_s[h], writes=[rwtmp], key="ws")
            k.op("dve", lambda e: e.tensor_tensor(out=wtb[:], in0=wtmp[:], in1=TRIL[:], op=ALU.mult),
                 reads=[rwtmp, rTRIL], writes=[rwtb])
            pt, rpt = nextPT()
            k.op("pe", lambda e, pt=pt: e.transpose(out=pt[:, 0:128], in_=wtb[:], identity=ID[:]),
                 reads=[rwtb, rID], writes=[rpt])
            k.op("act", lambda e, pt=pt, h=h: e.copy(out=WST[:, h, :], in_=pt[:, 0:128]), reads=[rpt], writes=[rWST])

        if stop == 'wst':
            k.finish()
            return nc
        for grp in GROUPS:
            own = [j for j in grp if j >= 1]
            nloc = len(grp)
            loc = {j: i for i, j in enumerate(grp)}
            yloc = {j: i for i, j in enumerate(own)}
            wb_kv, rwb_kv = ring.load([(0, 256, w_in[:, 1024:1280])])
            for j in grp:
                k.op("act", lambda e, j=j: e.copy(out=hb[:], in_=H[:, j, :]), reads=[rH[j]], writes=[rhb])
                for half in range(2):
                    pt, rpt = nextPT()
                    for i in range(8):
                        kc = half * 8 + i
                        k.op("pe", lambda e, pt=pt, kc=kc, i=i: e.transpose(out=pt[:, i * 128:(i + 1) * 128], in_=hb[:, kc * 128:(kc + 1) * 128], identity=ID[:]),
                             reads=[rhb, rID], writes=[rpt], sig=(i == 7))
                    k.op("dve", lambda e, pt=pt, half=half, c=loc[j]: e.tensor_copy(out=HT[:, half * 8:(half + 1) * 8, c * 128:(c + 1) * 128], in_=pt[:].rearrange("p (a b) -> p a b", a=8)),
                         reads=[rpt], writes=[rHT[loc[j]]])
                if j >= 1:
                    k.op("act", lambda e, j=j: e.mul(out=H[:, j, :], in_=H[:, j, :], mul=ALPHA), reads=[rH[j]], writes=[rH[j]])
            if stop == 'ht':
                k.finish()
                return nc
            wb_next, rwb_next = ring.load([(0, 256, w_in[:, 0:256])])
            for j in grp:
                pp, rpp = nextPP()
                c = loc[j]
                for kc in range(16):
                    k.op("pe", lambda e, pp=pp, kc=kc, c=c: e.matmul(pp[:, 0:256], lhsT=HT[:, kc, c * 128:(c + 1) * 128], rhs=wb_kv[:, kc, :], start=(kc == 0), stop=(kc == 15)),
                         reads=[rHT[c], rwb_kv], writes=[rpp], sig=(kc == 15))
                kv = pp[:, 0:128].rearrange("p (g d) -> p g d", g=2)
                cs, sn = COS[:, j, 0:2, :], SIN[:, j, 0:2, :]
                x1, x2 = kv[:, :, 0:8], kv[:, :, 8:16]
                k.op("dve", lambda e, x1=x1, cs=cs: e.tensor_tensor(out=t8[0][:, 0:2, :], in0=x1, in1=cs, op=ALU.mult), reads=[rpp, rCOS], writes=[rt8[0]])
                k.op("dve", lambda e, x2=x2, sn=sn: e.tensor_tensor(out=t8[1][:, 0:2, :], in0=x2, in1=sn, op=ALU.mult), reads=[rpp, rSIN], writes=[rt8[1]])
                k.op("dve", lambda e, x2=x2, cs=cs: e.tensor_tensor(out=t8[2][:, 0:2, :], in0=x2, in1=cs, op=ALU.mult), reads=[rpp, rCOS], writes=[rt8[2]])
                k.op("dve", lambda e, x1=x1, sn=sn: e.tensor_tensor(out=t8[3][:, 0:2, :], in0=x1, in1=sn, op=ALU.mult), reads=[rpp, rSIN], writes=[rt8[3]])
                k.op("dve", lambda e: e.tensor_tensor(out=kd[:, :, 0:8], in0=t8[0][:, 0:2, :], in1=t8[1][:, 0:2, :], op=ALU.subtract), reads=[rt8[0], rt8[1]], writes=[rkd])
                k.op("dve", lambda e: e.tensor_tensor(out=kd[:, :, 8:16], in0=t8[2][:, 0:2, :], in1=t8[3][:, 0:2, :], op=ALU.add), reads=[rt8[2], rt8[3]], writes=[rkd])
                k.op("act", lambda e, kv=kv: e.copy(out=kd[:, :, 16:64], in_=kv[:, :, 16:64]), reads=[rpp], writes=[rkd])
                k.op("act", lambda e, pp=pp, j=j: e.copy(out=V[:, j, :], in_=pp[:, 128:256]), reads=[rpp], writes=[rV[j]])
                pt, rpt = nextPT()
                for g in range(2):
                    k.op("pe", lambda e, pt=pt, g=g: e.transpose(out=pt[0:64, g * 128:(g + 1) * 128], in_=kd[:, g, :], identity=ID[:]),
                         reads=[rkd, rID], writes=[rpt], sig=(g == 1))
                k.op("dve", lambda e, pt=pt, j=j: e.tensor_copy(out=KT[0:64, :, j * 128:(j + 1) * 128], in_=pt[0:64, 0:256].rearrange("p (g t) -> p g t", g=2)),
                     reads=[rpt], writes=[rKT[j]])
            if stop == 'kv':
                k.finish()
                return nc
            for qi_ in range(4):
                wb, rwb = wb_next, rwb_next
                if qi_ < 3:
                    wb_next, rwb_next = ring.load([(0, 256, w_in[:, (qi_ + 1) * 256:(qi_ + 2) * 256])])
                else:
                    wb_next, rwb_next = ring.load([(0, 128, w_in[:, 1280:1408]), (128, 256, w_in[:, 2304:2432])])
                g = qi_ // 2
                for j in own:
                    c = loc[j]
                    pp, rpp = nextPP()
                    for kc in range(16):
                        k.op("pe", lambda e, pp=pp, kc=kc, c=c, wb=wb: e.matmul(pp[:, 0:256], lhsT=HT[:, kc, c * 128:(c + 1) * 128], rhs=wb[:, kc, :], start=(kc == 0), stop=(kc == 15)),
                             reads=[rHT[c], rwb], writes=[rpp], sig=(kc == 15))
                    q4 = pp[:, 0:256].rearrange("p (h d) -> p h d", h=4)
                    cs, sn = COS[:, j, :, :], SIN[:, j, :, :]
                    x1, x2 = q4[:, :, 0:8], q4[:, :, 8:16]
                    k.op("dve", lambda e, x1=x1, cs=cs: e.tensor_tensor(out=t8[0][:], in0=x1, in1=cs, op=ALU.mult), reads=[rpp, rCOS], writes=[rt8[0]])
                    k.op("dve", lambda e, x2=x2, sn=sn: e.tensor_tensor(out=t8[1][:], in0=x2, in1=sn, op=ALU.mult), reads=[rpp, rSIN], writes=[rt8[1]])
                    k.op("dve", lambda e, x2=x2, cs=cs: e.tensor_tensor(out=t8[2][:], in0=x2, in1=cs, op=ALU.mult), reads=[rpp, rCOS], writes=[rt8[2]])
                    k.op("dve", lambda e, x1=x1, sn=sn: e.tensor_tensor(out=t8[3][:], in0=x1, in1=sn, op=ALU.mult), reads=[rpp, rSIN], writes=[rt8[3]])
                    k.op("dve", lambda e: e.tensor_tensor(out=qb[:, :, 0:8], in0=t8[0][:], in1=t8[1][:], op=ALU.subtract), reads=[rt8[0], rt8[1]], writes=[rqb])
                    k.op("dve", lambda e: e.tensor_tensor(out=qb[:, :, 8:16], in0=t8[2][:], in1=t8[3][:], op=ALU.add), reads=[rt8[2], rt8[3]], writes=[rqb])
                    k.op("act", lambda e, q4=q4: e.copy(out=qb[:, :, 16:64], in_=q4[:, :, 16:64]), reads=[rpp], writes=[rqb])
                    pt, rpt = nextPT()
                    for hh in range(4):
                        k.op("pe", lambda e, pt=pt, hh=hh: e.transpose(out=pt[0:64, hh * 128:(hh + 1) * 128], in_=qb[:, hh, :], identity=ID[:]),
                             reads=[rqb, rID], writes=[rpt], sig=(hh == 3))
                    k.op("dve", lambda e, pt=pt: e.tensor_copy(out=qT[0:64, :, :], in_=pt[0:64, 0:512].rearrange("p (a t) -> p a t", a=4)), reads=[rpt], writes=[rqT])
                    for hh in range(4):
                        k.op("pe", lambda e, hh=hh, j=j, g=g: e.matmul(PS[:, hh, :], lhsT=qT[0:64, hh, :], rhs=KT[0:64, g, (j - 1) * 128:(j + 1) * 128], start=True, stop=True),
                             reads=[rqT, rKT[j - 1], rKT[j]], writes=[rPS], sig=(hh == 3))
                    if stop == 'a1':
                        k.finish()
                        return nc
                    mi = 0 if j == 1 else 1
                    k.op("dve", lambda e, mi=mi: e.scalar_tensor_tensor(out=sm[:], in0=PS[:], scalar=0.125, in1=MASK[:, mi, :].unsqueeze(1).to_broadcast([128, 4, 256]), op0=ALU.mult, op1=ALU.add),
                         reads=[rPS, rMASK], writes=[rsm])
                    k.op("dve", lambda e: e.tensor_reduce(out=mx[:], in_=sm[:], axis=AX.X, op=ALU.max), reads=[rsm], writes=[rmx])
                    k.op("dve", lambda e, qi_=qi_: e.tensor_tensor(out=mx[:], in0=mx[:], in1=SINK[:, qi_ * 4:qi_ * 4 + 4], op=ALU.max), reads=[rmx, rSINK], writes=[rmx])
                    k.op("dve", lambda e: e.tensor_tensor(out=sm[:], in0=sm[:], in1=mx[:].unsqueeze(2).to_broadcast([128, 4, 256]), op=ALU.subtract), reads=[rsm, rmx], writes=[rsm])
                    k.op("act", lambda e: e.activation(out=eb[:], in_=sm[:], func=AF.Exp), reads=[rsm], writes=[reb])
                    k.op("dve", lambda e, qi_=qi_: e.tensor_tensor(out=es_[:], in0=SINK[:, qi_ * 4:qi_ * 4 + 4], in1=mx[:], op=ALU.subtract), reads=[rmx, rSINK], writes=[res_])
                    k.op("act", lambda e: e.activation(out=es_[:], in_=es_[:], func=AF.Exp), reads=[res_], writes=[res_])
                    k.op("dve", lambda e: e.tensor_reduce(out=sx[:], in_=eb[:], axis=AX.X, op=ALU.add), reads=[reb], writes=[rsx])
                    k.op("dve", lambda e: e.tensor_tensor(out=sx[:], in0=sx[:], in1=es_[:], op=ALU.add), reads=[rsx, res_], writes=[rsx])
                    k.op("dve", lambda e: e.reciprocal(out=sx[:], in_=sx[:]), reads=[rsx], writes=[rsx])
                    if stop == 'a2':
                        k.finish()
                        return nc
                    pt, rpt = nextPT()
                    for kcx in range(2):
                        for hh in range(4):
                            idx = kcx * 4 + hh
                            k.op("pe", lambda e, pt=pt, kcx=kcx, hh=hh, idx=idx: e.transpose(out=pt[:, idx * 128:(idx + 1) * 128], in_=eb[:, hh, kcx * 128:(kcx + 1) * 128], identity=ID[:]),
                                 reads=[reb, rID], writes=[rpt], sig=(idx == 7))
                    k.op("act", lambda e, pt=pt: e.copy(out=eT[:], in_=pt[:].rearrange("p (a h t) -> p a h t", a=2, h=4)), reads=[rpt], writes=[reT])
                    if stop == 'a3':
                        k.finish()
                        return nc
                    for hh in range(4):
                        for kcx in range(2):
                            k.op("pe", lambda e, hh=hh, kcx=kcx, j=j, g=g: e.matmul(PV[:, hh * 64:(hh + 1) * 64], lhsT=eT[:, kcx, hh, :], rhs=V[:, j - 1 + kcx, g * 64:(g + 1) * 64], start=(kcx == 0), stop=(kcx == 1)),
                                 reads=[reT, rV[j - 1], rV[j]], writes=[rPV], sig=(hh == 3 and kcx == 1))
                    if stop == 'a4':
                        k.finish()
                        return nc
                    k.op("dve", lambda e: e.tensor_tensor(out=o32[:].rearrange("p (h d) -> p h d", h=4), in0=PV[:, 0:256].rearrange("p (h d) -> p h d", h=4), in1=sx[:].unsqueeze(2).to_broadcast([128, 4, 64]), op=ALU.mult),
                         reads=[rPV, rsx], writes=[ro32])
                    if stop == 'a5':
                        k.finish()
                        return nc
                    k.op("act", lambda e, j=j, qi_=qi_: e.activation(out=junk[:], in_=o32[:], func=AF.Square, accum_out=SSQ[:, j, qi_:qi_ + 1]), reads=[ro32], writes=[rjunk, rSSQ[j]])
                    if stop == 'a6':
                        k.finish()
                        return nc
                    k.op("act", lambda e: e.copy(out=yb[:], in_=o32[:]), reads=[ro32], writes=[ryb])
                    pt, rpt = nextPT()
                    for pr in range(2):
                        k.op("pe", lambda e, pt=pt, pr=pr: e.transpose(out=pt[:, pr * 128:(pr + 1) * 128], in_=yb[:, pr * 128:(pr + 1) * 128], identity=ID[:]),
                             reads=[ryb, rID], writes=[rpt], sig=(pr == 1))
                    for pr in range(2):
                        kc = 2 * qi_ + pr
                        k.op("dve", lambda e, pt=pt, pr=pr, kc=kc, yc=yloc[j]: e.tensor_scalar(out=YT[:, kc, yc * 128:(yc + 1) * 128], in0=pt[:, pr * 128:(pr + 1) * 128], scalar1=GNT[:, kc:kc + 1], scalar2=None, op0=ALU.mult),
                             reads=[rpt, rGNT], writes=[rYT[yloc[j]]])
            if stop == 'attn':
                k.finish()
                return nc
            for hd in range(8):
                wb, rwb = wb_next, rwb_next
                if hd < 7:
                    wb_next, rwb_next = ring.load([(0, 128, w_in[:, 1280 + (hd + 1) * 128:1280 + (hd + 2) * 128]),
                                                   (128, 256, w_in[:, 2304 + (hd + 1) * 128:2304 + (hd + 2) * 128])])
                else:
                    wb_next, rwb_next = ring.load([(0, 256, w_o[:, 0:256])])
                for j in own:
                    c = loc[j]
                    pp, rpp = nextPP()
                    for kc in range(16):
                        k.op("pe", lambda e, pp=pp, kc=kc, c=c, wb=wb: e.matmul(pp[:, 0:256], lhsT=HT[:, kc, c * 128:(c + 1) * 128], rhs=wb[:, kc, :], start=(kc == 0), stop=(kc == 15)),
                             reads=[rHT[c], rwb], writes=[rpp], sig=(kc == 15))
                    z = pp[:, 0:256]
                    k.op("act", lambda e, z=z: e.activation(out=g1[:], in_=z, func=AF.Square), reads=[rpp], writes=[rg1])
                    k.op("dve", lambda e: e.tensor_scalar(out=g1[:], in0=g1[:], scalar1=0.044715, scalar2=1.0, op0=ALU.mult, op1=ALU.add), reads=[rg1], writes=[rg1])
                    k.op("dve", lambda e, z=z: e.tensor_tensor(out=g1[:], in0=g1[:], in1=z, op=ALU.mult), reads=[rg1, rpp], writes=[rg1])
                    k.op("act", lambda e: e.activation(out=g1[:], in_=g1[:], func=AF.Sigmoid, scale=1.5957691216057308), reads=[rg1], writes=[rg1])
                    k.op("dve", lambda e, z=z: e.tensor_tensor(out=g2[:], in0=g1[:], in1=z, op=ALU.mult), reads=[rg1, rpp], writes=[rg2])
                    k.op("dve", lambda e: e.bn_stats(out=vst[:], in_=g2[:, 128:256]), reads=[rg2], writes=[rvst])
                    k.op("dve", lambda e: e.bn_aggr(out=vmv[:], in_=vst[:]), reads=[rvst], writes=[rvmv])
                    k.op("act", lambda e: e.activation(out=vrs[:], in_=vmv[:, 1:2], func=AF.Sqrt, bias=EPS, scale=1.0), reads=[rvmv], writes=[rvrs])
                    k.op("dve", lambda e: e.reciprocal(out=vrs[:], in_=vrs[:]), reads=[rvrs], writes=[rvrs])
                    k.op("dve", lambda e: e.tensor_scalar(out=g3[:, 0:128], in0=g2[:, 128:256], scalar1=vmv[:, 0:1], scalar2=vrs[:, 0:1], op0=ALU.subtract, op1=ALU.mult), reads=[rg2, rvmv, rvrs], writes=[rg3])
                    k.op("pool", lambda e, hd=hd: e.tensor_tensor(out=g3[:, 0:128], in0=g3[:, 0:128], in1=VNG[:, hd * 128:(hd + 1) * 128], op=ALU.mult), reads=[rg3, rVNG], writes=[rg3])
                    k.op("pool", lambda e, hd=hd: e.tensor_tensor(out=vh[:], in0=g3[:, 0:128], in1=VNB[:, hd * 128:(hd + 1) * 128], op=ALU.add), reads=[rg3, rVNB], writes=[rvh])
                    k.op("pe", lambda e, hd=hd: e.matmul(PV[:, 256:384], lhsT=WST[:, hd, :], rhs=vh[:], start=True, stop=True), reads=[rWST, rvh], writes=[rPV])
                    k.op("dve", lambda e, hd=hd: e.scalar_tensor_tensor(out=o32[:, 0:128], in0=PV[:, 256:384], scalar=BST[:, hd:hd + 1], in1=g2[:, 0:128], op0=ALU.add, op1=ALU.mult), reads=[rPV, rBST, rg2], writes=[ro32])
                    k.op("act", lambda e, j=j, hd=hd: e.activation(out=junk[:, 0:128], in_=o32[:, 0:128], func=AF.Square, accum_out=SSQ[:, j, 4 + hd:5 + hd]),
                         reads=[ro32], writes=[rjunk, rSSQ[j]])
                    k.op("act", lambda e: e.copy(out=yb[:, 0:128], in_=o32[:, 0:128]), reads=[ro32], writes=[ryb])
                    pt, rpt = nextPT()
                    k.op("pe", lambda e, pt=pt: e.transpose(out=pt[:, 0:128], in_=yb[:, 0:128], identity=ID[:]), reads=[ryb, rID], writes=[rpt])
                    k.op("dve", lambda e, pt=pt, hd=hd, yc=yloc[j]: e.tensor_scalar(out=YT[:, 8 + hd, yc * 128:(yc + 1) * 128], in0=pt[:, 0:128], scalar1=GNT[:, 8 + hd:9 + hd], scalar2=None, op0=ALU.mult),
                         reads=[rpt, rGNT], writes=[rYT[yloc[j]]])
            if stop == 'gmlp':
                k.finish()
                return nc
            for j in own:
                k.op("dve", lambda e, j=j: e.tensor_reduce(out=rsa[:, 0:1], in_=SSQ[:, j, 0:4], axis=AX.X, op=ALU.add), reads=[rSSQ[j]], writes=[rrsa])
                k.op("dve", lambda e, j=j: e.tensor_reduce(out=rsa[:, 1:2], in_=SSQ[:, j, 4:12], axis=AX.X, op=ALU.add), reads=[rSSQ[j]], writes=[rrsa])
                k.op("act", lambda e: e.activation(out=rsa[:], in_=rsa[:], func=AF.Sqrt, bias=EPS, scale=1.0 / 1024.0), reads=[rrsa], writes=[rrsa])
                k.op("dve", lambda e, j=j: e.reciprocal(out=SSQ[:, j, 0:2], in_=rsa[:]), reads=[rrsa], writes=[rSSQ[j]])
            for oi in range(8):
                wb, rwb = wb_next, rwb_next
                if oi < 7:
                    wb_next, rwb_next = ring.load([(0, 256, w_o[:, (oi + 1) * 256:(oi + 2) * 256])])
                for j in own:
                    yc = yloc[j]
                    pp, rpp = nextPP()
                    for half in range(2):
                        for i in range(8):
                            kc = half * 8 + i
                            k.op("pe", lambda e, pp=pp, kc=kc, yc=yc, wb=wb, half=half, i=i: e.matmul(pp[:, half * 256:(half + 1) * 256], lhsT=YT[:, kc, yc * 128:(yc + 1) * 128], rhs=wb[:, kc, :], start=(i == 0), stop=(i == 7)),
                                 reads=[rYT[yc], rwb], writes=[rpp], sig=(kc == 15))
                    hs = H[:, j, oi * 256:(oi + 1) * 256]
                    k.op("dve", lambda e, pp=pp, hs=hs, j=j: e.scalar_tensor_tensor(out=hs, in0=pp[:, 0:256], scalar=SSQ[:, j, 0:1], in1=hs, op0=ALU.mult, op1=ALU.add), reads=[rpp, rSSQ[j], rH[j]], writes=[rH[j]])
                    k.op("dve", lambda e, pp=pp, hs=hs, j=j: e.scalar_tensor_tensor(out=hs, in0=pp[:, 256:512], scalar=SSQ[:, j, 1:2], in1=hs, op0=ALU.mult, op1=ALU.add), reads=[rpp, rSSQ[j], rH[j]], writes=[rH[j]])
            if stop == 'wo':
                k.finish()
                return nc
            for j in own:
                emit_layernorm(k, H[:, j, :], rH[j], LG, rLG, LB, rLB, lt)
                k.dma("sp", h1_out[(j - 1) * 128:j * 128, :], H[:, j, :], reads=[rH[j]], key="oh", is_out=True)
                k.op("act", lambda e, j=j: e.copy(out=hb[:], in_=H[:, j, :]), reads=[rH[j]], writes=[rhb])
                k.op("dve", lambda e, j=j: e.tensor_tensor(out=hlo[:], in0=H[:, j, :], in1=hb[:], op=ALU.subtract), reads=[rH[j], rhb], writes=[rhlo])
                k.dma("sp", hbt_out[(j - 1) * 128:j * 128, :], hb[:], reads=[rhb], key="ob", is_out=True)
                for src, rsrc, dstT, rdstT in ((hb, rhb, hbT, rhbT), (hlo, rhlo, hloT, rhloT)):
                    for half in range(2):
                        pt, rpt = nextPT()
                        for i in range(8):
                            kc = half * 8 + i
                            k.op("pe", lambda e, pt=pt, kc=kc, i=i, src=src: e.transpose(out=pt[:, i * 128:(i + 1) * 128], in_=src[:, kc * 128:(kc + 1) * 128], identity=ID[:]),
                                 reads=[rsrc, rID], writes=[rpt], sig=(i == 7))
                        k.op("dve", lambda e, pt=pt, half=half, dstT=dstT: e.tensor_copy(out=dstT[:, half * 8:(half + 1) * 8, :], in_=pt[:].rearrange("p (a b) -> p a b", a=8)),
                             reads=[rpt], writes=[rdstT])
                k.dma("sp", hT_out[:, :, (j - 1) * 128:j * 128].rearrange("kc p t -> p kc t"), hbT[:], reads=[rhbT], key="ot", is_out=True)
                n = 0
                for (aT, raT, wq, rwq) in ((hbT, rhbT, WRH, rWRH), (hloT, rhloT, WRH, rWRH), (hbT, rhbT, WRL, rWRL)):
                    for kc in range(16):
                        k.op("pe", lambda e, kc=kc, aT=aT, wq=wq, n=n: e.matmul(PR[:, 0:NE], lhsT=aT[:, kc, :], rhs=wq[:, kc, :], start=(n == 0), stop=(n == 47)),
                             reads=[raT, rwq], writes=[rPR], sig=(n == 47))
                        n += 1
                k.op("dve", lambda e: e.tensor_tensor(out=lg[:], in0=PR[:, 0:NE], in1=BR[:], op=ALU.add), reads=[rPR, rBR], writes=[rlg])
                k.op("dve", lambda e: e.max(out=m8[:], in_=lg[:]), reads=[rlg], writes=[rm8])
                k.op("dve", lambda e: e.tensor_scalar(out=msk[:], in0=lg[:], scalar1=m8[:, 3:4], scalar2=None, op0=ALU.is_ge), reads=[rlg, rm8], writes=[rmsk])
                k.op("dve", lambda e: e.tensor_scalar(out=nm[:], in0=m8[:, 0:1], scalar1=-1.0, scalar2=None, op0=ALU.mult), reads=[rm8], writes=[rnm])
                k.op("act", lambda e: e.activation(out=lg[:], in_=lg[:], func=AF.Exp, bias=nm[:, 0:1], scale=1.0), reads=[rlg, rnm], writes=[rlg])
                k.op("dve", lambda e: e.tensor_tensor(out=lg[:], in0=lg[:], in1=msk[:], op=ALU.mult), reads=[rlg, rmsk], writes=[rlg])
                k.op("dve", lambda e: e.tensor_reduce(out=gs[:], in_=lg[:], axis=AX.X, op=ALU.add), reads=[rlg], writes=[rgs])
                k.op("dve", lambda e: e.reciprocal(out=gs[:], in_=gs[:]), reads=[rgs], writes=[rgs])
                k.op("dve", lambda e: e.tensor_scalar(out=lg[:], in0=lg[:], scalar1=gs[:, 0:1], scalar2=None, op0=ALU.mult), reads=[rlg, rgs], writes=[rlg])
                k.dma("sp", g_out[(j - 1) * 128:j * 128, :], lg[:], reads=[rlg], key="og", is_out=True)
        k.finish()
    return nc


TG = 1024


def build_moe():
    nc = bass.Bass("TRN2", target_bir_lowering=False)
    dt_in = lambda name, shape, dt=F32: nc.dram_tensor(name, list(shape), dt, kind="ExternalInput").ap()
    xT = dt_in("xT", [128, 16, NTOK], BF16)
    gT = dt_in("gT", [EPC, NTOK])
    w_gu = dt_in("w_gu", [EPC, D, 2 * D])
    bguT = dt_in("bguT", [128, EPC, 32])
    w_dn = dt_in("w_down", [EPC, D, D])
    b_dn = dt_in("b_down", [EPC, D])
    y_out = nc.dram_tensor("ypart", [NTOK, D], F32, kind="ExternalOutput").ap()
    NG = NTOK // TG
    with ExitStack() as es:
        k = KB(nc, es)
        XT = k.sb("XT", [128, 16, TG], BF16); rXT = Res()
        GB = k.sb("GB", [128, EPC, TG], F32); rGB = Res()
        G4 = k.sb("G4", [EPC, TG], F32); rG4 = Res()
        G4b = k.sb("G4b", [EPC, TG], BF16); rG4b = Res()
        BD = k.sb("BD", [EPC, D], F32); rBD = Res()
        BDb = k.sb("BDb", [EPC, D], BF16); rBDb = Res()
        BGU = k.sb("BGU", [128, EPC, 32], F32); rBGU = Res()
        YACC = k.sb("YACC", [128, TG // 128, D], F32); rY = [Res() for _ in range(TG // 128)]
        AT = k.sb("AT", [128, 16, TG], BF16); rAT = [Res() for _ in range(16)]
        ring = WRing(k, "wm", 4)
        T1 = [k.sb("T1_%d" % i, [128, 512], F32) for i in range(2)]; rT1 = [Res(), Res()]
        T2 = [k.sb("T2_%d" % i, [128, 512], F32) for i in range(2)]; rT2 = [Res(), Res()]
        T3 = [k.sb("T3_%d" % i, [128, 512], F32) for i in range(2)]; rT3 = [Res(), Res()]
        PA = [k.ps("PA%d" % i, [128, 512], F32) for i in range(2)]; rPA = [Res(), Res()]
        PB = [k.ps("PB%d" % i, [128, 512], F32) for i in range(2)]; rPB = [Res(), Res()]
        PD = [k.ps("PD%d" % i, [128, 512], F32) for i in range(2)]; rPD = [Res(), Res()]
        k.dma("sp", BGU[:], bguT, writes=[rBGU], key="c0")
        k.dma("sp", BD[:], b_dn, writes=[rBD], key="c1")
        k.op("dve", lambda e: e.tensor_copy(out=BDb[:], in_=BD[:]), reads=[rBD], writes=[rBDb])
        pieces = []
        for tg in range(NG):
            for ex in range(EPC):
                for fc in range(16):
                    pieces.append([(0, 128, w_gu[ex][:, fc * 128:(fc + 1) * 128]), (128, 256, w_gu[ex][:, D + fc * 128:D + (fc + 1) * 128])])
                for dp in range(8):
                    pieces.append([(0, 256, w_dn[ex][:, dp * 256:(dp + 1) * 256])])
        LA = 2
        loaded = []
        pi = [0]

        def get_piece():
            while len(loaded) < min(len(pieces), pi[0] + 1 + LA):
                loaded.append(ring.load(pieces[len(loaded)]))
            r = loaded[pi[0]]
            pi[0] += 1
            return r

        cnt = 0
        dcnt = 0
        for tg in range(NG):
            t0 = tg * TG
            k.dma("sp", XT[:], xT[:, :, t0:t0 + TG], writes=[rXT], key="xt")
            for ex in range(EPC):
                k.dma("sp", GB[:, ex, :], gT[ex, t0:t0 + TG].partition_broadcast(128), writes=[rGB], key="gb")
            k.dma("sp", G4[:], gT[:, t0:t0 + TG], writes=[rG4], key="g4")
            k.op("dve", lambda e: e.tensor_copy(out=G4b[:], in_=G4[:]), reads=[rG4], writes=[rG4b])
            for ex in range(EPC):
                for fc in range(16):
                    wb, rwb = get_piece()
                    for ns in range(2):
                        b = cnt % 2
                        cnt += 1
                        for kc in range(16):
                            k.op("pe", lambda e, b=b, kc=kc, wb=wb, ns=ns: e.matmul(PA[b][:], lhsT=wb[:, kc, 0:128], rhs=XT[:, kc, ns * 512:(ns + 1) * 512], start=(kc == 0), stop=(kc == 15)),
                                 reads=[rwb, rXT], writes=[rPA[b]], sig=(kc == 15))
                        for kc in range(16):
                            k.op("pe", lambda e, b=b, kc=kc, wb=wb, ns=ns: e.matmul(PB[b][:], lhsT=wb[:, kc, 128:256], rhs=XT[:, kc, ns * 512:(ns + 1) * 512], start=(kc == 0), stop=(kc == 15)),
                                 reads=[rwb, rXT], writes=[rPB[b]], sig=(kc == 15))
                        k.op("dve", lambda e, b=b, ex=ex, fc=fc: e.tensor_scalar(out=T1[b][:], in0=PA[b][:], scalar1=BGU[:, ex, fc:fc + 1], scalar2=7.0, op0=ALU.add, op1=ALU.min),
                             reads=[rPA[b], rBGU], writes=[rT1[b]])
                        k.op("act", lambda e, b=b: e.activation(out=T2[b][:], in_=T1[b][:], func=AF.Sigmoid, scale=1.702), reads=[rT1[b]], writes=[rT2[b]])
                        k.op("dve", lambda e, b=b, ex=ex, fc=fc: e.tensor_scalar(out=T3[b][:], in0=PB[b][:], scalar1=BGU[:, ex, 16 + fc:17 + fc], scalar2=7.0, op0=ALU.add, op1=ALU.min),
                             reads=[rPB[b], rBGU], writes=[rT3[b]])
                        k.op("dve", lambda e, b=b: e.tensor_scalar(out=T3[b][:], in0=T3[b][:], scalar1=-7.0, scalar2=1.0, op0=ALU.max, op1=ALU.add), reads=[rT3[b]], writes=[rT3[b]])
                        k.op("pool", lambda e, b=b: e.tensor_tensor(out=T1[b][:], in0=T1[b][:], in1=T2[b][:], op=ALU.mult), reads=[rT1[b], rT2[b]], writes=[rT1[b]])
                        k.op("pool", lambda e, b=b: e.tensor_tensor(out=T1[b][:], in0=T1[b][:], in1=T3[b][:], op=ALU.mult), reads=[rT1[b], rT3[b]], writes=[rT1[b]])
                        k.op("pool", lambda e, b=b, ex=ex, fc=fc, ns=ns: e.tensor_tensor(out=AT[:, fc, ns * 512:(ns + 1) * 512], in0=T1[b][:], in1=GB[:, ex, ns * 512:(ns + 1) * 512], op=ALU.mult),
                             reads=[rT1[b], rGB], writes=[rAT[fc]])
                for dp in range(8):
                    wb, rwb = get_piece()
                    for tc in range(TG // 128):
                        b = dcnt % 2
                        dcnt += 1
                        if ex == 0:
                            k.op("pe", lambda e, b=b, tc=tc, dp=dp: e.matmul(PD[b][:, 0:256], lhsT=G4b[:, tc * 128:(tc + 1) * 128], rhs=BDb[:, dp * 256:(dp + 1) * 256], start=True, stop=False),
                                 reads=[rG4b, rBDb], writes=[rPD[b]], sig=False)
                        for fc in range(16):
                            k.op("pe", lambda e, b=b, tc=tc, fc=fc, wb=wb, ex=ex: e.matmul(PD[b][:, 0:256], lhsT=AT[:, fc, tc * 128:(tc + 1) * 128], rhs=wb[:, fc, :], start=(fc == 0 and ex != 0), stop=(fc == 15)),
                                 reads=[rAT[fc], rwb], writes=[rPD[b]], sig=(fc == 15))
                        ys = YACC[:, tc, dp * 256:(dp + 1) * 256]
                        if ex == 0:
                            k.op("act", lambda e, b=b, ys=ys: e.copy(out=ys, in_=PD[b][:, 0:256]), reads=[rPD[b]], writes=[rY[tc]])
                        else:
                            k.op("dve", lambda e, b=b, ys=ys: e.tensor_tensor(out=ys, in0=ys, in1=PD[b][:, 0:256], op=ALU.add), reads=[rPD[b], rY[tc]], writes=[rY[tc]])
            for tc in range(TG // 128):
                k.dma("sp", y_out[t0 + tc * 128:t0 + (tc + 1) * 128, :], YACC[:, tc, :], reads=[rY[tc]], key="oy", is_out=True)
        k.finish()
    return nc


CAP = 256
NBLK = NTOK // 1024
NSLOT = NBLK * CAP
HSLOT = NSLOT // 2


def build_moe2():
    nc = bass.Bass("TRN2", target_bir_lowering=False)
    dt_in = lambda name, shape, dt=F32: nc.dram_tensor(name, list(shape), dt, kind="ExternalInput").ap()
    hbt = dt_in("hbt", [NTOK, D], BF16)
    gC = dt_in("gC", [128, NTOK // 128, EPC])
    iotad = dt_in("iotad", [128, CAP])
    sud = dt_in("sud", [128, 128], BF16)
    onesd = dt_in("onesd", [128, 128], BF16)
    identb = dt_in("identb", [128, 128], BF16)
    w_gu = dt_in("w_gu", [EPC, D, 2 * D])
    bguT = dt_in("bguT", [128, EPC, 32])
    w_dn = dt_in("w_down", [EPC, D, D])
    b_dn = dt_in("b_down", [EPC, D])
    y_out = nc.dram_tensor("ypart", [NTOK, D], F32, kind="ExternalOutput").ap()
    ysp = nc.dram_tensor("ysp", [EPC, NSLOT, D], BF16, kind="Internal").ap()
    NTC = NTOK // 128
    with ExitStack() as es:
        k = KB(nc, es)
        IOTA = k.sb("IOTA", [128, CAP], F32); rIOTA = Res()
        SU = k.sb("SU", [128, 128], BF16); rSU = Res()
        ONES = k.sb("ONES", [128, 128], BF16); rONES = Res()
        ID = k.sb("ID", [128, 128], BF16); rID = Res()
        GC = k.sb("GC", [128, NTC, EPC], F32); rGC = Res()
        MKb = k.sb("MKb", [128, NTC * EPC], BF16); rMKb = Res()
        MKf = k.sb("MKf", [128, NTC, EPC], F32); rMKf = Res()
        W1 = k.sb("W1", [128, NTC, EPC], F32); rW1 = Res()
        CNT = k.sb("CNT", [128, NTC, EPC], F32); rCNT = Res()
        PRE = k.sb("PRE", [128, NTC, EPC], F32); rPRE = Res()
        POSM = k.sb("POSM", [128, NTC, EPC], F32); rPOSM = Res()
        BGU = k.sb("BGU", [128, EPC, 32], F32); rBGU = Res()
        BDb1 = k.sb("BDb1", [1, D], BF16); rBDb1 = Res()
        XT = k.sb("XT", [128, 16, HSLOT], BF16); rXT = Res()
        AT = k.sb("AT", [128, 16, HSLOT], BF16); rAT = [Res() for _ in range(16)]
        HB = k.sb("HB", [128, 8, D], BF16); rHB = Res()
        S = [k.sb("S%d" % i, [128, 8, CAP], BF16) for i in range(2)]; rS = [Res(), Res()]
        YST = [k.sb("YST%d" % i, [128, 256], BF16) for i in range(4)]; rYST = [Res() for _ in range(4)]
        OUT = [k.sb("OUT%d" % i, [128, D], F32) for i in range(2)]; rOUT = [Res(), Res()]
        ring = WRing(k, "wm", 4)
        T1 = [k.sb("T1_%d" % i, [128, 512], F32) for i in range(2)]; rT1 = [Res(), Res()]
        T2 = [k.sb("T2_%d" % i, [128, 512], F32) for i in range(2)]; rT2 = [Res(), Res()]
        T3 = [k.sb("T3_%d" % i, [128, 512], F32) for i in range(2)]; rT3 = [Res(), Res()]
        PA = [k.ps("PA%d" % i, [128, 512], F32) for i in range(2)]; rPA = [Res(), Res()]
        PB = [k.ps("PB%d" % i, [128, 512], F32) for i in range(2)]; rPB = [Res(), Res()]
        PD = [k.ps("PD%d" % i, [128, 512], F32) for i in range(2)]; rPD = [Res(), Res()]
        PT = [k.ps("PT%d" % i, [128, 1024], BF16) for i in range(2)]; rPT = [Res(), Res()]
        ld = lambda dst, src, r, key: k.dma("sp", dst, src, writes=[r], key=key)
        ld(IOTA[:], iotad, rIOTA, "c0"); ld(SU[:], sud, rSU, "c1"); ld(ONES[:], onesd, rONES, "c2"); ld(ID[:], identb, rID, "c3")
        ld(GC[:], gC, rGC, "c4"); ld(BGU[:], bguT, rBGU, "c5")
        gcf = GC[:].rearrange("p a b -> p (a b)")
        k.op("dve", lambda e: e.tensor_scalar(out=MKb[:], in0=gcf, scalar1=0.0, scalar2=None, op0=ALU.is_gt), reads=[rGC], writes=[rMKb])
        k.op("dve", lambda e: e.tensor_scalar(out=MKf[:].rearrange("p a b -> p (a b)"), in0=gcf, scalar1=0.0, scalar2=None, op0=ALU.is_gt), reads=[rGC], writes=[rMKf])
        k.op("pe", lambda e: e.matmul(PA[0][:, 0:NTC * EPC], lhsT=SU[:], rhs=MKb[:], start=True, stop=True), reads=[rSU, rMKb], writes=[rPA[0]])
        k.op("pe", lambda e: e.matmul(PA[1][:, 0:NTC * EPC], lhsT=ONES[:], rhs=MKb[:], start=True, stop=True), reads=[rONES, rMKb], writes=[rPA[1]])
        k.op("dve", lambda e: e.tensor_copy(out=W1[:].rearrange("p a b -> p (a b)"), in_=PA[0][:, 0:NTC * EPC]), reads=[rPA[0]], writes=[rW1])
        k.op("dve", lambda e: e.tensor_copy(out=CNT[:].rearrange("p a b -> p (a b)"), in_=PA[1][:, 0:NTC * EPC]), reads=[rPA[1]], writes=[rCNT])
        pre4 = PRE[:].rearrange("p (j t) e -> p j t e", t=8)
        cnt4 = CNT[:].rearrange("p (j t) e -> p j t e", t=8)
        k.op("dve", lambda e: e.memset(PRE[:], 0.0), writes=[rPRE])
        for i in range(1, 8):
            k.op("dve", lambda e, i=i: e.tensor_tensor(out=pre4[:, :, i, :], in0=pre4[:, :, i - 1, :], in1=cnt4[:, :, i - 1, :], op=ALU.add), reads=[rPRE, rCNT], writes=[rPRE])
        k.op("dve", lambda e: e.tensor_tensor(out=POSM[:], in0=W1[:], in1=PRE[:], op=ALU.add), reads=[rW1, rPRE], writes=[rPOSM])
        k.op("dve", lambda e: e.scalar_tensor_tensor(out=POSM[:], in0=POSM[:], scalar=1.0, in1=MKf[:], op0=ALU.add, op1=ALU.mult), reads=[rPOSM, rMKf], writes=[rPOSM])
        k.op("dve", lambda e: e.tensor_scalar(out=POSM[:], in0=POSM[:], scalar1=-1.0, scalar2=None, op0=ALU.add), reads=[rPOSM], writes=[rPOSM])
        pieces = []
        for ex, _half in [(a, b) for a in range(EPC) for b in range(2)]:
            for fc in range(16):
                pieces.append([(0, 128, w_gu[ex][:, fc * 128:(fc + 1) * 128]), (128, 256, w_gu[ex][:, D + fc * 128:D + (fc + 1) * 128])])
            for dp in range(8):
                pieces.append([(0, 256, w_dn[ex][:, dp * 256:(dp + 1) * 256])])
        LA = 2
        loaded = []
        pi = [0]

        def get_piece():
            while len(loaded) < min(len(pieces), pi[0] + 1 + LA):
                loaded.append(ring.load(pieces[len(loaded)]))
            r = loaded[pi[0]]
            pi[0] += 1
            return r

        cnt = 0
        dcnt = 0
        scnt = 0
        ycnt = 0
        NS = HSLOT // 512
        for ex, half in [(a, b) for a in range(EPC) for b in range(2)]:
            for j in range(half * NBLK // 2, (half + 1) * NBLK // 2):
                jl = j - half * NBLK // 2
                k.dma("sp", HB[:], hbt[j * 1024:(j + 1) * 1024, :].rearrange("(tc p) d -> p tc d", p=128), writes=[rHB], key="hb")
                sb_ = scnt % 2
                scnt += 1
                for tc in range(8):
                    k.op("dve", lambda e, sb_=sb_, tc=tc, j=j, ex=ex: e.tensor_scalar(out=S[sb_][:, tc, :], in0=IOTA[:], scalar1=POSM[:, j * 8 + tc, ex:ex + 1], scalar2=None, op0=ALU.is_equal),
                         reads=[rIOTA, rPOSM], writes=[rS[sb_]])
                for dcp in range(8):
                    b = dcnt % 2
                    dcnt += 1
                    for dci in range(2):
                        dc = dcp * 2 + dci
                        for tc in range(8):
                            k.op("pe", lambda e, b=b, dci=dci, dc=dc, tc=tc, sb_=sb_: e.matmul(PD[b][:, dci * CAP:(dci + 1) * CAP], lhsT=HB[:, tc, dc * 128:(dc + 1) * 128], rhs=S[sb_][:, tc, :], start=(tc == 0), stop=(tc == 7)),
                                 reads=[rHB, rS[sb_]], writes=[rPD[b]], sig=(tc == 7 and dci == 1))
                    eng = "act" if dcp % 2 == 0 else "dve"
                    if eng == "act":
                        k.op("act", lambda e, b=b, dcp=dcp, jl=jl: e.copy(out=XT[:, 2 * dcp:2 * dcp + 2, jl * CAP:(jl + 1) * CAP], in_=PD[b][:, 0:2 * CAP].rearrange("p (a s) -> p a s", a=2)), reads=[rPD[b]], writes=[rXT])
                    else:
                        k.op("dve", lambda e, b=b, dcp=dcp, jl=jl: e.tensor_copy(out=XT[:, 2 * dcp:2 * dcp + 2, jl * CAP:(jl + 1) * CAP], in_=PD[b][:, 0:2 * CAP].rearrange("p (a s) -> p a s", a=2)), reads=[rPD[b]], writes=[rXT])
            k.dma("pool", BDb1[:], b_dn[ex:ex + 1, :], writes=[rBDb1], key="bd")
            for fc in range(16):
                wb, rwb = get_piece()
                for ns in range(NS):
                    b = cnt % 2
                    cnt += 1
                    for kc in range(16):
                        k.op("pe", lambda e, b=b, kc=kc, wb=wb, ns=ns: e.matmul(PA[b][:], lhsT=wb[:, kc, 0:128], rhs=XT[:, kc, ns * 512:(ns + 1) * 512], start=(kc == 0), stop=(kc == 15)),
                             reads=[rwb, rXT], writes=[rPA[b]], sig=(kc == 15))
                    for kc in range(16):
                        k.op("pe", lambda e, b=b, kc=kc, wb=wb, ns=ns: e.matmul(PB[b][:], lhsT=wb[:, kc, 128:256], rhs=XT[:, kc, ns * 512:(ns + 1) * 512], start=(kc == 0), stop=(kc == 15)),
                             reads=[rwb, rXT], writes=[rPB[b]], sig=(kc == 15))
                    k.op("dve", lambda e, b=b, ex=ex, fc=fc: e.tensor_scalar(out=T1[b][:], in0=PA[b][:], scalar1=BGU[:, ex, fc:fc + 1], scalar2=7.0, op0=ALU.add, op1=ALU.min),
                         reads=[rPA[b], rBGU], writes=[rT1[b]])
                    k.op("act", lambda e, b=b: e.activation(out=T2[b][:], in_=T1[b][:], func=AF.Sigmoid, scale=1.702), reads=[rT1[b]], writes=[rT2[b]])
                    k.op("dve", lambda e, b=b, ex=ex, fc=fc: e.tensor_scalar(out=T3[b][:], in0=PB[b][:], scalar1=BGU[:, ex, 16 + fc:17 + fc], scalar2=7.0, op0=ALU.add, op1=ALU.min),
                         reads=[rPB[b], rBGU], writes=[rT3[b]])
                    k.op("dve", lambda e, b=b: e.tensor_scalar(out=T3[b][:], in0=T3[b][:], scalar1=-7.0, scalar2=1.0, op0=ALU.max, op1=ALU.add), reads=[rT3[b]], writes=[rT3[b]])
                    k.op("pool", lambda e, b=b: e.tensor_tensor(out=T1[b][:], in0=T1[b][:], in1=T2[b][:], op=ALU.mult), reads=[rT1[b], rT2[b]], writes=[rT1[b]])
                    k.op("pool", lambda e, b=b, fc=fc, ns=ns: e.tensor_tensor(out=AT[:, fc, ns * 512:(ns + 1) * 512], in0=T1[b][:], in1=T3[b][:], op=ALU.mult),
                         reads=[rT1[b], rT3[b]], writes=[rAT[fc]])
            for dp in range(8):
                wb, rwb = get_piece()
                for sc in range(HSLOT // 128):
                    b = dcnt % 2
                    dcnt += 1
                    k.op("pe", lambda e, b=b, dp=dp, ex=ex: e.matmul(PD[b][:, 0:256], lhsT=ONES[0:1, :], rhs=BDb1[:, dp * 256:(dp + 1) * 256], start=True, stop=False),
                         reads=[rONES, rBDb1], writes=[rPD[b]], sig=False)
                    for fc in range(16):
                        k.op("pe", lambda e, b=b, sc=sc, fc=fc, wb=wb: e.matmul(PD[b][:, 0:256], lhsT=AT[:, fc, sc * 128:(sc + 1) * 128], rhs=wb[:, fc, :], start=False, stop=(fc == 15)),
                             reads=[rAT[fc], rwb], writes=[rPD[b]], sig=(fc == 15))
                    yb_ = ycnt % 4
                    ycnt += 1
                    if yb_ % 2 == 0:
                        k.op("act", lambda e, b=b, yb_=yb_: e.copy(out=YST[yb_][:], in_=PD[b][:, 0:256]), reads=[rPD[b]], writes=[rYST[yb_]])
                    else:
                        k.op("dve", lambda e, b=b, yb_=yb_: e.tensor_copy(out=YST[yb_][:], in_=PD[b][:, 0:256]), reads=[rPD[b]], writes=[rYST[yb_]])
                    k.dma("sp", ysp[ex, half * HSLOT + sc * 128:half * HSLOT + (sc + 1) * 128, dp * 256:(dp + 1) * 256], YST[yb_][:], reads=[rYST[yb_]], key="ys%d" % yb_)
        ysp_toks = [k.dsem["ys%d" % i] for i in range(4)]
        xtf = XT[:].rearrange("p a b -> p (a b)")
        atf = AT[:].rearrange("p a b -> p (a b)")
        Y1 = xtf[:, 0:EPC * D].rearrange("p (e d) -> p e d", e=EPC)
        Y2 = xtf[:, EPC * D:2 * EPC * D].rearrange("p (e d) -> p e d", e=EPC)
        SG1 = atf[:, 0:EPC * 1024].rearrange("p (e t) -> p e t", e=EPC)
        SG2 = atf[:, EPC * 1024:2 * EPC * 1024].rearrange("p (e t) -> p e t", e=EPC)
        rY12 = Res(); rSG = Res()
        first = True
        ocnt = 0
        for j in range(NBLK):
            for ex in range(EPC):
                wr = ([rY12, rXT] + rYST) if first else [rY12]
                tok = k.dma("sp", Y1[:, ex, :], ysp[ex, j * CAP:j * CAP + 128, :], writes=wr, key="y1")
                k.dma("sp", Y2[:, ex, :], ysp[ex, j * CAP + 128:(j + 1) * CAP, :], writes=[rY12], key="y1")
                first = False
            for ex in range(EPC):
                sb_ = scnt % 2
                scnt += 1
                for tc in range(8):
                    k.op("dve", lambda e, sb_=sb_, tc=tc, j=j, ex=ex: e.tensor_scalar(out=S[sb_][:, tc, :], in0=IOTA[:], scalar1=POSM[:, j * 8 + tc, ex:ex + 1], scalar2=GC[:, j * 8 + tc, ex:ex + 1], op0=ALU.is_equal, op1=ALU.mult),
                         reads=[rIOTA, rPOSM, rGC], writes=[rS[sb_]])
                for tc in range(8):
                    k.op("pe", lambda e, sb_=sb_, tc=tc: e.transpose(out=PT[0][:, tc * 128:(tc + 1) * 128], in_=S[sb_][:, tc, 0:128], identity=ID[:]), reads=[rS[sb_], rID], writes=[rPT[0]], sig=(tc == 7))
                for tc in range(8):
                    k.op("pe", lambda e, sb_=sb_, tc=tc: e.transpose(out=PT[1][:, tc * 128:(tc + 1) * 128], in_=S[sb_][:, tc, 128:CAP], identity=ID[:]), reads=[rS[sb_], rID], writes=[rPT[1]], sig=(tc == 7))
                k.op("act", lambda e, ex=ex: e.copy(out=SG1[:, ex, :], in_=PT[0][:]), reads=[rPT[0]], writes=[rSG] + rAT)
                k.op("dve", lambda e, ex=ex: e.tensor_copy(out=SG2[:, ex, :], in_=PT[1][:]), reads=[rPT[1]], writes=[rSG])
            for tc in range(8):
                ob = ocnt % 2
                ocnt += 1
                for dg in range(4):
                    pb_, rpb_ = (PA[dg], rPA[dg]) if dg < 2 else (PB[dg - 2], rPB[dg - 2])
                    n = 0
                    for ex in range(EPC):
                        k.op("pe", lambda e, pb_=pb_, ex=ex, tc=tc, dg=dg, n=n: e.matmul(pb_[:], lhsT=SG1[:, ex, tc * 128:(tc + 1) * 128], rhs=Y1[:, ex, dg * 512:(dg + 1) * 512], start=(n == 0), stop=False),
                             reads=[rSG, rY12], writes=[rpb_], sig=False)
                        n += 1
                        k.op("pe", lambda e, pb_=pb_, ex=ex, tc=tc, dg=dg, n=n: e.matmul(pb_[:], lhsT=SG2[:, ex, tc * 128:(tc + 1) * 128], rhs=Y2[:, ex, dg * 512:(dg + 1) * 512], start=False, stop=(n == 2 * EPC - 1)),
                             reads=[rSG, rY12], writes=[rpb_], sig=(n == 2 * EPC - 1))
                        n += 1
                    if dg % 2 == 0:
                        k.op("act", lambda e, pb_=pb_, ob=ob, dg=dg: e.copy(out=OUT[ob][:, dg * 512:(dg + 1) * 512], in_=pb_[:]), reads=[rpb_], writes=[rOUT[ob]])
                    else:
                        k.op("dve", lambda e, pb_=pb_, ob=ob, dg=dg: e.tensor_copy(out=OUT[ob][:, dg * 512:(dg + 1) * 512], in_=pb_[:]), reads=[rpb_], writes=[rOUT[ob]])
                k.dma("sp", y_out[j * 1024 + tc * 128:j * 1024 + (tc + 1) * 128, :], OUT[ob][:], reads=[rOUT[ob]], key="oy%d" % ob, is_out=True)
        k.finish()
    return nc


def build_post():
    nc = bass.Bass("TRN2", target_bir_lowering=False)
    dt_in = lambda name, shape, dt=F32: nc.dram_tensor(name, list(shape), dt, kind="ExternalInput").ap()
    h1 = dt_in("h1", [TPC, D])
    parts = dt_in("parts", [NCORES, TPC, D])
    pin = dt_in("p", [TPC, PLE])
    identb = dt_in("identb", [128, 128], BF16)
    ln2g = dt_in("ln2_g", [D]); ln2b = dt_in("ln2_b", [D])
    ln3g = dt_in("ln3_g", [D]); ln3b = dt_in("ln3_b", [D])
    wg = dt_in("w_ple_gate", [D, D])
    bg = dt_in("b_ple_gate", [D])
    wp = dt_in("w_ple_proj", [PLE, D])
    h3 = nc.dram_tensor("h3", [TPC, D], F32, kind="ExternalOutput").ap()
    NJ = TPC // 128
    with ExitStack() as es:
        k = KB(nc, es)
        ID = k.sb("ID", [128, 128], BF16); rID = Res()
        LG = k.sb("LG", [128, D], F32); rLG = Res()
        LB = k.sb("LB", [128, D], F32); rLB = Res()
        BG = k.sb("BG", [128, D], F32); rBG = Res()
        WP = k.sb("WP", [128, 2, D], BF16); rWP = Res()
        H = k.sb("H", [128, NJ, D], F32); rH = [Res() for _ in range(NJ)]
        PBUF = [k.sb("PBUF%d" % i, [128, D], F32) for i in range(3)]; rPBUF = [Res() for _ in range(3)]
        HT = k.sb("HT", [128, 16, 512], BF16); rHT = [Res() for _ in range(4)]
        PTT = k.sb("PTT", [128, 2, 512], BF16); rPTT = [Res() for _ in range(4)]
        hb = k.sb("hb", [128, D], BF16); rhb = Res()
        p32 = k.sb("p32", [128, PLE], F32); rp32 = Res()
        pb = k.sb("pb", [128, PLE], BF16); rpb = Res()
        t1 = [k.sb("t1_%d" % i, [128, 256], F32) for i in range(2)]; rt1 = [Res(), Res()]
        ring = WRing(k, "wq", 3)
        lt = ln_tmp(k, "lt")
        PT = [k.ps("PT%d" % i, [128, 1024], BF16) for i in range(2)]; rPT = [Res(), Res()]
        PP = [k.ps("PP%d" % i, [128, 512], F32) for i in range(2)]; rPP = [Res(), Res()]
        ptc = [0]; ppc = [0]

        def nextPT():
            i = ptc[0] % 2; ptc[0] += 1
            return PT[i], rPT[i]

        def nextPP():
            i = ppc[0] % 2; ppc[0] += 1
            return PP[i], rPP[i]

        k.dma("sp", ID[:], identb, writes=[rID], key="c0")
        k.dma("sp", LG[:], ln2g.partition_broadcast(128), writes=[rLG], key="c1")
        k.dma("sp", LB[:], ln2b.partition_broadcast(128), writes=[rLB], key="c2")
        k.dma("sp", BG[:], bg.partition_broadcast(128), writes=[rBG], key="c3")
        k.dma("pool", WP[:], wp.rearrange("(kc p) n -> p kc n", p=128), writes=[rWP], key="c4")
        pc = 0
        for j in range(NJ):
            k.dma("sp", H[:, j, :], h1[j * 128:(j + 1) * 128, :], writes=[rH[j]], key="h%d" % j)
            k.op("act", lambda e, j=j: e.mul(out=H[:, j, :], in_=H[:, j, :], mul=ALPHA), reads=[rH[j]], writes=[rH[j]])
            for c in range(NCORES):
                b = pc % 3
                pc += 1
                k.dma("sp", PBUF[b][:], parts[c, j * 128:(j + 1) * 128, :], writes=[rPBUF[b]], key="pb%d" % b)
                eng = "dve" if c % 2 == 0 else "pool"
                k.op(eng, lambda e, b=b, j=j: e.tensor_tensor(out=H[:, j, :], in0=H[:, j, :], in1=PBUF[b][:], op=ALU.add), reads=[rH[j], rPBUF[b]], writes=[rH[j]])
            emit_layernorm(k, H[:, j, :], rH[j], LG, rLG, LB, rLB, lt)
        k.dma("sp", LG[:], ln3g.partition_broadcast(128), writes=[rLG], key="c1")
        k.dma("sp", LB[:], ln3b.partition_broadcast(128), writes=[rLB], key="c2")
        for grp in ([0, 1, 2, 3], [4, 5, 6, 7]):
            wb_next, rwb_next = ring.load([(0, 256, wg[:, 0:256])])
            for c, j in enumerate(grp):
                k.op("act", lambda e, j=j: e.copy(out=hb[:], in_=H[:, j, :]), reads=[rH[j]], writes=[rhb])
                for half in range(2):
                    pt, rpt = nextPT()
                    for i in range(8):
                        kc = half * 8 + i
                        k.op("pe", lambda e, pt=pt, kc=kc, i=i: e.transpose(out=pt[:, i * 128:(i + 1) * 128], in_=hb[:, kc * 128:(kc + 1) * 128], identity=ID[:]),
                             reads=[rhb, rID], writes=[rpt], sig=(i == 7))
                    k.op("dve", lambda e, pt=pt, half=half, c=c: e.tensor_copy(out=HT[:, half * 8:(half + 1) * 8, c * 128:(c + 1) * 128], in_=pt[:].rearrange("p (a b) -> p a b", a=8)),
                         reads=[rpt], writes=[rHT[c]])
                k.op("act", lambda e, j=j: e.mul(out=H[:, j, :], in_=H[:, j, :], mul=ALPHA), reads=[rH[j]], writes=[rH[j]])
                k.dma("sp", p32[:], pin[j * 128:(j + 1) * 128, :], writes=[rp32], key="pp")
                k.op("act", lambda e: e.copy(out=pb[:], in_=p32[:]), reads=[rp32], writes=[rpb])
                pt, rpt = nextPT()
                for i in range(2):
                    k.op("pe", lambda e, pt=pt, i=i: e.transpose(out=pt[:, i * 128:(i + 1) * 128], in_=pb[:, i * 128:(i + 1) * 128], identity=ID[:]),
                         reads=[rpb, rID], writes=[rpt], sig=(i == 1))
                k.op("dve", lambda e, pt=pt, c=c: e.tensor_copy(out=PTT[:, :, c * 128:(c + 1) * 128], in_=pt[:, 0:256].rearrange("p (a b) -> p a b", a=2)),
                     reads=[rpt], writes=[rPTT[c]])
            for gi in range(8):
                wb, rwb = wb_next, rwb_next
                if gi < 7:
                    wb_next, rwb_next = ring.load([(0, 256, wg[:, (gi + 1) * 256:(gi + 2) * 256])])
                cols = slice(gi * 256, (gi + 1) * 256)
                for c, j in enumerate(grp):
                    pp, rpp = nextPP()
                    for kc in range(16):
                        k.op("pe", lambda e, pp=pp, kc=kc, c=c, wb=wb: e.matmul(pp[:, 0:256], lhsT=HT[:, kc, c * 128:(c + 1) * 128], rhs=wb[:, kc, :], start=(kc == 0), stop=(kc == 15)),
                             reads=[rHT[c], rwb], writes=[rpp], sig=False)
                    for kc in range(2):
                        k.op("pe", lambda e, pp=pp, kc=kc, c=c, cols=cols: e.matmul(pp[:, 256:512], lhsT=PTT[:, kc, c * 128:(c + 1) * 128], rhs=WP[:, kc, cols], start=(kc == 0), stop=(kc == 1)),
                             reads=[rPTT[c], rWP], writes=[rpp], sig=(kc == 1))
                    b = (gi * 4 + c) % 2
                    k.op("dve", lambda e, pp=pp, b=b, cols=cols: e.tensor_tensor(out=t1[b][:], in0=pp[:, 0:256], in1=BG[:, cols], op=ALU.add), reads=[rpp, rBG], writes=[rt1[b]])
                    k.op("act", lambda e, b=b: e.activation(out=t1[b][:], in_=t1[b][:], func=AF.Sigmoid), reads=[rt1[b]], writes=[rt1[b]])
                    k.op("dve", lambda e, pp=pp, b=b: e.tensor_tensor(out=t1[b][:], in0=t1[b][:], in1=pp[:, 256:512], op=ALU.mult), reads=[rt1[b], rpp], writes=[rt1[b]])
                    k.op("pool", lambda e, b=b, j=j, cols=cols: e.tensor_tensor(out=H[:, j, cols], in0=H[:, j, cols], in1=t1[b][:], op=ALU.add), reads=[rt1[b], rH[j]], writes=[rH[j]])
            for j in grp:
                emit_layernorm(k, H[:, j, :], rH[j], LG, rLG, LB, rLB, lt)
                k.dma("sp", h3[j * 128:(j + 1) * 128, :], H[:, j, :], reads=[rH[j]], key="oh", is_out=True)
        k.finish()
    return nc


def _consts():
    q = np.arange(128)[:, None]
    j = np.arange(128)[None, :]
    prev = np.where(j > q, 0.0, NEG).astype(np.float32)
    cur = np.where(j <= q, 0.0, NEG).astype(np.float32)
    std = np.concatenate([prev, cur], axis=1)
    first = np.concatenate([np.full((128, 128), NEG, np.float32), cur], axis=1)
    half = 8
    invf = np.exp(-np.log(500000.0) * np.arange(half, dtype=np.float32) * (2.0 / 16)).astype(np.float32)
    return dict(
        identb=np.eye(128).astype(ml_dtypes.bfloat16),
        identf=np.eye(128, dtype=np.float32),
        trild=(j <= q).astype(np.float32),
        invfd=np.ascontiguousarray(np.broadcast_to(invf[None, :], (128, 8))),
        mask_std=std, mask_first=first,
        iotad=np.ascontiguousarray(np.broadcast_to(np.arange(CAP, dtype=np.float32)[None, :], (128, CAP))),
        sud=(q < j).astype(ml_dtypes.bfloat16),
        onesd=np.ones((128, 128), ml_dtypes.bfloat16),
    )


def mixer_inputs(layer, h_full, positions, prm):
    cst = _consts()
    maps = []
    pos_flat = positions.reshape(-1)
    for c in range(NCORES):
        t0 = c * TPC
        seq_start = (t0 % SEQ) == 0
        xin = np.zeros((NCH * 128, D), np.float32)
        pos = np.zeros((NCH * 128,), np.int32)
        xin[128:] = h_full[t0:t0 + TPC]
        pos[128:] = pos_flat[t0:t0 + TPC]
        if not seq_start:
            xin[:128] = h_full[t0 - 128:t0]
            pos[:128] = pos_flat[t0 - 128:t0]
        m = dict(
            xin=xin, posi=np.ascontiguousarray(pos.reshape(NCH, 128).T),
            maskd=np.ascontiguousarray(np.stack([cst["mask_first"] if seq_start else cst["mask_std"], cst["mask_std"]], axis=1)),
            identb=cst["identb"], identf=cst["identf"], trild=cst["trild"], invfd=cst["invfd"],
            w_in=prm["w_in"][layer], sinks=prm["sinks"][layer], w_s=prm["w_s"][layer],
            bsT=np.ascontiguousarray(prm["b_s"][layer].T),
            vnorm_g=np.ascontiguousarray(prm["vnorm_g"][layer].reshape(-1)), vnorm_b=np.ascontiguousarray(prm["vnorm_b"][layer].reshape(-1)),
            gnT=np.ascontiguousarray(np.concatenate([prm["gnorm_attn"][layer], prm["gnorm_gmlp"][layer]]).reshape(16, 128).T),
            w_o=prm["w_o"][layer], ln1_g=prm["ln1_g"][layer], ln1_b=prm["ln1_b"][layer],
            w_router=prm["w_router"][layer], b_router=prm["b_router"][layer],
        )
        if layer == 0:
            m["ln_in_g"] = prm["ln_in_g"]
            m["ln_in_b"] = prm["ln_in_b"]
        maps.append(m)
    return maps


_PROGS = {}


def _prog(name, fn):
    if name not in _PROGS:
        _PROGS[name] = fn()
    return _PROGS[name]


def kernel(**inp):
    prm = {k_: np.asarray(v) for k_, v in inp.items()}
    cores = list(range(NCORES))
    cst = _consts()
    h_full = np.ascontiguousarray(prm["x"].reshape(NTOK, D))
    for layer in range(DEPTH):
        nc = _prog("mix%d" % (layer == 0), lambda: build_mixer(layer == 0))
        res = run_bass_kernel_spmd(nc, mixer_inputs(layer, h_full, prm["positions"], prm), core_ids=cores).results
        h1 = [res[c]["h1"] for c in cores]
        hbt = np.concatenate([res[c]["hbt"] for c in cores], axis=0)
        G = np.concatenate([res[c]["gates"] for c in cores], axis=0)
        del res
        nc = _prog("moe2", build_moe2)
        maps = []
        for c in cores:
            es_ = slice(c * EPC, (c + 1) * EPC)
            maps.append(dict(
                hbt=hbt, gC=np.ascontiguousarray(G[:, es_].reshape(NTOK // 128, 128, EPC).transpose(1, 0, 2)),
                iotad=cst["iotad"], sud=cst["sud"], onesd=cst["onesd"], identb=cst["identb"],
                w_gu=prm["w_gu"][layer, es_],
                bguT=np.ascontiguousarray(prm["b_gu"][layer, es_].reshape(EPC, 32, 128).transpose(2, 0, 1)),
                w_down=prm["w_down"][layer, es_], b_down=prm["b_down"][layer, es_]))
        res = run_bass_kernel_spmd(nc, maps, core_ids=cores).results
        yp = [res[c]["ypart"] for c in cores]
        del res, maps
        nc = _prog("post", build_post)
        p_l = prm["p"][layer].reshape(NTOK, PLE)
        maps = []
        for c in cores:
            ts = slice(c * TPC, (c + 1) * TPC)
            maps.append(dict(
                h1=h1[c], parts=np.ascontiguousarray(np.stack([yp[e][ts] for e in cores], axis=0)),
                p=np.ascontiguousarray(p_l[ts]), identb=cst["identb"],
                ln2_g=prm["ln2_g"][layer], ln2_b=prm["ln2_b"][layer], ln3_g=prm["ln3_g"][layer], ln3_b=prm["ln3_b"][layer],
                w_ple_gate=prm["w_ple_gate"][layer], b_ple_gate=prm["b_ple_gate"][layer], w_ple_proj=prm["w_ple_proj"][layer]))
        del yp
        res = run_bass_kernel_spmd(nc, maps, core_ids=cores).results
        h_full = np.concatenate([res[c]["h3"] for c in cores], axis=0)
        del res, maps
    return h_full.reshape(BATCH, SEQ, D).astype(np.float32)
```

```python
import numpy as np
import ml_dtypes
from contextlib import ExitStack
import concourse.bass as bass
import concourse.mybir as mybir
from concourse.bass_utils import run_bass_kernel_spmd

F32 = mybir.dt.float32
BF16 = mybir.dt.bfloat16
I32 = mybir.dt.int32
ALU = mybir.AluOpType
AF = mybir.ActivationFunctionType
AX = mybir.AxisListType

NCORES = 8
D = 2048
DEPTH = 2
SEQ = 2048
BATCH = 4
NTOK = BATCH * SEQ
TPC = NTOK // NCORES
D_ATTN = 1024
D_GMLP = 1024
D_IN = 3328
NE = 32
EPC = NE // NCORES
PLE = 256
ALPHA = (2.0 * DEPTH) ** 0.25
EPS = 1e-5
PI = float(np.pi)
NEG = -30000.0


class Res:
    __slots__ = ("w", "r")

    def __init__(self):
        self.w = None
        self.r = []


class KB:
    def __init__(self, nc, es):
        self.nc = nc
        self.es = es
        self.eng = {"pe": nc.tensor, "act": nc.scalar, "dve": nc.vector, "pool": nc.gpsimd, "sp": nc.sync}
        self.ops = {k: [] for k in self.eng}
        self.sem = {k: es.enter_context(nc.semaphore("s_" + k)) for k in self.eng}
        self.cnt = {k: 0 for k in self.eng}
        self.waited = {k: {} for k in self.eng}
        self.dsem = {}
        self.out_toks = []

    def sb(self, name, shape, dt):
        return self.es.enter_context(self.nc.sbuf_tensor(name, list(shape), dt))

    def ps(self, name, shape, dt):
        return self.es.enter_context(self.nc.psum_tensor(name, list(shape), dt))

    def _deps(self, e, reads, writes):
        toks = []
        for r in reads:
            if r.w is not None:
                toks.append(r.w)
        for w in writes:
            if w.w is not None:
                toks.append(w.w)
            toks.extend(w.r)
        waits = []
        wd = self.waited[e]
        for (skey, sem, val, teng) in toks:
            if teng == e and e == "pe":
                continue
            if wd.get(skey, 0) >= val:
                continue
            wd[skey] = val
            waits.append((sem, val))
        return waits

    def op(self, e, fn, reads=(), writes=(), sig=True):
        waits = self._deps(e, reads, writes)
        if sig:
            self.cnt[e] += 1
            val = self.cnt[e]
        else:
            val = self.cnt[e] + 1
        tok = (e, self.sem[e], val, e)
        for r in reads:
            r.r.append(tok)
        for w in writes:
            w.w = tok
            w.r = []
        self.ops[e].append((waits, fn, (self.sem[e], 1) if sig else None))
        return tok

    def dma(self, e, out, in_, reads=(), writes=(), key="misc", is_out=False):
        waits = self._deps(e, reads, writes)
        if key not in self.dsem:
            self.dsem[key] = [self.es.enter_context(self.nc.semaphore("d_" + str(key))), 0]
        ds = self.dsem[key]
        ds[1] += 16
        tok = ("d_" + str(key), ds[0], ds[1], "dma")
        for r in reads:
            r.r.append(tok)
        for w in writes:
            w.w = tok
            w.r = []
        self.ops[e].append((waits, (lambda eng, o=out, i=in_: eng.dma_start(out=o, in_=i)), (ds[0], 16)))
        if is_out:
            self.out_toks.append(tok)
        return tok

    def finish(self):
        waits = []
        seen = {}
        for (skey, sem, val, teng) in self.out_toks:
            if seen.get(skey, (None, 0))[1] < val:
                seen[skey] = (sem, val)
        for skey, (sem, val) in seen.items():
            waits.append((sem, val))
        self.ops["sp"].append((waits, None, None))
        with self.nc.Block() as block:
            def mk(name):
                def body(eng):
                    for waits, fn, inc in self.ops[name]:
                        for sem, val in waits:
                            eng.wait_ge(sem, val)
                        if fn is None:
                            continue
                        ins = fn(eng)
                        if inc is not None:
                            ins.then_inc(inc[0], inc[1])
                return body
            block.tensor(mk("pe"))
            block.scalar(mk("act"))
            block.vector(mk("dve"))
            block.gpsimd(mk("pool"))
            block.sync(mk("sp"))


def bcast_rows(ap, n=128):
    return ap.partition_broadcast(n)


def emit_layernorm(k, X, rX, G, rG, B, rB, tmp):
    st, rst, mv, rmv, rs, rrs = tmp
    for i in range(4):
        k.op("dve", lambda e, i=i: e.bn_stats(out=st[:, i, :], in_=X[:, i * 512:(i + 1) * 512]),
             reads=[rX], writes=[rst])
    k.op("dve", lambda e: e.bn_aggr(out=mv[:], in_=st[:].rearrange("p a b -> p (a b)")), reads=[rst], writes=[rmv])
    k.op("act", lambda e: e.activation(out=rs[:], in_=mv[:, 1:2], func=AF.Sqrt, bias=EPS, scale=1.0),
         reads=[rmv], writes=[rrs])
    k.op("dve", lambda e: e.reciprocal(out=rs[:], in_=rs[:]), reads=[rrs], writes=[rrs])
    k.op("dve", lambda e: e.tensor_scalar(out=X, in0=X, scalar1=mv[:, 0:1], scalar2=rs[:, 0:1],
                                          op0=ALU.subtract, op1=ALU.mult), reads=[rX, rmv, rrs], writes=[rX])
    k.op("pool", lambda e: e.tensor_tensor(out=X, in0=X, in1=G[:], op=ALU.mult), reads=[rX, rG], writes=[rX])
    k.op("pool", lambda e: e.tensor_tensor(out=X, in0=X, in1=B[:], op=ALU.add), reads=[rX, rB], writes=[rX])


def ln_tmp(k, name):
    return (k.sb(name + "_st", [128, 4, 6], F32), Res(), k.sb(name + "_mv", [128, 2], F32), Res(),
            k.sb(name + "_rs", [128, 1], F32), Res())


class WRing:
    def __init__(self, k, name, nslots, kc=16, ncol=256):
        self.k = k
        self.n = nslots
        self.buf = [k.sb("%s%d" % (name, i), [128, kc, ncol], BF16) for i in range(nslots)]
        self.res = [Res() for _ in range(nslots)]
        self.i = 0
        self.name = name

    def load(self, parts):
        s = self.i % self.n
        self.i += 1
        for (c0, c1, src) in parts:
            self.k.dma("pool", self.buf[s][:, :, c0:c1], src.rearrange("(kc p) n -> p kc n", p=128),
                       writes=[self.res[s]], key="%s%d" % (self.name, s))
        return self.buf[s], self.res[s]


NCH = 9
GROUPS = [[0, 1, 2, 3, 4], [5, 6, 7, 8]]


def build_mixer(layer0, stop=None):
    nc = bass.Bass("TRN2", target_bir_lowering=False)
    dt_in = lambda name, shape, dt=F32: nc.dram_tensor(name, list(shape), dt, kind="ExternalInput").ap()
    dt_out = lambda name, shape, dt=F32: nc.dram_tensor(name, list(shape), dt, kind="ExternalOutput").ap()
    xin = dt_in("xin", [NCH * 128, D])
    posi = dt_in("posi", [128, NCH], I32)
    maskd = dt_in("maskd", [128, 2, 256])
    identb = dt_in("identb", [128, 128], BF16)
    identf = dt_in("identf", [128, 128])
    trild = dt_in("trild", [128, 128])
    invfd = dt_in("invfd", [128, 8])
    if layer0:
        lning = dt_in("ln_in_g", [D])
        lninb = dt_in("ln_in_b", [D])
    w_in = dt_in("w_in", [D, D_IN])
    sinksd = dt_in("sinks", [16])
    w_s = dt_in("w_s", [8, 128, 128])
    bsT = dt_in("bsT", [128, 8])
    vng = dt_in("vnorm_g", [1024])
    vnb = dt_in("vnorm_b", [1024])
    gnT = dt_in("gnT", [128, 16])
    w_o = dt_in("w_o", [D, D])
    ln1g = dt_in("ln1_g", [D])
    ln1b = dt_in("ln1_b", [D])
    w_r = dt_in("w_router", [D, NE])
    b_r = dt_in("b_router", [NE])
    h1_out = dt_out("h1", [TPC, D])
    hT_out = dt_out("hT", [16, 128, TPC], BF16)
    hbt_out = dt_out("hbt", [TPC, D], BF16)
    g_out = dt_out("gates", [TPC, NE])

    with ExitStack() as es:
        k = KB(nc, es)
        ID = k.sb("ID", [128, 128], BF16); rID = Res()
        MASK = k.sb("MASK", [128, 2, 256], F32); rMASK = Res()
        TRIL = k.sb("TRIL", [128, 128], F32); rTRIL = Res()
        INVF = k.sb("INVF", [128, 8], F32); rINVF = Res()
        POSI = k.sb("POSI", [128, NCH], I32); rPOSI = Res()
        LG = k.sb("LG", [128, D], F32); rLG = Res()
        LB = k.sb("LB", [128, D], F32); rLB = Res()
        SINK = k.sb("SINK", [128, 16], F32); rSINK = Res()
        VNG = k.sb("VNG", [128, 1024], F32); rVNG = Res()
        VNB = k.sb("VNB", [128, 1024], F32); rVNB = Res()
        BST = k.sb("BST", [128, 8], F32); rBST = Res()
        GNT = k.sb("GNT", [128, 16], F32); rGNT = Res()
        WR = k.sb("WR", [128, 16, NE], F32); rWR = Res()
        BR = k.sb("BR", [128, NE], F32); rBR = Res()
        WST = k.sb("WST", [128, 8, 128], BF16); rWST = Res()
        H = k.sb("H", [128, NCH, D], F32); rH = [Res() for _ in range(NCH)]
        COS = k.sb("COS", [128, NCH, 4, 8], F32); rCOS = Res()
        SIN = k.sb("SIN", [128, NCH, 4, 8], F32); rSIN = Res()
        KT = k.sb("KT", [128, 2, NCH * 128], BF16); rKT = [Res() for _ in range(NCH)]
        V = k.sb("V", [128, NCH, 128], BF16); rV = [Res() for _ in range(NCH)]
        HT = k.sb("HT", [128, 16, 640], BF16); rHT = [Res() for _ in range(5)]
        YT = k.sb("YT", [128, 16, 512], BF16); rYT = [Res() for _ in range(4)]
        SSQ = k.sb("SSQ", [128, NCH, 12], F32); rSSQ = [Res() for _ in range(NCH)]
        ring = WRing(k, "wr", 3)
        lt = ln_tmp(k, "lt")
        hb = k.sb("hb", [128, D], BF16); rhb = Res()
        t8 = [k.sb("t8_%d" % i, [128, 4, 8], F32) for i in range(4)]; rt8 = [Res() for _ in range(4)]
        qb = k.sb("qb", [128, 4, 64], BF16); rqb = Res()
        kd = k.sb("kd", [128, 2, 64], BF16); rkd = Res()
        qT = k.sb("qT", [128, 4, 128], BF16); rqT = Res()
        sm = k.sb("sm", [128, 4, 256], F32); rsm = Res()
        eb = k.sb("eb", [128, 4, 256], BF16); reb = Res()
        eT = k.sb("eT", [128, 2, 4, 128], BF16); reT = Res()
        mx = k.sb("mx", [128, 4], F32); rmx = Res()
        sx = k.sb("sx", [128, 4], F32); rsx = Res()
        es_ = k.sb("es", [128, 4], F32); res_ = Res()
        o32 = k.sb("o32", [128, 256], F32); ro32 = Res()
        yb = k.sb("yb", [128, 256], BF16); ryb = Res()
        g1 = k.sb("g1", [128, 256], F32); rg1 = Res()
        junk, rjunk = g1, rg1
        g2 = k.sb("g2", [128, 256], F32); rg2 = Res()
        g3 = k.sb("g3", [128, 128], F32); rg3 = Res()
        vst = k.sb("vst", [128, 6], F32); rvst = Res()
        vmv = k.sb("vmv", [128, 2], F32); rvmv = Res()
        vrs = k.sb("vrs", [128, 1], F32); rvrs = Res()
        vh = k.sb("vh", [128, 128], BF16); rvh = Res()
        wtmp = k.sb("wtmp", [128, 128], F32); rwtmp = Res()
        wtb = k.sb("wtb", [128, 128], BF16); rwtb = Res()
        rsa = k.sb("rsa", [128, 2], F32); rrsa = Res()
        hloT = k.sb("hloT", [128, 16, 128], BF16); rhloT = Res()
        hlo = k.sb("hlo", [128, D], BF16); rhlo = Res()
        WRH = k.sb("WRH", [128, 16, NE], BF16); rWRH = Res()
        WRL = k.sb("WRL", [128, 16, NE], BF16); rWRL = Res()
        hbT = k.sb("hbT", [128, 16, 128], BF16); rhbT = Res()
        lg = k.sb("lg", [128, NE], F32); rlg = Res()
        m8 = k.sb("m8", [128, 8], F32); rm8 = Res()
        msk = k.sb("msk", [128, NE], F32); rmsk = Res()
        nm = k.sb("nm", [128, 1], F32); rnm = Res()
        gs = k.sb("gs", [128, 1], F32); rgs = Res()
        posf = k.sb("posf", [128, NCH], F32); rposf = Res()
        ang = k.sb("ang", [128, NCH, 8], F32); rang = Res()
        qf = k.sb("qf", [128, NCH, 8], F32); rqf = Res()
        qi = k.sb("qi", [128, NCH, 8], I32); rqi = Res()
        PT = [k.ps("PT%d" % i, [128, 1024], BF16) for i in range(2)]; rPT = [Res(), Res()]
        PP = [k.ps("PP%d" % i, [128, 512], F32) for i in range(2)]; rPP = [Res(), Res()]
        PS = k.ps("PS", [128, 4, 256], F32); rPS = Res()
        PV = k.ps("PV", [128, 512], F32); rPV = Res()
        PR = k.ps("PR", [128, 512], F32); rPR = Res()
        ptc = [0]
        ppc = [0]

        def nextPT():
            i = ptc[0] % 2; ptc[0] += 1
            return PT[i], rPT[i]

        def nextPP():
            i = ppc[0] % 2; ppc[0] += 1
            return PP[i], rPP[i]

        k.op("pool", lambda e: e.memset(SSQ[:], 0.0), writes=rSSQ)
        ld = lambda dst, src, r, key: k.dma("sp", dst, src, writes=[r], key=key)
        ld(ID[:], identb, rID, "c0"); ld(MASK[:], maskd, rMASK, "c2")
        ld(TRIL[:], trild, rTRIL, "c3"); ld(INVF[:], invfd, rINVF, "c4"); ld(POSI[:], posi, rPOSI, "c5")
        ld(SINK[:], sinksd.partition_broadcast(128), rSINK, "c6")
        ld(VNG[:], vng.partition_broadcast(128), rVNG, "c7"); ld(VNB[:], vnb.partition_broadcast(128), rVNB, "c8")
        ld(BST[:], bsT, rBST, "c9"); ld(GNT[:], gnT, rGNT, "c10")
        ld(WR[:], w_r.rearrange("(kc p) n -> p kc n", p=128), rWR, "c11")
        ld(BR[:], b_r.partition_broadcast(128), rBR, "c12")
        k.op("dve", lambda e: e.tensor_copy(out=WRH[:], in_=WR[:]), reads=[rWR], writes=[rWRH])
        k.op("dve", lambda e: e.tensor_tensor(out=WRL[:], in0=WR[:], in1=WRH[:], op=ALU.subtract), reads=[rWR, rWRH], writes=[rWRL])
        if layer0:
            ld(LG[:], lning.partition_broadcast(128), rLG, "c13"); ld(LB[:], lninb.partition_broadcast(128), rLB, "c14")
        for j in range(NCH):
            k.dma("sp", H[:, j, :], xin[j * 128:(j + 1) * 128, :], writes=[rH[j]], key="x%d" % j)
            if layer0:
                emit_layernorm(k, H[:, j, :], rH[j], LG, rLG, LB, rLB, lt)
        if stop == 'ln':
            k.finish()
            return nc
        ld(LG[:], ln1g.partition_broadcast(128), rLG, "c13"); ld(LB[:], ln1b.partition_broadcast(128), rLB, "c14")
        k.op("dve", lambda e: e.tensor_copy(out=posf[:], in_=POSI[:]), reads=[rPOSI], writes=[rposf])
        for which, TAB, rTAB in ((0, SIN, rSIN), (1, COS, rCOS)):
            for j in range(NCH):
                k.op("dve", lambda e, j=j, which=which: e.tensor_scalar(out=ang[:, j, :], in0=INVF[:], scalar1=posf[:, j:j + 1],
                                                           scalar2=(PI / 2 if which else 0.0), op0=ALU.mult, op1=ALU.add),
                     reads=[rINVF, rposf], writes=[rang])
            k.op("dve", lambda e: e.tensor_scalar(out=qf[:], in0=ang[:], scalar1=1.0 / (2 * PI), scalar2=None, op0=ALU.mult),
                 reads=[rang], writes=[rqf])
            k.op("dve", lambda e: e.tensor_copy(out=qi[:], in_=qf[:]), reads=[rqf], writes=[rqi])
            k.op("dve", lambda e: e.tensor_copy(out=qf[:], in_=qi[:]), reads=[rqi], writes=[rqf])
            k.op("dve", lambda e: e.scalar_tensor_tensor(out=ang[:], in0=qf[:], scalar=-2 * PI, in1=ang[:], op0=ALU.mult, op1=ALU.add),
                 reads=[rqf, rang], writes=[rang])
            k.op("dve", lambda e: e.tensor_scalar(out=qf[:], in0=ang[:], scalar1=PI, scalar2=None, op0=ALU.is_gt), reads=[rang], writes=[rqf])
            k.op("dve", lambda e: e.scalar_tensor_tensor(out=ang[:], in0=qf[:], scalar=-2 * PI, in1=ang[:], op0=ALU.mult, op1=ALU.add),
                 reads=[rqf, rang], writes=[rang])
            k.op("dve", lambda e: e.tensor_scalar(out=qf[:], in0=ang[:], scalar1=-PI, scalar2=None, op0=ALU.is_lt), reads=[rang], writes=[rqf])
            k.op("dve", lambda e: e.scalar_tensor_tensor(out=ang[:], in0=qf[:], scalar=2 * PI, in1=ang[:], op0=ALU.mult, op1=ALU.add),
                 reads=[rqf, rang], writes=[rang])
            for hh in range(4):
                k.op("act", lambda e, hh=hh, TAB=TAB: e.activation(out=TAB[:, :, hh, :], in_=ang[:], func=AF.Sin),
                     reads=[rang], writes=[rTAB])
        if stop == 'rope':
            k.finish()
            return nc
        for h in range(8):
            k.dma("sp", wtmp[:], w_s[h], writes=[rwtmp], key="ws")
            k.op("dve", lambda e: e.tensor_tensor(out=wtb[:], in0=wtmp[:], in1=TRIL[:], op=ALU.mult),
                 reads=[rwtmp, rTRIL], writes=[rwtb])
            pt, rpt = nextPT()
            k.op("pe", lambda e, pt=pt: e.transpose(out=pt[:, 0:128], in_=wtb[:], identity=ID[:]),
                 reads=[rwtb, rID], writes=[rpt])
            k.op("act", lambda e, pt=pt, h=h: e.copy(out=WST[:, h, :], in_=pt[:, 0:128]), reads=[rpt], writes=[rWST])

        if stop == 'wst':
            k.finish()
            return nc
        for grp in GROUPS:
            own = [j for j in grp if j >= 1]
            nloc = len(grp)
            loc = {j: i for i, j in enumerate(grp)}
            yloc = {j: i for i, j in enumerate(own)}
            wb_kv, rwb_kv = ring.load([(0, 256, w_in[:, 1024:1280])])
            for j in grp:
                k.op("act", lambda e, j=j: e.copy(out=hb[:], in_=H[:, j, :]), reads=[rH[j]], writes=[rhb])
                for half in range(2):
                    pt, rpt = nextPT()
                    for i in range(8):
                        kc = half * 8 + i
                        k.op("pe", lambda e, pt=pt, kc=kc, i=i: e.transpose(out=pt[:, i * 128:(i + 1) * 128], in_=hb[:, kc * 128:(kc + 1) * 128], identity=ID[:]),
                             reads=[rhb, rID], writes=[rpt], sig=(i == 7))
                    k.op("dve", lambda e, pt=pt, half=half, c=loc[j]: e.tensor_copy(out=HT[:, half * 8:(half + 1) * 8, c * 128:(c + 1) * 128], in_=pt[:].rearrange("p (a b) -> p a b", a=8)),
                         reads=[rpt], writes=[rHT[loc[j]]])
                if j >= 1:
                    k.op("act", lambda e, j=j: e.mul(out=H[:, j, :], in_=H[:, j, :], mul=ALPHA), reads=[rH[j]], writes=[rH[j]])
            if stop == 'ht':
                k.finish()
                return nc
            wb_next, rwb_next = ring.load([(0, 256, w_in[:, 0:256])])
            for j in grp:
                pp, rpp = nextPP()
                c = loc[j]
                for kc in range(16):
                    k.op("pe", lambda e, pp=pp, kc=kc, c=c: e.matmul(pp[:, 0:256], lhsT=HT[:, kc, c * 128:(c + 1) * 128], rhs=wb_kv[:, kc, :], start=(kc == 0), stop=(kc == 15)),
                         reads=[rHT[c], rwb_kv], writes=[rpp], sig=(kc == 15))
                kv = pp[:, 0:128].rearrange("p (g d) -> p g d", g=2)
                cs, sn = COS[:, j, 0:2, :], SIN[:, j, 0:2, :]
                x1, x2 = kv[:, :, 0:8], kv[:, :, 8:16]
                k.op("dve", lambda e, x1=x1, cs=cs: e.tensor_tensor(out=t8[0][:, 0:2, :], in0=x1, in1=cs, op=ALU.mult), reads=[rpp, rCOS], writes=[rt8[0]])
                k.op("dve", lambda e, x2=x2, sn=sn: e.tensor_tensor(out=t8[1][:, 0:2, :], in0=x2, in1=sn, op=ALU.mult), reads=[rpp, rSIN], writes=[rt8[1]])
                k.op("dve", lambda e, x2=x2, cs=cs: e.tensor_tensor(out=t8[2][:, 0:2, :], in0=x2, in1=cs, op=ALU.mult), reads=[rpp, rCOS], writes=[rt8[2]])
                k.op("dve", lambda e, x1=x1, sn=sn: e.tensor_tensor(out=t8[3][:, 0:2, :], in0=x1, in1=sn, op=ALU.mult), reads=[rpp, rSIN], writes=[rt8[3]])
                k.op("dve", lambda e: e.tensor_tensor(out=kd[:, :, 0:8], in0=t8[0][:, 0:2, :], in1=t8[1][:, 0:2, :], op=ALU.subtract), reads=[rt8[0], rt8[1]], writes=[rkd])
                k.op("dve", lambda e: e.tensor_tensor(out=kd[:, :, 8:16], in0=t8[2][:, 0:2, :], in1=t8[3][:, 0:2, :], op=ALU.add), reads=[rt8[2], rt8[3]], writes=[rkd])
                k.op("act", lambda e, kv=kv: e.copy(out=kd[:, :, 16:64], in_=kv[:, :, 16:64]), reads=[rpp], writes=[rkd])
                k.op("act", lambda e, pp=pp, j=j: e.copy(out=V[:, j, :], in_=pp[:, 128:256]), reads=[rpp], writes=[rV[j]])
                pt, rpt = nextPT()
                for g in range(2):
                    k.op("pe", lambda e, pt=pt, g=g: e.transpose(out=pt[0:64, g * 128:(g + 1) * 128], in_=kd[:, g, :], identity=ID[:]),
                         reads=[rkd, rID], writes=[rpt], sig=(g == 1))
                k.op("dve", lambda e, pt=pt, j=j: e.tensor_copy(out=KT[0:64, :, j * 128:(j + 1) * 128], in_=pt[0:64, 0:256].rearrange("p (g t) -> p g t", g=2)),
                     reads=[rpt], writes=[rKT[j]])
            if stop == 'kv':
                k.finish()
                return nc
            for qi_ in range(4):
                wb, rwb = wb_next, rwb_next
                if qi_ < 3:
                    wb_next, rwb_next = ring.load([(0, 256, w_in[:, (qi_ + 1) * 256:(qi_ + 2) * 256])])
                else:
                    wb_next, rwb_next = ring.load([(0, 128, w_in[:, 1280:1408]), (128, 256, w_in[:, 2304:2432])])
                g = qi_ // 2
                for j in own:
                    c = loc[j]
                    pp, rpp = nextPP()
                    for kc in range(16):
                        k.op("pe", lambda e, pp=pp, kc=kc, c=c, wb=wb: e.matmul(pp[:, 0:256], lhsT=HT[:, kc, c * 128:(c + 1) * 128], rhs=wb[:, kc, :], start=(kc == 0), stop=(kc == 15)),
                             reads=[rHT[c], rwb], writes=[rpp], sig=(kc == 15))
                    q4 = pp[:, 0:256].rearrange("p (h d) -> p h d", h=4)
                    cs, sn = COS[:, j, :, :], SIN[:, j, :, :]
                    x1, x2 = q4[:, :, 0:8], q4[:, :, 8:16]
                    k.op("dve", lambda e, x1=x1, cs=cs: e.tensor_tensor(out=t8[0][:], in0=x1, in1=cs, op=ALU.mult), reads=[rpp, rCOS], writes=[rt8[0]])
                    k.op("dve", lambda e, x2=x2, sn=sn: e.tensor_tensor(out=t8[1][:], in0=x2, in1=sn, op=ALU.mult), reads=[rpp, rSIN], writes=[rt8[1]])
                    k.op("dve", lambda e, x2=x2, cs=cs: e.tensor_tensor(out=t8[2][:], in0=x2, in1=cs, op=ALU.mult), reads=[rpp, rCOS], writes=[rt8[2]])
                    k.op("dve", lambda e, x1=x1, sn=sn: e.tensor_tensor(out=t8[3][:], in0=x1, in1=sn, op=ALU.mult), reads=[rpp, rSIN], writes=[rt8[3]])
                    k.op("dve", lambda e: e.tensor_tensor(out=qb[:, :, 0:8], in0=t8[0][:], in1=t8[1][:], op=ALU.subtract), reads=[rt8[0], rt8[1]], writes=[rqb])
                    k.op("dve", lambda e: e.tensor_tensor(out=qb[:, :, 8:16], in0=t8[2][:], in1=t8[3][:], op=ALU.add), reads=[rt8[2], rt8[3]], writes=[rqb])
                    k.op("act", lambda e, q4=q4: e.copy(out=qb[:, :, 16:64], in_=q4[:, :, 16:64]), reads=[rpp], writes=[rqb])
                    pt, rpt = nextPT()
                    for hh in range(4):
                        k.op("pe", lambda e, pt=pt, hh=hh: e.transpose(out=pt[0:64, hh * 128:(hh + 1) * 128], in_=qb[:, hh, :], identity=ID[:]),
                             reads=[rqb, rID], writes=[rpt], sig=(hh == 3))
                    k.op("dve", lambda e, pt=pt: e.tensor_copy(out=qT[0:64, :, :], in_=pt[0:64, 0:512].rearrange("p (a t) -> p a t", a=4)), reads=[rpt], writes=[rqT])
                    for hh in range(4):
                        k.op("pe", lambda e, hh=hh, j=j, g=g: e.matmul(PS[:, hh, :], lhsT=qT[0:64, hh, :], rhs=KT[0:64, g, (j - 1) * 128:(j + 1) * 128], start=True, stop=True),
                             reads=[rqT, rKT[j - 1], rKT[j]], writes=[rPS], sig=(hh == 3))
                    if stop == 'a1':
                        k.finish()
                        return nc
                    mi = 0 if j == 1 else 1
                    k.op("dve", lambda e, mi=mi: e.scalar_tensor_tensor(out=sm[:], in0=PS[:], scalar=0.125, in1=MASK[:, mi, :].unsqueeze(1).to_broadcast([128, 4, 256]), op0=ALU.mult, op1=ALU.add),
                         reads=[rPS, rMASK], writes=[rsm])
                    k.op("dve", lambda e: e.tensor_reduce(out=mx[:], in_=sm[:], axis=AX.X, op=ALU.max), reads=[rsm], writes=[rmx])
                    k.op("dve", lambda e, qi_=qi_: e.tensor_tensor(out=mx[:], in0=mx[:], in1=SINK[:, qi_ * 4:qi_ * 4 + 4], op=ALU.max), reads=[rmx, rSINK], writes=[rmx])
                    k.op("dve", lambda e: e.tensor_tensor(out=sm[:], in0=sm[:], in1=mx[:].unsqueeze(2).to_broadcast([128, 4, 256]), op=ALU.subtract), reads=[rsm, rmx], writes=[rsm])
                    k.op("act", lambda e: e.activation(out=eb[:], in_=sm[:], func=AF.Exp), reads=[rsm], writes=[reb])
                    k.op("dve", lambda e, qi_=qi_: e.tensor_tensor(out=es_[:], in0=SINK[:, qi_ * 4:qi_ * 4 + 4], in1=mx[:], op=ALU.subtract), reads=[rmx, rSINK], writes=[res_])
                    k.op("act", lambda e: e.activation(out=es_[:], in_=es_[:], func=AF.Exp), reads=[res_], writes=[res_])
                    k.op("dve", lambda e: e.tensor_reduce(out=sx[:], in_=eb[:], axis=AX.X, op=ALU.add), reads=[reb], writes=[rsx])
                    k.op("dve", lambda e: e.tensor_tensor(out=sx[:], in0=sx[:], in1=es_[:], op=ALU.add), reads=[rsx, res_], writes=[rsx])
                    k.op("dve", lambda e: e.reciprocal(out=sx[:], in_=sx[:]), reads=[rsx], writes=[rsx])
                    if stop == 'a2':
                        k.finish()
                        return nc
                    pt, rpt = nextPT()
                    for kcx in range(2):
                        for hh in range(4):
                            idx = kcx * 4 + hh
                            k.op("pe", lambda e, pt=pt, kcx=kcx, hh=hh, idx=idx: e.transpose(out=pt[:, idx * 128:(idx + 1) * 128], in_=eb[:, hh, kcx * 128:(kcx + 1) * 128], identity=ID[:]),
                                 reads=[reb, rID], writes=[rpt], sig=(idx == 7))
                    k.op("act", lambda e, pt=pt: e.copy(out=eT[:], in_=pt[:].rearrange("p (a h t) -> p a h t", a=2, h=4)), reads=[rpt], writes=[reT])
                    if stop == 'a3':
                        k.finish()
                        return nc
                    for hh in range(4):
                        for kcx in range(2):
                            k.op("pe", lambda e, hh=hh, kcx=kcx, j=j, g=g: e.matmul(PV[:, hh * 64:(hh + 1) * 64], lhsT=eT[:, kcx, hh, :], rhs=V[:, j - 1 + kcx, g * 64:(g + 1) * 64], start=(kcx == 0), stop=(kcx == 1)),
                                 reads=[reT, rV[j - 1], rV[j]], writes=[rPV], sig=(hh == 3 and kcx == 1))
                    if stop == 'a4':
                        k.finish()
                        return nc
                    k.op("dve", lambda e: e.tensor_tensor(out=o32[:].rearrange("p (h d) -> p h d", h=4), in0=PV[:, 0:256].rearrange("p (h d) -> p h d", h=4), in1=sx[:].unsqueeze(2).to_broadcast([128, 4, 64]), op=ALU.mult),
                         reads=[rPV, rsx], writes=[ro32])
                    if stop == 'a5':
                        k.finish()
                        return nc
                    k.op("act", lambda e, j=j, qi_=qi_: e.activation(out=junk[:], in_=o32[:], func=AF.Square, accum_out=SSQ[:, j, qi_:qi_ + 1]), reads=[ro32], writes=[rjunk, rSSQ[j]])
                    if stop == 'a6':
                        k.finish()
                        return nc
                    k.op("act", lambda e: e.copy(out=yb[:], in_=o32[:]), reads=[ro32], writes=[ryb])
                    pt, rpt = nextPT()
                    for pr in range(2):
                        k.op("pe", lambda e, pt=pt, pr=pr: e.transpose(out=pt[:, pr * 128:(pr + 1) * 128], in_=yb[:, pr * 128:(pr + 1) * 128], identity=ID[:]),
                             reads=[ryb, rID], writes=[rpt], sig=(pr == 1))
                    for pr in range(2):
                        kc = 2 * qi_ + pr
                        k.op("dve", lambda e, pt=pt, pr=pr, kc=kc, yc=yloc[j]: e.tensor_scalar(out=YT[:, kc, yc * 128:(yc + 1) * 128], in0=pt[:, pr * 128:(pr + 1) * 128], scalar1=GNT[:, kc:kc + 1], scalar2=None, op0=ALU.mult),
                             reads=[rpt, rGNT], writes=[rYT[yloc[j]]])
            if stop == 'attn':
                k.finish()
                return nc
            for hd in range(8):
                wb, rwb = wb_next, rwb_next
                if hd < 7:
                    wb_next, rwb_next = ring.load([(0, 128, w_in[:, 1280 + (hd + 1) * 128:1280 + (hd + 2) * 128]),
                                                   (128, 256, w_in[:, 2304 + (hd + 1) * 128:2304 + (hd + 2) * 128])])
                else:
                    wb_next, rwb_next = ring.load([(0, 256, w_o[:, 0:256])])
                for j in own:
                    c = loc[j]
                    pp, rpp = nextPP()
                    for kc in range(16):
                        k.op("pe", lambda e, pp=pp, kc=kc, c=c, wb=wb: e.matmul(pp[:, 0:256], lhsT=HT[:, kc, c * 128:(c + 1) * 128], rhs=wb[:, kc, :], start=(kc == 0), stop=(kc == 15)),
                             reads=[rHT[c], rwb], writes=[rpp], sig=(kc == 15))
                    z = pp[:, 0:256]
                    k.op("act", lambda e, z=z: e.activation(out=g1[:], in_=z, func=AF.Square), reads=[rpp], writes=[rg1])
                    k.op("dve", lambda e: e.tensor_scalar(out=g1[:], in0=g1[:], scalar1=0.044715, scalar2=1.0, op0=ALU.mult, op1=ALU.add), reads=[rg1], writes=[rg1])
                    k.op("dve", lambda e, z=z: e.tensor_tensor(out=g1[:], in0=g1[:], in1=z, op=ALU.mult), reads=[rg1, rpp], writes=[rg1])
                    k.op("act", lambda e: e.activation(out=g1[:], in_=g1[:], func=AF.Sigmoid, scale=1.5957691216057308), reads=[rg1], writes=[rg1])
                    k.op("dve", lambda e, z=z: e.tensor_tensor(out=g2[:], in0=g1[:], in1=z, op=ALU.mult), reads=[rg1, rpp], writes=[rg2])
                    k.op("dve", lambda e: e.bn_stats(out=vst[:], in_=g2[:, 128:256]), reads=[rg2], writes=[rvst])
                    k.op("dve", lambda e: e.bn_aggr(out=vmv[:], in_=vst[:]), reads=[rvst], writes=[rvmv])
                    k.op("act", lambda e: e.activation(out=vrs[:], in_=vmv[:, 1:2], func=AF.Sqrt, bias=EPS, scale=1.0), reads=[rvmv], writes=[rvrs])
                    k.op("dve", lambda e: e.reciprocal(out=vrs[:], in_=vrs[:]), reads=[rvrs], writes=[rvrs])
                    k.op("dve", lambda e: e.tensor_scalar(out=g3[:, 0:128], in0=g2[:, 128:256], scalar1=vmv[:, 0:1], scalar2=vrs[:, 0:1], op0=ALU.subtract, op1=ALU.mult), reads=[rg2, rvmv, rvrs], writes=[rg3])
                    k.op("pool", lambda e, hd=hd: e.tensor_tensor(out=g3[:, 0:128], in0=g3[:, 0:128], in1=VNG[:, hd * 128:(hd + 1) * 128], op=ALU.mult), reads=[rg3, rVNG], writes=[rg3])
                    k.op("pool", lambda e, hd=hd: e.tensor_tensor(out=vh[:], in0=g3[:, 0:128], in1=VNB[:, hd * 128:(hd + 1) * 128], op=ALU.add), reads=[rg3, rVNB], writes=[rvh])
                    k.op("pe", lambda e, hd=hd: e.matmul(PV[:, 256:384], lhsT=WST[:, hd, :], rhs=vh[:], start=True, stop=True), reads=[rWST, rvh], writes=[rPV])
                    k.op("dve", lambda e, hd=hd: e.scalar_tensor_tensor(out=o32[:, 0:128], in0=PV[:, 256:384], scalar=BST[:, hd:hd + 1], in1=g2[:, 0:128], op0=ALU.add, op1=ALU.mult), reads=[rPV, rBST, rg2], writes=[ro32])
                    k.op("act", lambda e, j=j, hd=hd: e.activation(out=junk[:, 0:128], in_=o32[:, 0:128], func=AF.Square, accum_out=SSQ[:, j, 4 + hd:5 + hd]),
                         reads=[ro32], writes=[rjunk, rSSQ[j]])
                    k.op("act", lambda e: e.copy(out=yb[:, 0:128], in_=o32[:, 0:128]), reads=[ro32], writes=[ryb])
                    pt, rpt = nextPT()
                    k.op("pe", lambda e, pt=pt: e.transpose(out=pt[:, 0:128], in_=yb[:, 0:128], identity=ID[:]), reads=[ryb, rID], writes=[rpt])
                    k.op("dve", lambda e, pt=pt, hd=hd, yc=yloc[j]: e.tensor_scalar(out=YT[:, 8 + hd, yc * 128:(yc + 1) * 128], in0=pt[:, 0:128], scalar1=GNT[:, 8 + hd:9 + hd], scalar2=None, op0=ALU.mult),
                         reads=[rpt, rGNT], writes=[rYT[yloc[j]]])
            if stop == 'gmlp':
                k.finish()
                return nc
            for j in own:
                k.op("dve", lambda e, j=j: e.tensor_reduce(out=rsa[:, 0:1], in_=SSQ[:, j, 0:4], axis=AX.X, op=ALU.add), reads=[rSSQ[j]], writes=[rrsa])
                k.op("dve", lambda e, j=j: e.tensor_reduce(out=rsa[:, 1:2], in_=SSQ[:, j, 4:12], axis=AX.X, op=ALU.add), reads=[rSSQ[j]], writes=[rrsa])
                k.op("act", lambda e: e.activation(out=rsa[:], in_=rsa[:], func=AF.Sqrt, bias=EPS, scale=1.0 / 1024.0), reads=[rrsa], writes=[rrsa])
                k.op("dve", lambda e, j=j: e.reciprocal(out=SSQ[:, j, 0:2], in_=rsa[:]), reads=[rrsa], writes=[rSSQ[j]])
            for oi in range(8):
                wb, rwb = wb_next, rwb_next
                if oi < 7:
                    wb_next, rwb_next = ring.load([(0, 256, w_o[:, (oi + 1) * 256:(oi + 2) * 256])])
                for j in own:
                    yc = yloc[j]
                    pp, rpp = nextPP()
                    for half in range(2):
                        for i in range(8):
                            kc = half * 8 + i
                            k.op("pe", lambda e, pp=pp, kc=kc, yc=yc, wb=wb, half=half, i=i: e.matmul(pp[:, half * 256:(half + 1) * 256], lhsT=YT[:, kc, yc * 128:(yc + 1) * 128], rhs=wb[:, kc, :], start=(i == 0), stop=(i == 7)),
                                 reads=[rYT[yc], rwb], writes=[rpp], sig=(kc == 15))
                    hs = H[:, j, oi * 256:(oi + 1) * 256]
                    k.op("dve", lambda e, pp=pp, hs=hs, j=j: e.scalar_tensor_tensor(out=hs, in0=pp[:, 0:256], scalar=SSQ[:, j, 0:1], in1=hs, op0=ALU.mult, op1=ALU.add), reads=[rpp, rSSQ[j], rH[j]], writes=[rH[j]])
                    k.op("dve", lambda e, pp=pp, hs=hs, j=j: e.scalar_tensor_tensor(out=hs, in0=pp[:, 256:512], scalar=SSQ[:, j, 1:2], in1=hs, op0=ALU.mult, op1=ALU.add), reads=[rpp, rSSQ[j], rH[j]], writes=[rH[j]])
            if stop == 'wo':
                k.finish()
                return nc
            for j in own:
                emit_layernorm(k, H[:, j, :], rH[j], LG, rLG, LB, rLB, lt)
                k.dma("sp", h1_out[(j - 1) * 128:j * 128, :], H[:, j, :], reads=[rH[j]], key="oh", is_out=True)
                k.op("act", lambda e, j=j: e.copy(out=hb[:], in_=H[:, j, :]), reads=[rH[j]], writes=[rhb])
                k.op("dve", lambda e, j=j: e.tensor_tensor(out=hlo[:], in0=H[:, j, :], in1=hb[:], op=ALU.subtract), reads=[rH[j], rhb], writes=[rhlo])
                k.dma("sp", hbt_out[(j - 1) * 128:j * 128, :], hb[:], reads=[rhb], key="ob", is_out=True)
                for src, rsrc, dstT, rdstT in ((hb, rhb, hbT, rhbT), (hlo, rhlo, hloT, rhloT)):
                    for half in range(2):
                        pt, rpt = nextPT()
                        for i in range(8):
                            kc = half * 8 + i
                            k.op("pe", lambda e, pt=pt, kc=kc, i=i, src=src: e.transpose(out=pt[:, i * 128:(i + 1) * 128], in_=src[:, kc * 128:(kc + 1) * 128], identity=ID[:]),
                                 reads=[rsrc, rID], writes=[rpt], sig=(i == 7))
                        k.op("dve", lambda e, pt=pt, half=half, dstT=dstT: e.tensor_copy(out=dstT[:, half * 8:(half + 1) * 8, :], in_=pt[:].rearrange("p (a b) -> p a b", a=8)),
                             reads=[rpt], writes=[rdstT])
                k.dma("sp", hT_out[:, :, (j - 1) * 128:j * 128].rearrange("kc p t -> p kc t"), hbT[:], reads=[rhbT], key="ot", is_out=True)
                n = 0
                for (aT, raT, wq, rwq) in ((hbT, rhbT, WRH, rWRH), (hloT, rhloT, WRH, rWRH), (hbT, rhbT, WRL, rWRL)):
                    for kc in range(16):
                        k.op("pe", lambda e, kc=kc, aT=aT, wq=wq, n=n: e.matmul(PR[:, 0:NE], lhsT=aT[:, kc, :], rhs=wq[:, kc, :], start=(n == 0), stop=(n == 47)),
                             reads=[raT, rwq], writes=[rPR], sig=(n == 47))
                        n += 1
                k.op("dve", lambda e: e.tensor_tensor(out=lg[:], in0=PR[:, 0:NE], in1=BR[:], op=ALU.add), reads=[rPR, rBR], writes=[rlg])
                k.op("dve", lambda e: e.max(out=m8[:], in_=lg[:]), reads=[rlg], writes=[rm8])
                k.op("dve", lambda e: e.tensor_scalar(out=msk[:], in0=lg[:], scalar1=m8[:, 3:4], scalar2=None, op0=ALU.is_ge), reads=[rlg, rm8], writes=[rmsk])
                k.op("dve", lambda e: e.tensor_scalar(out=nm[:], in0=m8[:, 0:1], scalar1=-1.0, scalar2=None, op0=ALU.mult), reads=[rm8], writes=[rnm])
                k.op("act", lambda e: e.activation(out=lg[:], in_=lg[:], func=AF.Exp, bias=nm[:, 0:1], scale=1.0), reads=[rlg, rnm], writes=[rlg])
                k.op("dve", lambda e: e.tensor_tensor(out=lg[:], in0=lg[:], in1=msk[:], op=ALU.mult), reads=[rlg, rmsk], writes=[rlg])
                k.op("dve", lambda e: e.tensor_reduce(out=gs[:], in_=lg[:], axis=AX.X, op=ALU.add), reads=[rlg], writes=[rgs])
                k.op("dve", lambda e: e.reciprocal(out=gs[:], in_=gs[:]), reads=[rgs], writes=[rgs])
                k.op("dve", lambda e: e.tensor_scalar(out=lg[:], in0=lg[:], scalar1=gs[:, 0:1], scalar2=None, op0=ALU.mult), reads=[rlg, rgs], writes=[rlg])
                k.dma("sp", g_out[(j - 1) * 128:j * 128, :], lg[:], reads=[rlg], key="og", is_out=True)
        k.finish()
    return nc


TG = 1024


def build_moe():
    nc = bass.Bass("TRN2", target_bir_lowering=False)
    dt_in = lambda name, shape, dt=F32: nc.dram_tensor(name, list(shape), dt, kind="ExternalInput").ap()
    xT = dt_in("xT", [128, 16, NTOK], BF16)
    gT = dt_in("gT", [EPC, NTOK])
    w_gu = dt_in("w_gu", [EPC, D, 2 * D])
    bguT = dt_in("bguT", [128, EPC, 32])
    w_dn = dt_in("w_down", [EPC, D, D])
    b_dn = dt_in("b_down", [EPC, D])
    y_out = nc.dram_tensor("ypart", [NTOK, D], F32, kind="ExternalOutput").ap()
    NG = NTOK // TG
    with ExitStack() as es:
        k = KB(nc, es)
        XT = k.sb("XT", [128, 16, TG], BF16); rXT = Res()
        GB = k.sb("GB", [128, EPC, TG], F32); rGB = Res()
        G4 = k.sb("G4", [EPC, TG], F32); rG4 = Res()
        G4b = k.sb("G4b", [EPC, TG], BF16); rG4b = Res()
        BD = k.sb("BD", [EPC, D], F32); rBD = Res()
        BDb = k.sb("BDb", [EPC, D], BF16); rBDb = Res()
        BGU = k.sb("BGU", [128, EPC, 32], F32); rBGU = Res()
        YACC = k.sb("YACC", [128, TG // 128, D], F32); rY = [Res() for _ in range(TG // 128)]
        AT = k.sb("AT", [128, 16, TG], BF16); rAT = [Res() for _ in range(16)]
        ring = WRing(k, "wm", 4)
        T1 = [k.sb("T1_%d" % i, [128, 512], F32) for i in range(2)]; rT1 = [Res(), Res()]
        T2 = [k.sb("T2_%d" % i, [128, 512], F32) for i in range(2)]; rT2 = [Res(), Res()]
        T3 = [k.sb("T3_%d" % i, [128, 512], F32) for i in range(2)]; rT3 = [Res(), Res()]
        PA = [k.ps("PA%d" % i, [128, 512], F32) for i in range(2)]; rPA = [Res(), Res()]
        PB = [k.ps("PB%d" % i, [128, 512], F32) for i in range(2)]; rPB = [Res(), Res()]
        PD = [k.ps("PD%d" % i, [128, 512], F32) for i in range(2)]; rPD = [Res(), Res()]
        k.dma("sp", BGU[:], bguT, writes=[rBGU], key="c0")
        k.dma("sp", BD[:], b_dn, writes=[rBD], key="c1")
        k.op("dve", lambda e: e.tensor_copy(out=BDb[:], in_=BD[:]), reads=[rBD], writes=[rBDb])
        pieces = []
        for tg in range(NG):
            for ex in range(EPC):
                for fc in range(16):
                    pieces.append([(0, 128, w_gu[ex][:, fc * 128:(fc + 1) * 128]), (128, 256, w_gu[ex][:, D + fc * 128:D + (fc + 1) * 128])])
                for dp in range(8):
                    pieces.append([(0, 256, w_dn[ex][:, dp * 256:(dp + 1) * 256])])
        LA = 2
        loaded = []
        pi = [0]

        def get_piece():
            while len(loaded) < min(len(pieces), pi[0] + 1 + LA):
                loaded.append(ring.load(pieces[len(loaded)]))
            r = loaded[pi[0]]
            pi[0] += 1
            return r

        cnt = 0
        dcnt = 0
        for tg in range(NG):
            t0 = tg * TG
            k.dma("sp", XT[:], xT[:, :, t0:t0 + TG], writes=[rXT], key="xt")
            for ex in range(EPC):
                k.dma("sp", GB[:, ex, :], gT[ex, t0:t0 + TG].partition_broadcast(128), writes=[rGB], key="gb")
            k.dma("sp", G4[:], gT[:, t0:t0 + TG], writes=[rG4], key="g4")
            k.op("dve", lambda e: e.tensor_copy(out=G4b[:], in_=G4[:]), reads=[rG4], writes=[rG4b])
            for ex in range(EPC):
                for fc in range(16):
                    wb, rwb = get_piece()
                    for ns in range(2):
                        b = cnt % 2
                        cnt += 1
                        for kc in range(16):
                            k.op("pe", lambda e, b=b, kc=kc, wb=wb, ns=ns: e.matmul(PA[b][:], lhsT=wb[:, kc, 0:128], rhs=XT[:, kc, ns * 512:(ns + 1) * 512], start=(kc == 0), stop=(kc == 15)),
                                 reads=[rwb, rXT], writes=[rPA[b]], sig=(kc == 15))
                        for kc in range(16):
                            k.op("pe", lambda e, b=b, kc=kc, wb=wb, ns=ns: e.matmul(PB[b][:], lhsT=wb[:, kc, 128:256], rhs=XT[:, kc, ns * 512:(ns + 1) * 512], start=(kc == 0), stop=(kc == 15)),
                                 reads=[rwb, rXT], writes=[rPB[b]], sig=(kc == 15))
                        k.op("dve", lambda e, b=b, ex=ex, fc=fc: e.tensor_scalar(out=T1[b][:], in0=PA[b][:], scalar1=BGU[:, ex, fc:fc + 1], scalar2=7.0, op0=ALU.add, op1=ALU.min),
                             reads=[rPA[b], rBGU], writes=[rT1[b]])
                        k.op("act", lambda e, b=b: e.activation(out=T2[b][:], in_=T1[b][:], func=AF.Sigmoid, scale=1.702), reads=[rT1[b]], writes=[rT2[b]])
                        k.op("dve", lambda e, b=b, ex=ex, fc=fc: e.tensor_scalar(out=T3[b][:], in0=PB[b][:], scalar1=BGU[:, ex, 16 + fc:17 + fc], scalar2=7.0, op0=ALU.add, op1=ALU.min),
                             reads=[rPB[b], rBGU], writes=[rT3[b]])
                        k.op("dve", lambda e, b=b: e.tensor_scalar(out=T3[b][:], in0=T3[b][:], scalar1=-7.0, scalar2=1.0, op0=ALU.max, op1=ALU.add), reads=[rT3[b]], writes=[rT3[b]])
                        k.op("pool", lambda e, b=b: e.tensor_tensor(out=T1[b][:], in0=T1[b][:], in1=T2[b][:], op=ALU.mult), reads=[rT1[b], rT2[b]], writes=[rT1[b]])
                        k.op("pool", lambda e, b=b: e.tensor_tensor(out=T1[b][:], in0=T1[b][:], in1=T3[b][:], op=ALU.mult), reads=[rT1[b], rT3[b]], writes=[rT1[b]])
                        k.op("pool", lambda e, b=b, ex=ex, fc=fc, ns=ns: e.tensor_tensor(out=AT[:, fc, ns * 512:(ns + 1) * 512], in0=T1[b][:], in1=GB[:, ex, ns * 512:(ns + 1) * 512], op=ALU.mult),
                             reads=[rT1[b], rGB], writes=[rAT[fc]])
                for dp in range(8):
                    wb, rwb = get_piece()
                    for tc in range(TG // 128):
                        b = dcnt % 2
                        dcnt += 1
                        if ex == 0:
                            k.op("pe", lambda e, b=b, tc=tc, dp=dp: e.matmul(PD[b][:, 0:256], lhsT=G4b[:, tc * 128:(tc + 1) * 128], rhs=BDb[:, dp * 256:(dp + 1) * 256], start=True, stop=False),
                                 reads=[rG4b, rBDb], writes=[rPD[b]], sig=False)
                        for fc in range(16):
                            k.op("pe", lambda e, b=b, tc=tc, fc=fc, wb=wb, ex=ex: e.matmul(PD[b][:, 0:256], lhsT=AT[:, fc, tc * 128:(tc + 1) * 128], rhs=wb[:, fc, :], start=(fc == 0 and ex != 0), stop=(fc == 15)),
                                 reads=[rAT[fc], rwb], writes=[rPD[b]], sig=(fc == 15))
                        ys = YACC[:, tc, dp * 256:(dp + 1) * 256]
                        if ex == 0:
                            k.op("act", lambda e, b=b, ys=ys: e.copy(out=ys, in_=PD[b][:, 0:256]), reads=[rPD[b]], writes=[rY[tc]])
                        else:
                            k.op("dve", lambda e, b=b, ys=ys: e.tensor_tensor(out=ys, in0=ys, in1=PD[b][:, 0:256], op=ALU.add), reads=[rPD[b], rY[tc]], writes=[rY[tc]])
            for tc in range(TG // 128):
                k.dma("sp", y_out[t0 + tc * 128:t0 + (tc + 1) * 128, :], YACC[:, tc, :], reads=[rY[tc]], key="oy", is_out=True)
        k.finish()
    return nc


CAP = 256
NBLK = NTOK // 1024
NSLOT = NBLK * CAP
HSLOT = NSLOT // 2


def build_moe2():
    nc = bass.Bass("TRN2", target_bir_lowering=False)
    dt_in = lambda name, shape, dt=F32: nc.dram_tensor(name, list(shape), dt, kind="ExternalInput").ap()
    hbt = dt_in("hbt", [NTOK, D], BF16)
    gC = dt_in("gC", [128, NTOK // 128, EPC])
    iotad = dt_in("iotad", [128, CAP])
    sud = dt_in("sud", [128, 128], BF16)
    onesd = dt_in("onesd", [128, 128], BF16)
    identb = dt_in("identb", [128, 128], BF16)
    w_gu = dt_in("w_gu", [EPC, D, 2 * D])
    bguT = dt_in("bguT", [128, EPC, 32])
    w_dn = dt_in("w_down", [EPC, D, D])
    b_dn = dt_in("b_down", [EPC, D])
    y_out = nc.dram_tensor("ypart", [NTOK, D], F32, kind="ExternalOutput").ap()
    ysp = nc.dram_tensor("ysp", [EPC, NSLOT, D], BF16, kind="Internal").ap()
    NTC = NTOK // 128
    with ExitStack() as es:
        k = KB(nc, es)
        IOTA = k.sb("IOTA", [128, CAP], F32); rIOTA = Res()
        SU = k.sb("SU", [128, 128], BF16); rSU = Res()
        ONES = k.sb("ONES", [128, 128], BF16); rONES = Res()
        ID = k.sb("ID", [128, 128], BF16); rID = Res()
        GC = k.sb("GC", [128, NTC, EPC], F32); rGC = Res()
        MKb = k.sb("MKb", [128, NTC * EPC], BF16); rMKb = Res()
        MKf = k.sb("MKf", [128, NTC, EPC], F32); rMKf = Res()
        W1 = k.sb("W1", [128, NTC, EPC], F32); rW1 = Res()
        CNT = k.sb("CNT", [128, NTC, EPC], F32); rCNT = Res()
        PRE = k.sb("PRE", [128, NTC, EPC], F32); rPRE = Res()
        POSM = k.sb("POSM", [128, NTC, EPC], F32); rPOSM = Res()
        BGU = k.sb("BGU", [128, EPC, 32], F32); rBGU = Res()
        BDb1 = k.sb("BDb1", [1, D], BF16); rBDb1 = Res()
        XT = k.sb("XT", [128, 16, HSLOT], BF16); rXT = Res()
        AT = k.sb("AT", [128, 16, HSLOT], BF16); rAT = [Res() for _ in range(16)]
        HBs = [k.sb("HB%d" % i, [128, 8, D], BF16) for i in range(2)]; rHBs = [Res(), Res()]
        S = [k.sb("S%d" % i, [128, 8, CAP], BF16) for i in range(2)]; rS = [Res(), Res()]
        YST = [k.sb("YST%d" % i, [128, 256], BF16) for i in range(4)]; rYST = [Res() for _ in range(4)]
        OUT = [k.sb("OUT%d" % i, [128, D], F32) for i in range(2)]; rOUT = [Res(), Res()]
        ring = WRing(k, "wm", 3)
        T1 = [k.sb("T1_%d" % i, [128, 512], F32) for i in range(2)]; rT1 = [Res(), Res()]
        T2 = [k.sb("T2_%d" % i, [128, 512], F32) for i in range(2)]; rT2 = [Res(), Res()]
        T3 = [k.sb("T3_%d" % i, [128, 512], F32) for i in range(2)]; rT3 = [Res(), Res()]
        PA = [k.ps("PA%d" % i, [128, 512], F32) for i in range(2)]; rPA = [Res(), Res()]
        PB = [k.ps("PB%d" % i, [128, 512], F32) for i in range(2)]; rPB = [Res(), Res()]
        PD = [k.ps("PD%d" % i, [128, 512], F32) for i in range(2)]; rPD = [Res(), Res()]
        PT = [k.ps("PT%d" % i, [128, 1024], BF16) for i in range(2)]; rPT = [Res(), Res()]
        ld = lambda dst, src, r, key: k.dma("sp", dst, src, writes=[r], key=key)
        ld(IOTA[:], iotad, rIOTA, "c0"); ld(SU[:], sud, rSU, "c1"); ld(ONES[:], onesd, rONES, "c2"); ld(ID[:], identb, rID, "c3")
        ld(GC[:], gC, rGC, "c4"); ld(BGU[:], bguT, rBGU, "c5")
        gcf = GC[:].rearrange("p a b -> p (a b)")
        k.op("dve", lambda e: e.tensor_scalar(out=MKb[:], in0=gcf, scalar1=0.0, scalar2=None, op0=ALU.is_gt), reads=[rGC], writes=[rMKb])
        k.op("dve", lambda e: e.tensor_scalar(out=MKf[:].rearrange("p a b -> p (a b)"), in0=gcf, scalar1=0.0, scalar2=None, op0=ALU.is_gt), reads=[rGC], writes=[rMKf])
        k.op("pe", lambda e: e.matmul(PA[0][:, 0:NTC * EPC], lhsT=SU[:], rhs=MKb[:], start=True, stop=True), reads=[rSU, rMKb], writes=[rPA[0]])
        k.op("pe", lambda e: e.matmul(PA[1][:, 0:NTC * EPC], lhsT=ONES[:], rhs=MKb[:], start=True, stop=True), reads=[rONES, rMKb], writes=[rPA[1]])
        k.op("dve", lambda e: e.tensor_copy(out=W1[:].rearrange("p a b -> p (a b)"), in_=PA[0][:, 0:NTC * EPC]), reads=[rPA[0]], writes=[rW1])
        k.op("dve", lambda e: e.tensor_copy(out=CNT[:].rearrange("p a b -> p (a b)"), in_=PA[1][:, 0:NTC * EPC]), reads=[rPA[1]], writes=[rCNT])
        pre4 = PRE[:].rearrange("p (j t) e -> p j t e", t=8)
        cnt4 = CNT[:].rearrange("p (j t) e -> p j t e", t=8)
        k.op("dve", lambda e: e.memset(PRE[:], 0.0), writes=[rPRE])
        for i in range(1, 8):
            k.op("dve", lambda e, i=i: e.tensor_tensor(out=pre4[:, :, i, :], in0=pre4[:, :, i - 1, :], in1=cnt4[:, :, i - 1, :], op=ALU.add), reads=[rPRE, rCNT], writes=[rPRE])
        k.op("dve", lambda e: e.tensor_tensor(out=POSM[:], in0=W1[:], in1=PRE[:], op=ALU.add), reads=[rW1, rPRE], writes=[rPOSM])
        k.op("dve", lambda e: e.scalar_tensor_tensor(out=POSM[:], in0=POSM[:], scalar=1.0, in1=MKf[:], op0=ALU.add, op1=ALU.mult), reads=[rPOSM, rMKf], writes=[rPOSM])
        k.op("dve", lambda e: e.tensor_scalar(out=POSM[:], in0=POSM[:], scalar1=-1.0, scalar2=None, op0=ALU.add), reads=[rPOSM], writes=[rPOSM])
        pieces = []
        for ex, _half in [(a, b) for a in range(EPC) for b in range(2)]:
            for fc in range(16):
                pieces.append([(0, 128, w_gu[ex][:, fc * 128:(fc + 1) * 128]), (128, 256, w_gu[ex][:, D + fc * 128:D + (fc + 1) * 128])])
            for dp in range(8):
                pieces.append([(0, 256, w_dn[ex][:, dp * 256:(dp + 1) * 256])])
        LA = 2
        loaded = []
        pi = [0]

        def get_piece():
            while len(loaded) < min(len(pieces), pi[0] + 1 + LA):
                loaded.append(ring.load(pieces[len(loaded)]))
            r = loaded[pi[0]]
            pi[0] += 1
            return r

        cnt = 0
        dcnt = 0
        scnt = 0
        ycnt = 0
        NS = HSLOT // 512
        for ex, half in [(a, b) for a in range(EPC) for b in range(2)]:
            for j in range(half * NBLK // 2, (half + 1) * NBLK // 2):
                jl = j - half * NBLK // 2
                HB, rHB = HBs[scnt % 2], rHBs[scnt % 2]
                k.dma("sp", HB[:], hbt[j * 1024:(j + 1) * 1024, :].rearrange("(tc p) d -> p tc d", p=128), writes=[rHB], key="hb%d" % (scnt % 2))
                sb_ = scnt % 2
                scnt += 1
                for tc in range(8):
                    k.op("dve", lambda e, sb_=sb_, tc=tc, j=j, ex=ex: e.tensor_scalar(out=S[sb_][:, tc, :], in0=IOTA[:], scalar1=POSM[:, j * 8 + tc, ex:ex + 1], scalar2=None, op0=ALU.is_equal),
                         reads=[rIOTA, rPOSM], writes=[rS[sb_]])
                for dcp in range(8):
                    b = dcnt % 2
                    dcnt += 1
                    for dci in range(2):
                        dc = dcp * 2 + dci
                        for tc in range(8):
                            k.op("pe", lambda e, b=b, dci=dci, dc=dc, tc=tc, sb_=sb_, HB=HB: e.matmul(PD[b][:, dci * CAP:(dci + 1) * CAP], lhsT=HB[:, tc, dc * 128:(dc + 1) * 128], rhs=S[sb_][:, tc, :], start=(tc == 0), stop=(tc == 7)),
                                 reads=[rHB, rS[sb_]], writes=[rPD[b]], sig=(tc == 7 and dci == 1))
                    eng = "act" if dcp % 2 == 0 else "dve"
                    if eng == "act":
                        k.op("act", lambda e, b=b, dcp=dcp, jl=jl: e.copy(out=XT[:, 2 * dcp:2 * dcp + 2, jl * CAP:(jl + 1) * CAP], in_=PD[b][:, 0:2 * CAP].rearrange("p (a s) -> p a s", a=2)), reads=[rPD[b]], writes=[rXT])
                    else:
                        k.op("dve", lambda e, b=b, dcp=dcp, jl=jl: e.tensor_copy(out=XT[:, 2 * dcp:2 * dcp + 2, jl * CAP:(jl + 1) * CAP], in_=PD[b][:, 0:2 * CAP].rearrange("p (a s) -> p a s", a=2)), reads=[rPD[b]], writes=[rXT])
            k.dma("pool", BDb1[:], b_dn[ex:ex + 1, :], writes=[rBDb1], key="bd")
            for fc in range(16):
                wb, rwb = get_piece()
                for ns in range(NS):
                    b = cnt % 2
                    cnt += 1
                    for kc in range(16):
                        k.op("pe", lambda e, b=b, kc=kc, wb=wb, ns=ns: e.matmul(PA[b][:], lhsT=wb[:, kc, 0:128], rhs=XT[:, kc, ns * 512:(ns + 1) * 512], start=(kc == 0), stop=(kc == 15)),
                             reads=[rwb, rXT], writes=[rPA[b]], sig=(kc == 15))
                    for kc in range(16):
                        k.op("pe", lambda e, b=b, kc=kc, wb=wb, ns=ns: e.matmul(PB[b][:], lhsT=wb[:, kc, 128:256], rhs=XT[:, kc, ns * 512:(ns + 1) * 512], start=(kc == 0), stop=(kc == 15)),
                             reads=[rwb, rXT], writes=[rPB[b]], sig=(kc == 15))
                    k.op("dve", lambda e, b=b, ex=ex, fc=fc: e.tensor_scalar(out=T1[b][:], in0=PA[b][:], scalar1=BGU[:, ex, fc:fc + 1], scalar2=7.0, op0=ALU.add, op1=ALU.min),
                         reads=[rPA[b], rBGU], writes=[rT1[b]])
                    k.op("act", lambda e, b=b: e.activation(out=T2[b][:], in_=T1[b][:], func=AF.Sigmoid, scale=1.702), reads=[rT1[b]], writes=[rT2[b]])
                    k.op("dve", lambda e, b=b, ex=ex, fc=fc: e.tensor_scalar(out=T3[b][:], in0=PB[b][:], scalar1=BGU[:, ex, 16 + fc:17 + fc], scalar2=7.0, op0=ALU.add, op1=ALU.min),
                         reads=[rPB[b], rBGU], writes=[rT3[b]])
                    k.op("dve", lambda e, b=b: e.tensor_scalar(out=T3[b][:], in0=T3[b][:], scalar1=-7.0, scalar2=1.0, op0=ALU.max, op1=ALU.add), reads=[rT3[b]], writes=[rT3[b]])
                    k.op("pool", lambda e, b=b: e.tensor_tensor(out=T1[b][:], in0=T1[b][:], in1=T2[b][:], op=ALU.mult), reads=[rT1[b], rT2[b]], writes=[rT1[b]])
                    k.op("pool", lambda e, b=b, fc=fc, ns=ns: e.tensor_tensor(out=AT[:, fc, ns * 512:(ns + 1) * 512], in0=T1[b][:], in1=T3[b][:], op=ALU.mult),
                         reads=[rT1[b], rT3[b]], writes=[rAT[fc]])
            for dp in range(8):
                wb, rwb = get_piece()
                for sc in range(HSLOT // 128):
                    b = dcnt % 2
                    dcnt += 1
                    k.op("pe", lambda e, b=b, dp=dp, ex=ex: e.matmul(PD[b][:, 0:256], lhsT=ONES[0:1, :], rhs=BDb1[:, dp * 256:(dp + 1) * 256], start=True, stop=False),
                         reads=[rONES, rBDb1], writes=[rPD[b]], sig=False)
                    for fc in range(16):
                        k.op("pe", lambda e, b=b, sc=sc, fc=fc, wb=wb: e.matmul(PD[b][:, 0:256], lhsT=AT[:, fc, sc * 128:(sc + 1) * 128], rhs=wb[:, fc, :], start=False, stop=(fc == 15)),
                             reads=[rAT[fc], rwb], writes=[rPD[b]], sig=(fc == 15))
                    yb_ = ycnt % 4
                    ycnt += 1
                    if yb_ % 2 == 0:
                        k.op("act", lambda e, b=b, yb_=yb_: e.copy(out=YST[yb_][:], in_=PD[b][:, 0:256]), reads=[rPD[b]], writes=[rYST[yb_]])
                    else:
                        k.op("dve", lambda e, b=b, yb_=yb_: e.tensor_copy(out=YST[yb_][:], in_=PD[b][:, 0:256]), reads=[rPD[b]], writes=[rYST[yb_]])
                    k.dma("sp", ysp[ex, half * HSLOT + sc * 128:half * HSLOT + (sc + 1) * 128, dp * 256:(dp + 1) * 256], YST[yb_][:], reads=[rYST[yb_]], key="ys%d" % yb_)
        xtf = XT[:].rearrange("p a b -> p (a b)")
        atf = AT[:].rearrange("p a b -> p (a b)")
        hbf = HBs[0][:].rearrange("p a b -> p (a b)")
        Ybuf = []
        for src in (xtf, hbf):
            Ybuf.append((src[:, 0:EPC * D].rearrange("p (e d) -> p e d", e=EPC), src[:, EPC * D:2 * EPC * D].rearrange("p (e d) -> p e d", e=EPC)))
        SGbuf = []
        for o in (0, 2 * EPC * 1024):
            SGbuf.append((atf[:, o:o + EPC * 1024].rearrange("p (e t) -> p e t", e=EPC), atf[:, o + EPC * 1024:o + 2 * EPC * 1024].rearrange("p (e t) -> p e t", e=EPC)))
        rYb = [Res(), Res()]; rSGb = [Res(), Res()]
        banks = [(PA[0], rPA[0]), (PA[1], rPA[1]), (PB[0], rPB[0]), (PB[1], rPB[1]), (PD[0], rPD[0]), (PD[1], rPD[1])]
        bcnt = 0
        ocnt = 0
        for j in range(NBLK):
            par = j % 2
            Y1, Y2 = Ybuf[par]
            SG1, SG2 = SGbuf[par]
            rY12, rSG = rYb[par], rSGb[par]
            for ex in range(EPC):
                if j < 2 and ex == 0:
                    wr = [rY12] + rYST + ([rXT] if par == 0 else rHBs)
                else:
                    wr = [rY12]
                k.dma("sp", Y1[:, ex, :], ysp[ex, j * CAP:j * CAP + 128, :], writes=wr, key="y1%d" % par)
                k.dma("sp", Y2[:, ex, :], ysp[ex, j * CAP + 128:(j + 1) * CAP, :], writes=[rY12], key="y1%d" % par)
            for ex in range(EPC):
                sb_ = scnt % 2
                scnt += 1
                for tc in range(8):
                    k.op("dve", lambda e, sb_=sb_, tc=tc, j=j, ex=ex: e.tensor_scalar(out=S[sb_][:, tc, :], in0=IOTA[:], scalar1=POSM[:, j * 8 + tc, ex:ex + 1], scalar2=GC[:, j * 8 + tc, ex:ex + 1], op0=ALU.is_equal, op1=ALU.mult),
                         reads=[rIOTA, rPOSM, rGC], writes=[rS[sb_]])
                for tc in range(8):
                    k.op("pe", lambda e, sb_=sb_, tc=tc: e.transpose(out=PT[0][:, tc * 128:(tc + 1) * 128], in_=S[sb_][:, tc, 0:128], identity=ID[:]), reads=[rS[sb_], rID], writes=[rPT[0]], sig=(tc == 7))
                for tc in range(8):
                    k.op("pe", lambda e, sb_=sb_, tc=tc: e.transpose(out=PT[1][:, tc * 128:(tc + 1) * 128], in_=S[sb_][:, tc, 128:CAP], identity=ID[:]), reads=[rS[sb_], rID], writes=[rPT[1]], sig=(tc == 7))
                k.op("act", lambda e, ex=ex, SG1=SG1: e.copy(out=SG1[:, ex, :], in_=PT[0][:]), reads=[rPT[0]], writes=[rSG] + (rAT if j < 2 else []))
                k.op("dve", lambda e, ex=ex, SG2=SG2: e.tensor_copy(out=SG2[:, ex, :], in_=PT[1][:]), reads=[rPT[1]], writes=[rSG])
            for tc in range(8):
                ob = ocnt % 2
                ocnt += 1
                for dg in range(4):
                    pb_, rpb_ = banks[bcnt % 6]
                    bcnt += 1
                    n = 0
                    for ex in range(EPC):
                        k.op("pe", lambda e, pb_=pb_, ex=ex, tc=tc, dg=dg, n=n, SG1=SG1, Y1=Y1: e.matmul(pb_[:], lhsT=SG1[:, ex, tc * 128:(tc + 1) * 128], rhs=Y1[:, ex, dg * 512:(dg + 1) * 512], start=(n == 0), stop=False),
                             reads=[rSG, rY12], writes=[rpb_], sig=False)
                        n += 1
                        k.op("pe", lambda e, pb_=pb_, ex=ex, tc=tc, dg=dg, n=n, SG2=SG2, Y2=Y2: e.matmul(pb_[:], lhsT=SG2[:, ex, tc * 128:(tc + 1) * 128], rhs=Y2[:, ex, dg * 512:(dg + 1) * 512], start=False, stop=(n == 2 * EPC - 1)),
                             reads=[rSG, rY12], writes=[rpb_], sig=(n == 2 * EPC - 1))
                        n += 1
                    if dg % 2 == 0:
                        k.op("act", lambda e, pb_=pb_, ob=ob, dg=dg: e.copy(out=OUT[ob][:, dg * 512:(dg + 1) * 512], in_=pb_[:]), reads=[rpb_], writes=[rOUT[ob]])
                    else:
                        k.op("dve", lambda e, pb_=pb_, ob=ob, dg=dg: e.tensor_copy(out=OUT[ob][:, dg * 512:(dg + 1) * 512], in_=pb_[:]), reads=[rpb_], writes=[rOUT[ob]])
                k.dma("sp", y_out[j * 1024 + tc * 128:j * 1024 + (tc + 1) * 128, :], OUT[ob][:], reads=[rOUT[ob]], key="oy%d" % ob, is_out=True)
        k.finish()
    return nc


def build_post():
    nc = bass.Bass("TRN2", target_bir_lowering=False)
    dt_in = lambda name, shape, dt=F32: nc.dram_tensor(name, list(shape), dt, kind="ExternalInput").ap()
    h1 = dt_in("h1", [TPC, D])
    parts = dt_in("parts", [NCORES, TPC, D])
    pin = dt_in("p", [TPC, PLE])
    identb = dt_in("identb", [128, 128], BF16)
    ln2g = dt_in("ln2_g", [D]); ln2b = dt_in("ln2_b", [D])
    ln3g = dt_in("ln3_g", [D]); ln3b = dt_in("ln3_b", [D])
    wg = dt_in("w_ple_gate", [D, D])
    bg = dt_in("b_ple_gate", [D])
    wp = dt_in("w_ple_proj", [PLE, D])
    h3 = nc.dram_tensor("h3", [TPC, D], F32, kind="ExternalOutput").ap()
    NJ = TPC // 128
    with ExitStack() as es:
        k = KB(nc, es)
        ID = k.sb("ID", [128, 128], BF16); rID = Res()
        LG = k.sb("LG", [128, D], F32); rLG = Res()
        LB = k.sb("LB", [128, D], F32); rLB = Res()
        BG = k.sb("BG", [128, D], F32); rBG = Res()
        WP = k.sb("WP", [128, 2, D], BF16); rWP = Res()
        H = k.sb("H", [128, NJ, D], F32); rH = [Res() for _ in range(NJ)]
        PBUF = [k.sb("PBUF%d" % i, [128, D], F32) for i in range(3)]; rPBUF = [Res() for _ in range(3)]
        HT = k.sb("HT", [128, 16, 512], BF16); rHT = [Res() for _ in range(4)]
        PTT = k.sb("PTT", [128, 2, 512], BF16); rPTT = [Res() for _ in range(4)]
        hb = k.sb("hb", [128, D], BF16); rhb = Res()
        p32 = k.sb("p32", [128, PLE], F32); rp32 = Res()
        pb = k.sb("pb", [128, PLE], BF16); rpb = Res()
        t1 = [k.sb("t1_%d" % i, [128, 256], F32) for i in range(2)]; rt1 = [Res(), Res()]
        ring = WRing(k, "wq", 3)
        lt = ln_tmp(k, "lt")
        PT = [k.ps("PT%d" % i, [128, 1024], BF16) for i in range(2)]; rPT = [Res(), Res()]
        PP = [k.ps("PP%d" % i, [128, 512], F32) for i in range(2)]; rPP = [Res(), Res()]
        ptc = [0]; ppc = [0]

        def nextPT():
            i = ptc[0] % 2; ptc[0] += 1
            return PT[i], rPT[i]

        def nextPP():
            i = ppc[0] % 2; ppc[0] += 1
            return PP[i], rPP[i]

        k.dma("sp", ID[:], identb, writes=[rID], key="c0")
        k.dma("sp", LG[:], ln2g.partition_broadcast(128), writes=[rLG], key="c1")
        k.dma("sp", LB[:], ln2b.partition_broadcast(128), writes=[rLB], key="c2")
        k.dma("sp", BG[:], bg.partition_broadcast(128), writes=[rBG], key="c3")
        k.dma("pool", WP[:], wp.rearrange("(kc p) n -> p kc n", p=128), writes=[rWP], key="c4")
        pc = 0
        for j in range(NJ):
            k.dma("sp", H[:, j, :], h1[j * 128:(j + 1) * 128, :], writes=[rH[j]], key="h%d" % j)
            k.op("act", lambda e, j=j: e.mul(out=H[:, j, :], in_=H[:, j, :], mul=ALPHA), reads=[rH[j]], writes=[rH[j]])
            for c in range(NCORES):
                b = pc % 3
                pc += 1
                k.dma("sp", PBUF[b][:], parts[c, j * 128:(j + 1) * 128, :], writes=[rPBUF[b]], key="pb%d" % b)
                eng = "dve" if c % 2 == 0 else "pool"
                k.op(eng, lambda e, b=b, j=j: e.tensor_tensor(out=H[:, j, :], in0=H[:, j, :], in1=PBUF[b][:], op=ALU.add), reads=[rH[j], rPBUF[b]], writes=[rH[j]])
            emit_layernorm(k, H[:, j, :], rH[j], LG, rLG, LB, rLB, lt)
        k.dma("sp", LG[:], ln3g.partition_broadcast(128), writes=[rLG], key="c1")
        k.dma("sp", LB[:], ln3b.partition_broadcast(128), writes=[rLB], key="c2")
        for grp in ([0, 1, 2, 3], [4, 5, 6, 7]):
            wb_next, rwb_next = ring.load([(0, 256, wg[:, 0:256])])
            for c, j in enumerate(grp):
                k.op("act", lambda e, j=j: e.copy(out=hb[:], in_=H[:, j, :]), reads=[rH[j]], writes=[rhb])
                for half in range(2):
                    pt, rpt = nextPT()
                    for i in range(8):
                        kc = half * 8 + i
                        k.op("pe", lambda e, pt=pt, kc=kc, i=i: e.transpose(out=pt[:, i * 128:(i + 1) * 128], in_=hb[:, kc * 128:(kc + 1) * 128], identity=ID[:]),
                             reads=[rhb, rID], writes=[rpt], sig=(i == 7))
                    k.op("dve", lambda e, pt=pt, half=half, c=c: e.tensor_copy(out=HT[:, half * 8:(half + 1) * 8, c * 128:(c + 1) * 128], in_=pt[:].rearrange("p (a b) -> p a b", a=8)),
                         reads=[rpt], writes=[rHT[c]])
                k.op("act", lambda e, j=j: e.mul(out=H[:, j, :], in_=H[:, j, :], mul=ALPHA), reads=[rH[j]], writes=[rH[j]])
                k.dma("sp", p32[:], pin[j * 128:(j + 1) * 128, :], writes=[rp32], key="pp")
                k.op("act", lambda e: e.copy(out=pb[:], in_=p32[:]), reads=[rp32], writes=[rpb])
                pt, rpt = nextPT()
                for i in range(2):
                    k.op("pe", lambda e, pt=pt, i=i: e.transpose(out=pt[:, i * 128:(i + 1) * 128], in_=pb[:, i * 128:(i + 1) * 128], identity=ID[:]),
                         reads=[rpb, rID], writes=[rpt], sig=(i == 1))
                k.op("dve", lambda e, pt=pt, c=c: e.tensor_copy(out=PTT[:, :, c * 128:(c + 1) * 128], in_=pt[:, 0:256].rearrange("p (a b) -> p a b", a=2)),
                     reads=[rpt], writes=[rPTT[c]])
            for gi in range(8):
                wb, rwb = wb_next, rwb_next
                if gi < 7:
                    wb_next, rwb_next = ring.load([(0, 256, wg[:, (gi + 1) * 256:(gi + 2) * 256])])
                cols = slice(gi * 256, (gi + 1) * 256)
                for c, j in enumerate(grp):
                    pp, rpp = nextPP()
                    for kc in range(16):
                        k.op("pe", lambda e, pp=pp, kc=kc, c=c, wb=wb: e.matmul(pp[:, 0:256], lhsT=HT[:, kc, c * 128:(c + 1) * 128], rhs=wb[:, kc, :], start=(kc == 0), stop=(kc == 15)),
                             reads=[rHT[c], rwb], writes=[rpp], sig=False)
                    for kc in range(2):
                        k.op("pe", lambda e, pp=pp, kc=kc, c=c, cols=cols: e.matmul(pp[:, 256:512], lhsT=PTT[:, kc, c * 128:(c + 1) * 128], rhs=WP[:, kc, cols], start=(kc == 0), stop=(kc == 1)),
                             reads=[rPTT[c], rWP], writes=[rpp], sig=(kc == 1))
                    b = (gi * 4 + c) % 2
                    k.op("dve", lambda e, pp=pp, b=b, cols=cols: e.tensor_tensor(out=t1[b][:], in0=pp[:, 0:256], in1=BG[:, cols], op=ALU.add), reads=[rpp, rBG], writes=[rt1[b]])
                    k.op("act", lambda e, b=b: e.activation(out=t1[b][:], in_=t1[b][:], func=AF.Sigmoid), reads=[rt1[b]], writes=[rt1[b]])
                    k.op("dve", lambda e, pp=pp, b=b: e.tensor_tensor(out=t1[b][:], in0=t1[b][:], in1=pp[:, 256:512], op=ALU.mult), reads=[rt1[b], rpp], writes=[rt1[b]])
                    k.op("pool", lambda e, b=b, j=j, cols=cols: e.tensor_tensor(out=H[:, j, cols], in0=H[:, j, cols], in1=t1[b][:], op=ALU.add), reads=[rt1[b], rH[j]], writes=[rH[j]])
            for j in grp:
                emit_layernorm(k, H[:, j, :], rH[j], LG, rLG, LB, rLB, lt)
                k.dma("sp", h3[j * 128:(j + 1) * 128, :], H[:, j, :], reads=[rH[j]], key="oh", is_out=True)
        k.finish()
    return nc


def _consts():
    q = np.arange(128)[:, None]
    j = np.arange(128)[None, :]
    prev = np.where(j > q, 0.0, NEG).astype(np.float32)
    cur = np.where(j <= q, 0.0, NEG).astype(np.float32)
    std = np.concatenate([prev, cur], axis=1)
    first = np.concatenate([np.full((128, 128), NEG, np.float32), cur], axis=1)
    half = 8
    invf = np.exp(-np.log(500000.0) * np.arange(half, dtype=np.float32) * (2.0 / 16)).astype(np.float32)
    return dict(
        identb=np.eye(128).astype(ml_dtypes.bfloat16),
        identf=np.eye(128, dtype=np.float32),
        trild=(j <= q).astype(np.float32),
        invfd=np.ascontiguousarray(np.broadcast_to(invf[None, :], (128, 8))),
        mask_std=std, mask_first=first,
        iotad=np.ascontiguousarray(np.broadcast_to(np.arange(CAP, dtype=np.float32)[None, :], (128, CAP))),
        sud=(q < j).astype(ml_dtypes.bfloat16),
        onesd=np.ones((128, 128), ml_dtypes.bfloat16),
    )


def mixer_inputs(layer, h_full, positions, prm):
    cst = _consts()
    maps = []
    pos_flat = positions.reshape(-1)
    for c in range(NCORES):
        t0 = c * TPC
        seq_start = (t0 % SEQ) == 0
        xin = np.zeros((NCH * 128, D), np.float32)
        pos = np.zeros((NCH * 128,), np.int32)
        xin[128:] = h_full[t0:t0 + TPC]
        pos[128:] = pos_flat[t0:t0 + TPC]
        if not seq_start:
            xin[:128] = h_full[t0 - 128:t0]
            pos[:128] = pos_flat[t0 - 128:t0]
        m = dict(
            xin=xin, posi=np.ascontiguousarray(pos.reshape(NCH, 128).T),
            maskd=np.ascontiguousarray(np.stack([cst["mask_first"] if seq_start else cst["mask_std"], cst["mask_std"]], axis=1)),
            identb=cst["identb"], identf=cst["identf"], trild=cst["trild"], invfd=cst["invfd"],
            w_in=prm["w_in"][layer], sinks=prm["sinks"][layer], w_s=prm["w_s"][layer],
            bsT=np.ascontiguousarray(prm["b_s"][layer].T),
            vnorm_g=np.ascontiguousarray(prm["vnorm_g"][layer].reshape(-1)), vnorm_b=np.ascontiguousarray(prm["vnorm_b"][layer].reshape(-1)),
            gnT=np.ascontiguousarray(np.concatenate([prm["gnorm_attn"][layer], prm["gnorm_gmlp"][layer]]).reshape(16, 128).T),
            w_o=prm["w_o"][layer], ln1_g=prm["ln1_g"][layer], ln1_b=prm["ln1_b"][layer],
            w_router=prm["w_router"][layer], b_router=prm["b_router"][layer],
        )
        if layer == 0:
            m["ln_in_g"] = prm["ln_in_g"]
            m["ln_in_b"] = prm["ln_in_b"]
        maps.append(m)
    return maps


_PROGS = {}


def _prog(name, fn):
    if name not in _PROGS:
        _PROGS[name] = fn()
    return _PROGS[name]


def kernel(**inp):
    prm = {k_: np.asarray(v) for k_, v in inp.items()}
    cores = list(range(NCORES))
    cst = _consts()
    h_full = np.ascontiguousarray(prm["x"].reshape(NTOK, D))
    for layer in range(DEPTH):
        nc = _prog("mix%d" % (layer == 0), lambda: build_mixer(layer == 0))
        res = run_bass_kernel_spmd(nc, mixer_inputs(layer, h_full, prm["positions"], prm), core_ids=cores).results
        h1 = [res[c]["h1"] for c in cores]
        hbt = np.concatenate([res[c]["hbt"] for c in cores], axis=0)
        G = np.concatenate([res[c]["gates"] for c in cores], axis=0)
        del res
        nc = _prog("moe2", build_moe2)
        maps = []
        for c in cores:
            es_ = slice(c * EPC, (c + 1) * EPC)
            maps.append(dict(
                hbt=hbt, gC=np.ascontiguousarray(G[:, es_].reshape(NTOK // 128, 128, EPC).transpose(1, 0, 2)),
                iotad=cst["iotad"], sud=cst["sud"], onesd=cst["onesd"], identb=cst["identb"],
                w_gu=prm["w_gu"][layer, es_],
                bguT=np.ascontiguousarray(prm["b_gu"][layer, es_].reshape(EPC, 32, 128).transpose(2, 0, 1)),
                w_down=prm["w_down"][layer, es_], b_down=prm["b_down"][layer, es_]))
        res = run_bass_kernel_spmd(nc, maps, core_ids=cores).results
        yp = [res[c]["ypart"] for c in cores]
        del res, maps
        nc = _prog("post", build_post)
        p_l = prm["p"][layer].reshape(NTOK, PLE)
        maps = []
        for c in cores:
            ts = slice(c * TPC, (c + 1) * TPC)
            maps.append(dict(
                h1=h1[c], parts=np.ascontiguousarray(np.stack([yp[e][ts] for e in cores], axis=0)),
                p=np.ascontiguousarray(p_l[ts]), identb=cst["identb"],
                ln2_g=prm["ln2_g"][layer], ln2_b=prm["ln2_b"][layer], ln3_g=prm["ln3_g"][layer], ln3_b=prm["ln3_b"][layer],
                w_ple_gate=prm["w_ple_gate"][layer], b_ple_gate=prm["b_ple_gate"][layer], w_ple_proj=prm["w_ple_proj"][layer]))
        del yp
        res = run_bass_kernel_spmd(nc, maps, core_ids=cores).results
        h_full = np.concatenate([res[c]["h3"] for c in cores], axis=0)
        del res, maps
    return h_full.reshape(BATCH, SEQ, D).astype(np.float32)
```

```python
import numpy as np
import ml_dtypes
from contextlib import ExitStack
import concourse.bass as bass
import concourse.mybir as mybir
from concourse.bass_utils import run_bass_kernel_spmd

F32 = mybir.dt.float32
BF16 = mybir.dt.bfloat16
I32 = mybir.dt.int32
ALU = mybir.AluOpType
AF = mybir.ActivationFunctionType
AX = mybir.AxisListType

NCORES = 8
D = 2048
DEPTH = 2
SEQ = 2048
BATCH = 4
NTOK = BATCH * SEQ
TPC = NTOK // NCORES
D_ATTN = 1024
D_GMLP = 1024
D_IN = 3328
NE = 32
EPC = NE // NCORES
PLE = 256
ALPHA = (2.0 * DEPTH) ** 0.25
EPS = 1e-5
PI = float(np.pi)
NEG = -30000.0
EMBED_WAIT = True


class Res:
    __slots__ = ("w", "r")

    def __init__(self):
        self.w = None
        self.r = []


class KB:
    def __init__(self, nc, es):
        self.nc = nc
        self.es = es
        self.eng = {"pe": nc.tensor, "act": nc.scalar, "dve": nc.vector, "pool": nc.gpsimd, "sp": nc.sync}
        self.ops = {k: [] for k in self.eng}
        self.sem = {k: es.enter_context(nc.semaphore("s_" + k)) for k in self.eng}
        self.cnt = {k: 0 for k in self.eng}
        self.waited = {k: {} for k in self.eng}
        self.dsem = {}
        self.out_toks = []

    def sb(self, name, shape, dt):
        return self.es.enter_context(self.nc.sbuf_tensor(name, list(shape), dt))

    def ps(self, name, shape, dt):
        return self.es.enter_context(self.nc.psum_tensor(name, list(shape), dt))

    def _deps(self, e, reads, writes):
        toks = []
        for r in reads:
            if r.w is not None:
                toks.append(r.w)
        for w in writes:
            if w.w is not None:
                toks.append(w.w)
            toks.extend(w.r)
        waits = []
        wd = self.waited[e]
        for (skey, sem, val, teng) in toks:
            if teng == e and e == "pe":
                continue
            if wd.get(skey, 0) >= val:
                continue
            wd[skey] = val
            waits.append((sem, val))
        return waits

    def op(self, e, fn, reads=(), writes=(), sig=True):
        waits = self._deps(e, reads, writes)
        if sig:
            self.cnt[e] += 1
            val = self.cnt[e]
        else:
            val = self.cnt[e] + 1
        tok = (e, self.sem[e], val, e)
        for r in reads:
            r.r.append(tok)
        for w in writes:
            w.w = tok
            w.r = []
        self.ops[e].append((waits, fn, (self.sem[e], 1) if sig else None))
        return tok

    def dma(self, e, out, in_, reads=(), writes=(), key="misc", is_out=False):
        waits = self._deps(e, reads, writes)
        if key not in self.dsem:
            self.dsem[key] = [self.es.enter_context(self.nc.semaphore("d_" + str(key))), 0]
        ds = self.dsem[key]
        ds[1] += 16
        tok = ("d_" + str(key), ds[0], ds[1], "dma")
        for r in reads:
            r.r.append(tok)
        for w in writes:
            w.w = tok
            w.r = []
        self.ops[e].append((waits, (lambda eng, o=out, i=in_: eng.dma_start(out=o, in_=i)), (ds[0], 16)))
        if is_out:
            self.out_toks.append(tok)
        return tok

    def finish(self):
        waits = []
        seen = {}
        for (skey, sem, val, teng) in self.out_toks:
            if seen.get(skey, (None, 0))[1] < val:
                seen[skey] = (sem, val)
        for skey, (sem, val) in seen.items():
            waits.append((sem, val))
        self.ops["sp"].append((waits, None, None))
        with self.nc.Block() as block:
            def mk(name):
                def body(eng):
                    for waits, fn, inc in self.ops[name]:
                        if fn is None:
                            for sem, val in waits:
                                eng.wait_ge(sem, val)
                            continue
                        emb = None
                        if EMBED_WAIT and waits:
                            emb = waits[-1]
                            waits = waits[:-1]
                        for sem, val in waits:
                            eng.wait_ge(sem, val)
                        ins = fn(eng)
                        if emb is not None:
                            ins._wait_ge(emb[0], emb[1])
                        if inc is not None:
                            ins.then_inc(inc[0], inc[1])
                return body
            block.tensor(mk("pe"))
            block.scalar(mk("act"))
            block.vector(mk("dve"))
            block.gpsimd(mk("pool"))
            block.sync(mk("sp"))


def bcast_rows(ap, n=128):
    return ap.partition_broadcast(n)


def emit_layernorm(k, X, rX, G, rG, B, rB, tmp):
    st, rst, mv, rmv, rs, rrs = tmp
    for i in range(4):
        k.op("dve", lambda e, i=i: e.bn_stats(out=st[:, i, :], in_=X[:, i * 512:(i + 1) * 512]),
             reads=[rX], writes=[rst])
    k.op("dve", lambda e: e.bn_aggr(out=mv[:], in_=st[:].rearrange("p a b -> p (a b)")), reads=[rst], writes=[rmv])
    k.op("act", lambda e: e.activation(out=rs[:], in_=mv[:, 1:2], func=AF.Sqrt, bias=EPS, scale=1.0),
         reads=[rmv], writes=[rrs])
    k.op("dve", lambda e: e.reciprocal(out=rs[:], in_=rs[:]), reads=[rrs], writes=[rrs])
    k.op("dve", lambda e: e.tensor_scalar(out=X, in0=X, scalar1=mv[:, 0:1], scalar2=rs[:, 0:1],
                                          op0=ALU.subtract, op1=ALU.mult), reads=[rX, rmv, rrs], writes=[rX])
    k.op("pool", lambda e: e.tensor_tensor(out=X, in0=X, in1=G[:], op=ALU.mult), reads=[rX, rG], writes=[rX])
    k.op("pool", lambda e: e.tensor_tensor(out=X, in0=X, in1=B[:], op=ALU.add), reads=[rX, rB], writes=[rX])


def ln_tmp(k, name):
    return (k.sb(name + "_st", [128, 4, 6], F32), Res(), k.sb(name + "_mv", [128, 2], F32), Res(),
            k.sb(name + "_rs", [128, 1], F32), Res())


class WRing:
    def __init__(self, k, name, nslots, kc=16, ncol=256):
        self.k = k
        self.n = nslots
        self.buf = [k.sb("%s%d" % (name, i), [128, kc, ncol], BF16) for i in range(nslots)]
        self.res = [Res() for _ in range(nslots)]
        self.i = 0
        self.name = name

    def load(self, parts):
        s = self.i % self.n
        self.i += 1
        for (c0, c1, src) in parts:
            self.k.dma("pool", self.buf[s][:, :, c0:c1], src.rearrange("(kc p) n -> p kc n", p=128),
                       writes=[self.res[s]], key="%s%d" % (self.name, s))
        return self.buf[s], self.res[s]


NCH = 9
GROUPS = [[0, 1, 2, 3, 4], [5, 6, 7, 8]]


def build_mixer(layer0, stop=None):
    nc = bass.Bass("TRN2", target_bir_lowering=False)
    dt_in = lambda name, shape, dt=F32: nc.dram_tensor(name, list(shape), dt, kind="ExternalInput").ap()
    dt_out = lambda name, shape, dt=F32: nc.dram_tensor(name, list(shape), dt, kind="ExternalOutput").ap()
    xin = dt_in("xin", [NCH * 128, D])
    posi = dt_in("posi", [128, NCH], I32)
    maskd = dt_in("maskd", [128, 2, 256])
    identb = dt_in("identb", [128, 128], BF16)
    identf = dt_in("identf", [128, 128])
    trild = dt_in("trild", [128, 128])
    invfd = dt_in("invfd", [128, 8])
    if layer0:
        lning = dt_in("ln_in_g", [D])
        lninb = dt_in("ln_in_b", [D])
    w_in = dt_in("w_in", [D, D_IN])
    sinksd = dt_in("sinks", [16])
    w_s = dt_in("w_s", [8, 128, 128])
    bsT = dt_in("bsT", [128, 8])
    vng = dt_in("vnorm_g", [1024])
    vnb = dt_in("vnorm_b", [1024])
    gnT = dt_in("gnT", [128, 16])
    w_o = dt_in("w_o", [D, D])
    ln1g = dt_in("ln1_g", [D])
    ln1b = dt_in("ln1_b", [D])
    w_r = dt_in("w_router", [D, NE])
    b_r = dt_in("b_router", [NE])
    h1_out = dt_out("h1", [TPC, D])
    hT_out = dt_out("hT", [16, 128, TPC], BF16)
    hbt_out = dt_out("hbt", [TPC, D], BF16)
    g_out = dt_out("gates", [TPC, NE])

    with ExitStack() as es:
        k = KB(nc, es)
        ID = k.sb("ID", [128, 128], BF16); rID = Res()
        MASK = k.sb("MASK", [128, 2, 256], F32); rMASK = Res()
        TRIL = k.sb("TRIL", [128, 128], F32); rTRIL = Res()
        INVF = k.sb("INVF", [128, 8], F32); rINVF = Res()
        POSI = k.sb("POSI", [128, NCH], I32); rPOSI = Res()
        LG = k.sb("LG", [128, D], F32); rLG = Res()
        LB = k.sb("LB", [128, D], F32); rLB = Res()
        SINK = k.sb("SINK", [128, 16], F32); rSINK = Res()
        VNG = k.sb("VNG", [128, 1024], F32); rVNG = Res()
        VNB = k.sb("VNB", [128, 1024], F32); rVNB = Res()
        BST = k.sb("BST", [128, 8], F32); rBST = Res()
        GNT = k.sb("GNT", [128, 16], F32); rGNT = Res()
        WR = k.sb("WR", [128, 16, NE], F32); rWR = Res()
        BR = k.sb("BR", [128, NE], F32); rBR = Res()
        WST = k.sb("WST", [128, 8, 128], BF16); rWST = Res()
        H = k.sb("H", [128, NCH, D], F32); rH = [Res() for _ in range(NCH)]
        COS = k.sb("COS", [128, NCH, 4, 8], F32); rCOS = Res()
        SIN = k.sb("SIN", [128, NCH, 4, 8], F32); rSIN = Res()
        KT = k.sb("KT", [128, 2, NCH * 128], BF16); rKT = [Res() for _ in range(NCH)]
        V = k.sb("V", [128, NCH, 128], BF16); rV = [Res() for _ in range(NCH)]
        HT = k.sb("HT", [128, 16, 640], BF16); rHT = [Res() for _ in range(5)]
        YT = k.sb("YT", [128, 16, 512], BF16); rYT = [Res() for _ in range(4)]
        SSQ = k.sb("SSQ", [128, NCH, 12], F32); rSSQ = [Res() for _ in range(NCH)]
        ring = WRing(k, "wr", 3)
        lt = ln_tmp(k, "lt")
        hb = k.sb("hb", [128, D], BF16); rhb = Res()
        t8 = [k.sb("t8_%d" % i, [128, 4, 8], F32) for i in range(4)]; rt8 = [Res() for _ in range(4)]
        qb = k.sb("qb", [128, 4, 64], BF16); rqb = Res()
        kd = k.sb("kd", [128, 2, 64], BF16); rkd = Res()
        qT = k.sb("qT", [128, 4, 128], BF16); rqT = Res()
        sm = k.sb("sm", [128, 4, 256], F32); rsm = Res()
        eb = k.sb("eb", [128, 4, 256], BF16); reb = Res()
        eT = k.sb("eT", [128, 2, 4, 128], BF16); reT = Res()
        mx = k.sb("mx", [128, 4], F32); rmx = Res()
        sx = k.sb("sx", [128, 4], F32); rsx = Res()
        es_ = k.sb("es", [128, 4], F32); res_ = Res()
        o32 = k.sb("o32", [128, 256], F32); ro32 = Res()
        yb = k.sb("yb", [128, 256], BF16); ryb = Res()
        g1 = k.sb("g1", [128, 256], F32); rg1 = Res()
        junk, rjunk = g1, rg1
        g2 = k.sb("g2", [128, 256], F32); rg2 = Res()
        g3 = k.sb("g3", [128, 128], F32); rg3 = Res()
        vst = k.sb("vst", [128, 6], F32); rvst = Res()
        vmv = k.sb("vmv", [128, 2], F32); rvmv = Res()
        vrs = k.sb("vrs", [128, 1], F32); rvrs = Res()
        vh = k.sb("vh", [128, 128], BF16); rvh = Res()
        wtmp = k.sb("wtmp", [128, 128], F32); rwtmp = Res()
        wtb = k.sb("wtb", [128, 128], BF16); rwtb = Res()
        rsa = k.sb("rsa", [128, 2], F32); rrsa = Res()
        hloT = k.sb("hloT", [128, 16, 128], BF16); rhloT = Res()
        hlo = k.sb("hlo", [128, D], BF16); rhlo = Res()
        WRH = k.sb("WRH", [128, 16, NE], BF16); rWRH = Res()
        WRL = k.sb("WRL", [128, 16, NE], BF16); rWRL = Res()
        hbT = k.sb("hbT", [128, 16, 128], BF16); rhbT = Res()
        lg = k.sb("lg", [128, NE], F32); rlg = Res()
        m8 = k.sb("m8", [128, 8], F32); rm8 = Res()
        msk = k.sb("msk", [128, NE], F32); rmsk = Res()
        nm = k.sb("nm", [128, 1], F32); rnm = Res()
        gs = k.sb("gs", [128, 1], F32); rgs = Res()
        posf = k.sb("posf", [128, NCH], F32); rposf = Res()
        ang = k.sb("ang", [128, NCH, 8], F32); rang = Res()
        qf = k.sb("qf", [128, NCH, 8], F32); rqf = Res()
        qi = k.sb("qi", [128, NCH, 8], I32); rqi = Res()
        PT = [k.ps("PT%d" % i, [128, 1024], BF16) for i in range(2)]; rPT = [Res(), Res()]
        PP = [k.ps("PP%d" % i, [128, 512], F32) for i in range(2)]; rPP = [Res(), Res()]
        PS = k.ps("PS", [128, 4, 256], F32); rPS = Res()
        PV = k.ps("PV", [128, 512], F32); rPV = Res()
        PR = k.ps("PR", [128, 512], F32); rPR = Res()
        ptc = [0]
        ppc = [0]

        def nextPT():
            i = ptc[0] % 2; ptc[0] += 1
            return PT[i], rPT[i]

        def nextPP():
            i = ppc[0] % 2; ppc[0] += 1
            return PP[i], rPP[i]

        k.op("pool", lambda e: e.memset(SSQ[:], 0.0), writes=rSSQ)
        ld = lambda dst, src, r, key: k.dma("sp", dst, src, writes=[r], key=key)
        ld(ID[:], identb, rID, "c0"); ld(MASK[:], maskd, rMASK, "c2")
        ld(TRIL[:], trild, rTRIL, "c3"); ld(INVF[:], invfd, rINVF, "c4"); ld(POSI[:], posi, rPOSI, "c5")
        ld(SINK[:], sinksd.partition_broadcast(128), rSINK, "c6")
        ld(VNG[:], vng.partition_broadcast(128), rVNG, "c7"); ld(VNB[:], vnb.partition_broadcast(128), rVNB, "c8")
        ld(BST[:], bsT, rBST, "c9"); ld(GNT[:], gnT, rGNT, "c10")
        ld(WR[:], w_r.rearrange("(kc p) n -> p kc n", p=128), rWR, "c11")
        ld(BR[:], b_r.partition_broadcast(128), rBR, "c12")
        k.op("dve", lambda e: e.tensor_copy(out=WRH[:], in_=WR[:]), reads=[rWR], writes=[rWRH])
        k.op("dve", lambda e: e.tensor_tensor(out=WRL[:], in0=WR[:], in1=WRH[:], op=ALU.subtract), reads=[rWR, rWRH], writes=[rWRL])
        if layer0:
            ld(LG[:], lning.partition_broadcast(128), rLG, "c13"); ld(LB[:], lninb.partition_broadcast(128), rLB, "c14")
        for j in range(NCH):
            k.dma("sp", H[:, j, :], xin[j * 128:(j + 1) * 128, :], writes=[rH[j]], key="x%d" % j)
            if layer0:
                emit_layernorm(k, H[:, j, :], rH[j], LG, rLG, LB, rLB, lt)
        if stop == 'ln':
            k.finish()
            return nc
        ld(LG[:], ln1g.partition_broadcast(128), rLG, "c13"); ld(LB[:], ln1b.partition_broadcast(128), rLB, "c14")
        k.op("dve", lambda e: e.tensor_copy(out=posf[:], in_=POSI[:]), reads=[rPOSI], writes=[rposf])
        for which, TAB, rTAB in ((0, SIN, rSIN), (1, COS, rCOS)):
            for j in range(NCH):
                k.op("dve", lambda e, j=j, which=which: e.tensor_scalar(out=ang[:, j, :], in0=INVF[:], scalar1=posf[:, j:j + 1],
                                                           scalar2=(PI / 2 if which else 0.0), op0=ALU.mult, op1=ALU.add),
                     reads=[rINVF, rposf], writes=[rang])
            k.op("dve", lambda e: e.tensor_scalar(out=qf[:], in0=ang[:], scalar1=1.0 / (2 * PI), scalar2=None, op0=ALU.mult),
                 reads=[rang], writes=[rqf])
            k.op("dve", lambda e: e.tensor_copy(out=qi[:], in_=qf[:]), reads=[rqf], writes=[rqi])
            k.op("dve", lambda e: e.tensor_copy(out=qf[:], in_=qi[:]), reads=[rqi], writes=[rqf])
            k.op("dve", lambda e: e.scalar_tensor_tensor(out=ang[:], in0=qf[:], scalar=-2 * PI, in1=ang[:], op0=ALU.mult, op1=ALU.add),
                 reads=[rqf, rang], writes=[rang])
            k.op("dve", lambda e: e.tensor_scalar(out=qf[:], in0=ang[:], scalar1=PI, scalar2=None, op0=ALU.is_gt), reads=[rang], writes=[rqf])
            k.op("dve", lambda e: e.scalar_tensor_tensor(out=ang[:], in0=qf[:], scalar=-2 * PI, in1=ang[:], op0=ALU.mult, op1=ALU.add),
                 reads=[rqf, rang], writes=[rang])
            k.op("dve", lambda e: e.tensor_scalar(out=qf[:], in0=ang[:], scalar1=-PI, scalar2=None, op0=ALU.is_lt), reads=[rang], writes=[rqf])
            k.op("dve", lambda e: e.scalar_tensor_tensor(out=ang[:], in0=qf[:], scalar=2 * PI, in1=ang[:], op0=ALU.mult, op1=ALU.add),
                 reads=[rqf, rang], writes=[rang])
            for hh in range(4):
                k.op("act", lambda e, hh=hh, TAB=TAB: e.activation(out=TAB[:, :, hh, :], in_=ang[:], func=AF.Sin),
                     reads=[rang], writes=[rTAB])
        if stop == 'rope':
            k.finish()
            return nc
        for h in range(8):
            k.dma("sp", wtmp[:], w_s[h], writes=[rwtmp], key="ws")
            k.op("dve", lambda e: e.tensor_tensor(out=wtb[:], in0=wtmp[:], in1=TRIL[:], op=ALU.mult),
                 reads=[rwtmp, rTRIL], writes=[rwtb])
            pt, rpt = nextPT()
            k.op("pe", lambda e, pt=pt: e.transpose(out=pt[:, 0:128], in_=wtb[:], identity=ID[:]),
                 reads=[rwtb, rID], writes=[rpt])
            k.op("act", lambda e, pt=pt, h=h: e.copy(out=WST[:, h, :], in_=pt[:, 0:128]), reads=[rpt], writes=[rWST])

        if stop == 'wst':
            k.finish()
            return nc
        for grp in GROUPS:
            own = [j for j in grp if j >= 1]
            nloc = len(grp)
            loc = {j: i for i, j in enumerate(grp)}
            yloc = {j: i for i, j in enumerate(own)}
            wb_kv, rwb_kv = ring.load([(0, 256, w_in[:, 1024:1280])])
            for j in grp:
                k.op("act", lambda e, j=j: e.copy(out=hb[:], in_=H[:, j, :]), reads=[rH[j]], writes=[rhb])
                for half in range(2):
                    pt, rpt = nextPT()
                    for i in range(8):
                        kc = half * 8 + i
                        k.op("pe", lambda e, pt=pt, kc=kc, i=i: e.transpose(out=pt[:, i * 128:(i + 1) * 128], in_=hb[:, kc * 128:(kc + 1) * 128], identity=ID[:]),
                             reads=[rhb, rID], writes=[rpt], sig=(i == 7))
                    k.op("dve", lambda e, pt=pt, half=half, c=loc[j]: e.tensor_copy(out=HT[:, half * 8:(half + 1) * 8, c * 128:(c + 1) * 128], in_=pt[:].rearrange("p (a b) -> p a b", a=8)),
                         reads=[rpt], writes=[rHT[loc[j]]])
                if j >= 1:
                    k.op("act", lambda e, j=j: e.mul(out=H[:, j, :], in_=H[:, j, :], mul=ALPHA), reads=[rH[j]], writes=[rH[j]])
            if stop == 'ht':
                k.finish()
                return nc
            wb_next, rwb_next = ring.load([(0, 256, w_in[:, 0:256])])
            for j in grp:
                pp, rpp = nextPP()
                c = loc[j]
                for kc in range(16):
                    k.op("pe", lambda e, pp=pp, kc=kc, c=c: e.matmul(pp[:, 0:256], lhsT=HT[:, kc, c * 128:(c + 1) * 128], rhs=wb_kv[:, kc, :], start=(kc == 0), stop=(kc == 15)),
                         reads=[rHT[c], rwb_kv], writes=[rpp], sig=(kc == 15))
                kv = pp[:, 0:128].rearrange("p (g d) -> p g d", g=2)
                cs, sn = COS[:, j, 0:2, :], SIN[:, j, 0:2, :]
                x1, x2 = kv[:, :, 0:8], kv[:, :, 8:16]
                k.op("dve", lambda e, x1=x1, cs=cs: e.tensor_tensor(out=t8[0][:, 0:2, :], in0=x1, in1=cs, op=ALU.mult), reads=[rpp, rCOS], writes=[rt8[0]])
                k.op("dve", lambda e, x2=x2, sn=sn: e.tensor_tensor(out=t8[1][:, 0:2, :], in0=x2, in1=sn, op=ALU.mult), reads=[rpp, rSIN], writes=[rt8[1]])
                k.op("dve", lambda e, x2=x2, cs=cs: e.tensor_tensor(out=t8[2][:, 0:2, :], in0=x2, in1=cs, op=ALU.mult), reads=[rpp, rCOS], writes=[rt8[2]])
                k.op("dve", lambda e, x1=x1, sn=sn: e.tensor_tensor(out=t8[3][:, 0:2, :], in0=x1, in1=sn, op=ALU.mult), reads=[rpp, rSIN], writes=[rt8[3]])
                k.op("dve", lambda e: e.tensor_tensor(out=kd[:, :, 0:8], in0=t8[0][:, 0:2, :], in1=t8[1][:, 0:2, :], op=ALU.subtract), reads=[rt8[0], rt8[1]], writes=[rkd])
                k.op("dve", lambda e: e.tensor_tensor(out=kd[:, :, 8:16], in0=t8[2][:, 0:2, :], in1=t8[3][:, 0:2, :], op=ALU.add), reads=[rt8[2], rt8[3]], writes=[rkd])
                k.op("act", lambda e, kv=kv: e.copy(out=kd[:, :, 16:64], in_=kv[:, :, 16:64]), reads=[rpp], writes=[rkd])
                k.op("act", lambda e, pp=pp, j=j: e.copy(out=V[:, j, :], in_=pp[:, 128:256]), reads=[rpp], writes=[rV[j]])
                pt, rpt = nextPT()
                for g in range(2):
                    k.op("pe", lambda e, pt=pt, g=g: e.transpose(out=pt[0:64, g * 128:(g + 1) * 128], in_=kd[:, g, :], identity=ID[:]),
                         reads=[rkd, rID], writes=[rpt], sig=(g == 1))
                k.op("dve", lambda e, pt=pt, j=j: e.tensor_copy(out=KT[0:64, :, j * 128:(j + 1) * 128], in_=pt[0:64, 0:256].rearrange("p (g t) -> p g t", g=2)),
                     reads=[rpt], writes=[rKT[j]])
            if stop == 'kv':
                k.finish()
                return nc
            for qi_ in range(4):
                wb, rwb = wb_next, rwb_next
                if qi_ < 3:
                    wb_next, rwb_next = ring.load([(0, 256, w_in[:, (qi_ + 1) * 256:(qi_ + 2) * 256])])
                else:
                    wb_next, rwb_next = ring.load([(0, 128, w_in[:, 1280:1408]), (128, 256, w_in[:, 2304:2432])])
                g = qi_ // 2
                for j in own:
                    c = loc[j]
                    pp, rpp = nextPP()
                    for kc in range(16):
                        k.op("pe", lambda e, pp=pp, kc=kc, c=c, wb=wb: e.matmul(pp[:, 0:256], lhsT=HT[:, kc, c * 128:(c + 1) * 128], rhs=wb[:, kc, :], start=(kc == 0), stop=(kc == 15)),
                             reads=[rHT[c], rwb], writes=[rpp], sig=(kc == 15))
                    q4 = pp[:, 0:256].rearrange("p (h d) -> p h d", h=4)
                    cs, sn = COS[:, j, :, :], SIN[:, j, :, :]
                    x1, x2 = q4[:, :, 0:8], q4[:, :, 8:16]
                    k.op("dve", lambda e, x1=x1, cs=cs: e.tensor_tensor(out=t8[0][:], in0=x1, in1=cs, op=ALU.mult), reads=[rpp, rCOS], writes=[rt8[0]])
                    k.op("dve", lambda e, x2=x2, sn=sn: e.tensor_tensor(out=t8[1][:], in0=x2, in1=sn, op=ALU.mult), reads=[rpp, rSIN], writes=[rt8[1]])
                    k.op("dve", lambda e, x2=x2, cs=cs: e.tensor_tensor(out=t8[2][:], in0=x2, in1=cs, op=ALU.mult), reads=[rpp, rCOS], writes=[rt8[2]])
                    k.op("dve", lambda e, x1=x1, sn=sn: e.tensor_tensor(out=t8[3][:], in0=x1, in1=sn, op=ALU.mult), reads=[rpp, rSIN], writes=[rt8[3]])
                    k.op("dve", lambda e: e.tensor_tensor(out=qb[:, :, 0:8], in0=t8[0][:], in1=t8[1][:], op=ALU.subtract), reads=[rt8[0], rt8[1]], writes=[rqb])
                    k.op("dve", lambda e: e.tensor_tensor(out=qb[:, :, 8:16], in0=t8[2][:], in1=t8[3][:], op=ALU.add), reads=[rt8[2], rt8[3]], writes=[rqb])
                    k.op("act", lambda e, q4=q4: e.copy(out=qb[:, :, 16:64], in_=q4[:, :, 16:64]), reads=[rpp], writes=[rqb])
                    pt, rpt = nextPT()
                    for hh in range(4):
                        k.op("pe", lambda e, pt=pt, hh=hh: e.transpose(out=pt[0:64, hh * 128:(hh + 1) * 128], in_=qb[:, hh, :], identity=ID[:]),
                             reads=[rqb, rID], writes=[rpt], sig=(hh == 3))
                    k.op("dve", lambda e, pt=pt: e.tensor_copy(out=qT[0:64, :, :], in_=pt[0:64, 0:512].rearrange("p (a t) -> p a t", a=4)), reads=[rpt], writes=[rqT])
                    for hh in range(4):
                        k.op("pe", lambda e, hh=hh, j=j, g=g: e.matmul(PS[:, hh, :], lhsT=qT[0:64, hh, :], rhs=KT[0:64, g, (j - 1) * 128:(j + 1) * 128], start=True, stop=True),
                             reads=[rqT, rKT[j - 1], rKT[j]], writes=[rPS], sig=(hh == 3))
                    if stop == 'a1':
                        k.finish()
                        return nc
                    mi = 0 if j == 1 else 1
                    k.op("dve", lambda e, mi=mi: e.scalar_tensor_tensor(out=sm[:], in0=PS[:], scalar=0.125, in1=MASK[:, mi, :].unsqueeze(1).to_broadcast([128, 4, 256]), op0=ALU.mult, op1=ALU.add),
                         reads=[rPS, rMASK], writes=[rsm])
                    k.op("dve", lambda e: e.tensor_reduce(out=mx[:], in_=sm[:], axis=AX.X, op=ALU.max), reads=[rsm], writes=[rmx])
                    k.op("dve", lambda e, qi_=qi_: e.tensor_tensor(out=mx[:], in0=mx[:], in1=SINK[:, qi_ * 4:qi_ * 4 + 4], op=ALU.max), reads=[rmx, rSINK], writes=[rmx])
                    k.op("dve", lambda e: e.tensor_tensor(out=sm[:], in0=sm[:], in1=mx[:].unsqueeze(2).to_broadcast([128, 4, 256]), op=ALU.subtract), reads=[rsm, rmx], writes=[rsm])
                    k.op("act", lambda e: e.activation(out=eb[:], in_=sm[:], func=AF.Exp), reads=[rsm], writes=[reb])
                    k.op("dve", lambda e, qi_=qi_: e.tensor_tensor(out=es_[:], in0=SINK[:, qi_ * 4:qi_ * 4 + 4], in1=mx[:], op=ALU.subtract), reads=[rmx, rSINK], writes=[res_])
                    k.op("act", lambda e: e.activation(out=es_[:], in_=es_[:], func=AF.Exp), reads=[res_], writes=[res_])
                    k.op("dve", lambda e: e.tensor_reduce(out=sx[:], in_=eb[:], axis=AX.X, op=ALU.add), reads=[reb], writes=[rsx])
                    k.op("dve", lambda e: e.tensor_tensor(out=sx[:], in0=sx[:], in1=es_[:], op=ALU.add), reads=[rsx, res_], writes=[rsx])
                    k.op("dve", lambda e: e.reciprocal(out=sx[:], in_=sx[:]), reads=[rsx], writes=[rsx])
                    if stop == 'a2':
                        k.finish()
                        return nc
                    pt, rpt = nextPT()
                    for kcx in range(2):
                        for hh in range(4):
                            idx = kcx * 4 + hh
                            k.op("pe", lambda e, pt=pt, kcx=kcx, hh=hh, idx=idx: e.transpose(out=pt[:, idx * 128:(idx + 1) * 128], in_=eb[:, hh, kcx * 128:(kcx + 1) * 128], identity=ID[:]),
                                 reads=[reb, rID], writes=[rpt], sig=(idx == 7))
                    k.op("act", lambda e, pt=pt: e.copy(out=eT[:], in_=pt[:].rearrange("p (a h t) -> p a h t", a=2, h=4)), reads=[rpt], writes=[reT])
                    if stop == 'a3':
                        k.finish()
                        return nc
                    for hh in range(4):
                        for kcx in range(2):
                            k.op("pe", lambda e, hh=hh, kcx=kcx, j=j, g=g: e.matmul(PV[:, hh * 64:(hh + 1) * 64], lhsT=eT[:, kcx, hh, :], rhs=V[:, j - 1 + kcx, g * 64:(g + 1) * 64], start=(kcx == 0), stop=(kcx == 1)),
                                 reads=[reT, rV[j - 1], rV[j]], writes=[rPV], sig=(hh == 3 and kcx == 1))
                    if stop == 'a4':
                        k.finish()
                        return nc
                    k.op("dve", lambda e: e.tensor_tensor(out=o32[:].rearrange("p (h d) -> p h d", h=4), in0=PV[:, 0:256].rearrange("p (h d) -> p h d", h=4), in1=sx[:].unsqueeze(2).to_broadcast([128, 4, 64]), op=ALU.mult),
                         reads=[rPV, rsx], writes=[ro32])
                    if stop == 'a5':
                        k.finish()
                        return nc
                    k.op("act", lambda e, j=j, qi_=qi_: e.activation(out=junk[:], in_=o32[:], func=AF.Square, accum_out=SSQ[:, j, qi_:qi_ + 1]), reads=[ro32], writes=[rjunk, rSSQ[j]])
                    if stop == 'a6':
                        k.finish()
                        return nc
                    k.op("act", lambda e: e.copy(out=yb[:], in_=o32[:]), reads=[ro32], writes=[ryb])
                    pt, rpt = nextPT()
                    for pr in range(2):
                        k.op("pe", lambda e, pt=pt, pr=pr: e.transpose(out=pt[:, pr * 128:(pr + 1) * 128], in_=yb[:, pr * 128:(pr + 1) * 128], identity=ID[:]),
                             reads=[ryb, rID], writes=[rpt], sig=(pr == 1))
                    for pr in range(2):
                        kc = 2 * qi_ + pr
                        k.op("dve", lambda e, pt=pt, pr=pr, kc=kc, yc=yloc[j]: e.tensor_scalar(out=YT[:, kc, yc * 128:(yc + 1) * 128], in0=pt[:, pr * 128:(pr + 1) * 128], scalar1=GNT[:, kc:kc + 1], scalar2=None, op0=ALU.mult),
                             reads=[rpt, rGNT], writes=[rYT[yloc[j]]])
            if stop == 'attn':
                k.finish()
                return nc
            for hd in range(8):
                wb, rwb = wb_next, rwb_next
                if hd < 7:
                    wb_next, rwb_next = ring.load([(0, 128, w_in[:, 1280 + (hd + 1) * 128:1280 + (hd + 2) * 128]),
                                                   (128, 256, w_in[:, 2304 + (hd + 1) * 128:2304 + (hd + 2) * 128])])
                else:
                    wb_next, rwb_next = ring.load([(0, 256, w_o[:, 0:256])])
                for j in own:
                    c = loc[j]
                    pp, rpp = nextPP()
                    for kc in range(16):
                        k.op("pe", lambda e, pp=pp, kc=kc, c=c, wb=wb: e.matmul(pp[:, 0:256], lhsT=HT[:, kc, c * 128:(c + 1) * 128], rhs=wb[:, kc, :], start=(kc == 0), stop=(kc == 15)),
                             reads=[rHT[c], rwb], writes=[rpp], sig=(kc == 15))
                    z = pp[:, 0:256]
                    k.op("act", lambda e, z=z: e.activation(out=g1[:], in_=z, func=AF.Square), reads=[rpp], writes=[rg1])
                    k.op("dve", lambda e: e.tensor_scalar(out=g1[:], in0=g1[:], scalar1=0.044715, scalar2=1.0, op0=ALU.mult, op1=ALU.add), reads=[rg1], writes=[rg1])
                    k.op("dve", lambda e, z=z: e.tensor_tensor(out=g1[:], in0=g1[:], in1=z, op=ALU.mult), reads=[rg1, rpp], writes=[rg1])
                    k.op("act", lambda e: e.activation(out=g1[:], in_=g1[:], func=AF.Sigmoid, scale=1.5957691216057308), reads=[rg1], writes=[rg1])
                    k.op("dve", lambda e, z=z: e.tensor_tensor(out=g2[:], in0=g1[:], in1=z, op=ALU.mult), reads=[rg1, rpp], writes=[rg2])
                    k.op("dve", lambda e: e.bn_stats(out=vst[:], in_=g2[:, 128:256]), reads=[rg2], writes=[rvst])
                    k.op("dve", lambda e: e.bn_aggr(out=vmv[:], in_=vst[:]), reads=[rvst], writes=[rvmv])
                    k.op("act", lambda e: e.activation(out=vrs[:], in_=vmv[:, 1:2], func=AF.Sqrt, bias=EPS, scale=1.0), reads=[rvmv], writes=[rvrs])
                    k.op("dve", lambda e: e.reciprocal(out=vrs[:], in_=vrs[:]), reads=[rvrs], writes=[rvrs])
                    k.op("dve", lambda e: e.tensor_scalar(out=g3[:, 0:128], in0=g2[:, 128:256], scalar1=vmv[:, 0:1], scalar2=vrs[:, 0:1], op0=ALU.subtract, op1=ALU.mult), reads=[rg2, rvmv, rvrs], writes=[rg3])
                    k.op("pool", lambda e, hd=hd: e.tensor_tensor(out=g3[:, 0:128], in0=g3[:, 0:128], in1=VNG[:, hd * 128:(hd + 1) * 128], op=ALU.mult), reads=[rg3, rVNG], writes=[rg3])
                    k.op("pool", lambda e, hd=hd: e.tensor_tensor(out=vh[:], in0=g3[:, 0:128], in1=VNB[:, hd * 128:(hd + 1) * 128], op=ALU.add), reads=[rg3, rVNB], writes=[rvh])
                    k.op("pe", lambda e, hd=hd: e.matmul(PV[:, 256:384], lhsT=WST[:, hd, :], rhs=vh[:], start=True, stop=True), reads=[rWST, rvh], writes=[rPV])
                    k.op("dve", lambda e, hd=hd: e.scalar_tensor_tensor(out=o32[:, 0:128], in0=PV[:, 256:384], scalar=BST[:, hd:hd + 1], in1=g2[:, 0:128], op0=ALU.add, op1=ALU.mult), reads=[rPV, rBST, rg2], writes=[ro32])
                    k.op("act", lambda e, j=j, hd=hd: e.activation(out=junk[:, 0:128], in_=o32[:, 0:128], func=AF.Square, accum_out=SSQ[:, j, 4 + hd:5 + hd]),
                         reads=[ro32], writes=[rjunk, rSSQ[j]])
                    k.op("act", lambda e: e.copy(out=yb[:, 0:128], in_=o32[:, 0:128]), reads=[ro32], writes=[ryb])
                    pt, rpt = nextPT()
                    k.op("pe", lambda e, pt=pt: e.transpose(out=pt[:, 0:128], in_=yb[:, 0:128], identity=ID[:]), reads=[ryb, rID], writes=[rpt])
                    k.op("dve", lambda e, pt=pt, hd=hd, yc=yloc[j]: e.tensor_scalar(out=YT[:, 8 + hd, yc * 128:(yc + 1) * 128], in0=pt[:, 0:128], scalar1=GNT[:, 8 + hd:9 + hd], scalar2=None, op0=ALU.mult),
                         reads=[rpt, rGNT], writes=[rYT[yloc[j]]])
            if stop == 'gmlp':
                k.finish()
                return nc
            for j in own:
                k.op("dve", lambda e, j=j: e.tensor_reduce(out=rsa[:, 0:1], in_=SSQ[:, j, 0:4], axis=AX.X, op=ALU.add), reads=[rSSQ[j]], writes=[rrsa])
                k.op("dve", lambda e, j=j: e.tensor_reduce(out=rsa[:, 1:2], in_=SSQ[:, j, 4:12], axis=AX.X, op=ALU.add), reads=[rSSQ[j]], writes=[rrsa])
                k.op("act", lambda e: e.activation(out=rsa[:], in_=rsa[:], func=AF.Sqrt, bias=EPS, scale=1.0 / 1024.0), reads=[rrsa], writes=[rrsa])
                k.op("dve", lambda e, j=j: e.reciprocal(out=SSQ[:, j, 0:2], in_=rsa[:]), reads=[rrsa], writes=[rSSQ[j]])
            for oi in range(8):
                wb, rwb = wb_next, rwb_next
                if oi < 7:
                    wb_next, rwb_next = ring.load([(0, 256, w_o[:, (oi + 1) * 256:(oi + 2) * 256])])
                for j in own:
                    yc = yloc[j]
                    pp, rpp = nextPP()
                    for half in range(2):
                        for i in range(8):
                            kc = half * 8 + i
                            k.op("pe", lambda e, pp=pp, kc=kc, yc=yc, wb=wb, half=half, i=i: e.matmul(pp[:, half * 256:(half + 1) * 256], lhsT=YT[:, kc, yc * 128:(yc + 1) * 128], rhs=wb[:, kc, :], start=(i == 0), stop=(i == 7)),
                                 reads=[rYT[yc], rwb], writes=[rpp], sig=(kc == 15))
                    hs = H[:, j, oi * 256:(oi + 1) * 256]
                    k.op("dve", lambda e, pp=pp, hs=hs, j=j: e.scalar_tensor_tensor(out=hs, in0=pp[:, 0:256], scalar=SSQ[:, j, 0:1], in1=hs, op0=ALU.mult, op1=ALU.add), reads=[rpp, rSSQ[j], rH[j]], writes=[rH[j]])
                    k.op("dve", lambda e, pp=pp, hs=hs, j=j: e.scalar_tensor_tensor(out=hs, in0=pp[:, 256:512], scalar=SSQ[:, j, 1:2], in1=hs, op0=ALU.mult, op1=ALU.add), reads=[rpp, rSSQ[j], rH[j]], writes=[rH[j]])
            if stop == 'wo':
                k.finish()
                return nc
            for j in own:
                emit_layernorm(k, H[:, j, :], rH[j], LG, rLG, LB, rLB, lt)
                k.dma("sp", h1_out[(j - 1) * 128:j * 128, :], H[:, j, :], reads=[rH[j]], key="oh", is_out=True)
                k.op("act", lambda e, j=j: e.copy(out=hb[:], in_=H[:, j, :]), reads=[rH[j]], writes=[rhb])
                k.op("dve", lambda e, j=j: e.tensor_tensor(out=hlo[:], in0=H[:, j, :], in1=hb[:], op=ALU.subtract), reads=[rH[j], rhb], writes=[rhlo])
                k.dma("sp", hbt_out[(j - 1) * 128:j * 128, :], hb[:], reads=[rhb], key="ob", is_out=True)
                for src, rsrc, dstT, rdstT in ((hb, rhb, hbT, rhbT), (hlo, rhlo, hloT, rhloT)):
                    for half in range(2):
                        pt, rpt = nextPT()
                        for i in range(8):
                            kc = half * 8 + i
                            k.op("pe", lambda e, pt=pt, kc=kc, i=i, src=src: e.transpose(out=pt[:, i * 128:(i + 1) * 128], in_=src[:, kc * 128:(kc + 1) * 128], identity=ID[:]),
                                 reads=[rsrc, rID], writes=[rpt], sig=(i == 7))
                        k.op("dve", lambda e, pt=pt, half=half, dstT=dstT: e.tensor_copy(out=dstT[:, half * 8:(half + 1) * 8, :], in_=pt[:].rearrange("p (a b) -> p a b", a=8)),
                             reads=[rpt], writes=[rdstT])
                k.dma("sp", hT_out[:, :, (j - 1) * 128:j * 128].rearrange("kc p t -> p kc t"), hbT[:], reads=[rhbT], key="ot", is_out=True)
                n = 0
                for (aT, raT, wq, rwq) in ((hbT, rhbT, WRH, rWRH), (hloT, rhloT, WRH, rWRH), (hbT, rhbT, WRL, rWRL)):
                    for kc in range(16):
                        k.op("pe", lambda e, kc=kc, aT=aT, wq=wq, n=n: e.matmul(PR[:, 0:NE], lhsT=aT[:, kc, :], rhs=wq[:, kc, :], start=(n == 0), stop=(n == 47)),
                             reads=[raT, rwq], writes=[rPR], sig=(n == 47))
                        n += 1
                k.op("dve", lambda e: e.tensor_tensor(out=lg[:], in0=PR[:, 0:NE], in1=BR[:], op=ALU.add), reads=[rPR, rBR], writes=[rlg])
                k.op("dve", lambda e: e.max(out=m8[:], in_=lg[:]), reads=[rlg], writes=[rm8])
                k.op("dve", lambda e: e.tensor_scalar(out=msk[:], in0=lg[:], scalar1=m8[:, 3:4], scalar2=None, op0=ALU.is_ge), reads=[rlg, rm8], writes=[rmsk])
                k.op("dve", lambda e: e.tensor_scalar(out=nm[:], in0=m8[:, 0:1], scalar1=-1.0, scalar2=None, op0=ALU.mult), reads=[rm8], writes=[rnm])
                k.op("act", lambda e: e.activation(out=lg[:], in_=lg[:], func=AF.Exp, bias=nm[:, 0:1], scale=1.0), reads=[rlg, rnm], writes=[rlg])
                k.op("dve", lambda e: e.tensor_tensor(out=lg[:], in0=lg[:], in1=msk[:], op=ALU.mult), reads=[rlg, rmsk], writes=[rlg])
                k.op("dve", lambda e: e.tensor_reduce(out=gs[:], in_=lg[:], axis=AX.X, op=ALU.add), reads=[rlg], writes=[rgs])
                k.op("dve", lambda e: e.reciprocal(out=gs[:], in_=gs[:]), reads=[rgs], writes=[rgs])
                k.op("dve", lambda e: e.tensor_scalar(out=lg[:], in0=lg[:], scalar1=gs[:, 0:1], scalar2=None, op0=ALU.mult), reads=[rlg, rgs], writes=[rlg])
                k.dma("sp", g_out[(j - 1) * 128:j * 128, :], lg[:], reads=[rlg], key="og", is_out=True)
        k.finish()
    return nc


TG = 1024


def build_moe():
    nc = bass.Bass("TRN2", target_bir_lowering=False)
    dt_in = lambda name, shape, dt=F32: nc.dram_tensor(name, list(shape), dt, kind="ExternalInput").ap()
    xT = dt_in("xT", [128, 16, NTOK], BF16)
    gT = dt_in("gT", [EPC, NTOK])
    w_gu = dt_in("w_gu", [EPC, D, 2 * D])
    bguT = dt_in("bguT", [128, EPC, 32])
    w_dn = dt_in("w_down", [EPC, D, D])
    b_dn = dt_in("b_down", [EPC, D])
    y_out = nc.dram_tensor("ypart", [NTOK, D], F32, kind="ExternalOutput").ap()
    NG = NTOK // TG
    with ExitStack() as es:
        k = KB(nc, es)
        XT = k.sb("XT", [128, 16, TG], BF16); rXT = Res()
        GB = k.sb("GB", [128, EPC, TG], F32); rGB = Res()
        G4 = k.sb("G4", [EPC, TG], F32); rG4 = Res()
        G4b = k.sb("G4b", [EPC, TG], BF16); rG4b = Res()
        BD = k.sb("BD", [EPC, D], F32); rBD = Res()
        BDb = k.sb("BDb", [EPC, D], BF16); rBDb = Res()
        BGU = k.sb("BGU", [128, EPC, 32], F32); rBGU = Res()
        YACC = k.sb("YACC", [128, TG // 128, D], F32); rY = [Res() for _ in range(TG // 128)]
        AT = k.sb("AT", [128, 16, TG], BF16); rAT = [Res() for _ in range(16)]
        ring = WRing(k, "wm", 4)
        T1 = [k.sb("T1_%d" % i, [128, 512], F32) for i in range(2)]; rT1 = [Res(), Res()]
        T2 = [k.sb("T2_%d" % i, [128, 512], F32) for i in range(2)]; rT2 = [Res(), Res()]
        T3 = [k.sb("T3_%d" % i, [128, 512], F32) for i in range(2)]; rT3 = [Res(), Res()]
        PA = [k.ps("PA%d" % i, [128, 512], F32) for i in range(2)]; rPA = [Res(), Res()]
        PB = [k.ps("PB%d" % i, [128, 512], F32) for i in range(2)]; rPB = [Res(), Res()]
        PD = [k.ps("PD%d" % i, [128, 512], F32) for i in range(2)]; rPD = [Res(), Res()]
        k.dma("sp", BGU[:], bguT, writes=[rBGU], key="c0")
        k.dma("sp", BD[:], b_dn, writes=[rBD], key="c1")
        k.op("dve", lambda e: e.tensor_copy(out=BDb[:], in_=BD[:]), reads=[rBD], writes=[rBDb])
        pieces = []
        for tg in range(NG):
            for ex in range(EPC):
                for fc in range(16):
                    pieces.append([(0, 128, w_gu[ex][:, fc * 128:(fc + 1) * 128]), (128, 256, w_gu[ex][:, D + fc * 128:D + (fc + 1) * 128])])
                for dp in range(8):
                    pieces.append([(0, 256, w_dn[ex][:, dp * 256:(dp + 1) * 256])])
        LA = 2
        loaded = []
        pi = [0]

        def get_piece():
            while len(loaded) < min(len(pieces), pi[0] + 1 + LA):
                loaded.append(ring.load(pieces[len(loaded)]))
            r = loaded[pi[0]]
            pi[0] += 1
            return r

        cnt = 0
        dcnt = 0
        for tg in range(NG):
            t0 = tg * TG
            k.dma("sp", XT[:], xT[:, :, t0:t0 + TG], writes=[rXT], key="xt")
            for ex in range(EPC):
                k.dma("sp", GB[:, ex, :], gT[ex, t0:t0 + TG].partition_broadcast(128), writes=[rGB], key="gb")
            k.dma("sp", G4[:], gT[:, t0:t0 + TG], writes=[rG4], key="g4")
            k.op("dve", lambda e: e.tensor_copy(out=G4b[:], in_=G4[:]), reads=[rG4], writes=[rG4b])
            for ex in range(EPC):
                for fc in range(16):
                    wb, rwb = get_piece()
                    for ns in range(2):
                        b = cnt % 2
                        cnt += 1
                        for kc in range(16):
                            k.op("pe", lambda e, b=b, kc=kc, wb=wb, ns=ns: e.matmul(PA[b][:], lhsT=wb[:, kc, 0:128], rhs=XT[:, kc, ns * 512:(ns + 1) * 512], start=(kc == 0), stop=(kc == 15)),
                                 reads=[rwb, rXT], writes=[rPA[b]], sig=(kc == 15))
                        for kc in range(16):
                            k.op("pe", lambda e, b=b, kc=kc, wb=wb, ns=ns: e.matmul(PB[b][:], lhsT=wb[:, kc, 128:256], rhs=XT[:, kc, ns * 512:(ns + 1) * 512], start=(kc == 0), stop=(kc == 15)),
                                 reads=[rwb, rXT], writes=[rPB[b]], sig=(kc == 15))
                        k.op("dve", lambda e, b=b, ex=ex, fc=fc: e.tensor_scalar(out=T1[b][:], in0=PA[b][:], scalar1=BGU[:, ex, fc:fc + 1], scalar2=7.0, op0=ALU.add, op1=ALU.min),
                             reads=[rPA[b], rBGU], writes=[rT1[b]])
                        k.op("act", lambda e, b=b: e.activation(out=T2[b][:], in_=T1[b][:], func=AF.Sigmoid, scale=1.702), reads=[rT1[b]], writes=[rT2[b]])
                        k.op("dve", lambda e, b=b, ex=ex, fc=fc: e.tensor_scalar(out=T3[b][:], in0=PB[b][:], scalar1=BGU[:, ex, 16 + fc:17 + fc], scalar2=7.0, op0=ALU.add, op1=ALU.min),
                             reads=[rPB[b], rBGU], writes=[rT3[b]])
                        k.op("dve", lambda e, b=b: e.tensor_scalar(out=T3[b][:], in0=T3[b][:], scalar1=-7.0, scalar2=1.0, op0=ALU.max, op1=ALU.add), reads=[rT3[b]], writes=[rT3[b]])
                        k.op("pool", lambda e, b=b: e.tensor_tensor(out=T1[b][:], in0=T1[b][:], in1=T2[b][:], op=ALU.mult), reads=[rT1[b], rT2[b]], writes=[rT1[b]])
                        k.op("pool", lambda e, b=b: e.tensor_tensor(out=T1[b][:], in0=T1[b][:], in1=T3[b][:], op=ALU.mult), reads=[rT1[b], rT3[b]], writes=[rT1[b]])
                        k.op("pool", lambda e, b=b, ex=ex, fc=fc, ns=ns: e.tensor_tensor(out=AT[:, fc, ns * 512:(ns + 1) * 512], in0=T1[b][:], in1=GB[:, ex, ns * 512:(ns + 1) * 512], op=ALU.mult),
                             reads=[rT1[b], rGB], writes=[rAT[fc]])
                for dp in range(8):
                    wb, rwb = get_piece()
                    for tc in range(TG // 128):
                        b = dcnt % 2
                        dcnt += 1
                        if ex == 0:
                            k.op("pe", lambda e, b=b, tc=tc, dp=dp: e.matmul(PD[b][:, 0:256], lhsT=G4b[:, tc * 128:(tc + 1) * 128], rhs=BDb[:, dp * 256:(dp + 1) * 256], start=True, stop=False),
                                 reads=[rG4b, rBDb], writes=[rPD[b]], sig=False)
                        for fc in range(16):
                            k.op("pe", lambda e, b=b, tc=tc, fc=fc, wb=wb, ex=ex: e.matmul(PD[b][:, 0:256], lhsT=AT[:, fc, tc * 128:(tc + 1) * 128], rhs=wb[:, fc, :], start=(fc == 0 and ex != 0), stop=(fc == 15)),
                                 reads=[rAT[fc], rwb], writes=[rPD[b]], sig=(fc == 15))
                        ys = YACC[:, tc, dp * 256:(dp + 1) * 256]
                        if ex == 0:
                            k.op("act", lambda e, b=b, ys=ys: e.copy(out=ys, in_=PD[b][:, 0:256]), reads=[rPD[b]], writes=[rY[tc]])
                        else:
                            k.op("dve", lambda e, b=b, ys=ys: e.tensor_tensor(out=ys, in0=ys, in1=PD[b][:, 0:256], op=ALU.add), reads=[rPD[b], rY[tc]], writes=[rY[tc]])
            for tc in range(TG // 128):
                k.dma("sp", y_out[t0 + tc * 128:t0 + (tc + 1) * 128, :], YACC[:, tc, :], reads=[rY[tc]], key="oy", is_out=True)
        k.finish()
    return nc


CAP = 256
NBLK = NTOK // 1024
NSLOT = NBLK * CAP
HSLOT = NSLOT // 2


def build_moe2():
    nc = bass.Bass("TRN2", target_bir_lowering=False)
    dt_in = lambda name, shape, dt=F32: nc.dram_tensor(name, list(shape), dt, kind="ExternalInput").ap()
    hbt = dt_in("hbt", [NTOK, D], BF16)
    gC = dt_in("gC", [128, NTOK // 128, EPC])
    iotad = dt_in("iotad", [128, CAP])
    sud = dt_in("sud", [128, 128], BF16)
    onesd = dt_in("onesd", [128, 128], BF16)
    identb = dt_in("identb", [128, 128], BF16)
    w_gu = dt_in("w_gu", [EPC, D, 2 * D])
    bguT = dt_in("bguT", [128, EPC, 32])
    w_dn = dt_in("w_down", [EPC, D, D])
    b_dn = dt_in("b_down", [EPC, D])
    y_out = nc.dram_tensor("ypart", [NTOK, D], F32, kind="ExternalOutput").ap()
    ysp = nc.dram_tensor("ysp", [EPC, NSLOT, D], BF16, kind="Internal").ap()
    NTC = NTOK // 128
    with ExitStack() as es:
        k = KB(nc, es)
        IOTA = k.sb("IOTA", [128, CAP], F32); rIOTA = Res()
        SU = k.sb("SU", [128, 128], BF16); rSU = Res()
        ONES = k.sb("ONES", [128, 128], BF16); rONES = Res()
        ID = k.sb("ID", [128, 128], BF16); rID = Res()
        GC = k.sb("GC", [128, NTC, EPC], F32); rGC = Res()
        MKb = k.sb("MKb", [128, NTC * EPC], BF16); rMKb = Res()
        MKf = k.sb("MKf", [128, NTC, EPC], F32); rMKf = Res()
        W1 = k.sb("W1", [128, NTC, EPC], F32); rW1 = Res()
        CNT = k.sb("CNT", [128, NTC, EPC], F32); rCNT = Res()
        PRE = k.sb("PRE", [128, NTC, EPC], F32); rPRE = Res()
        POSM = k.sb("POSM", [128, NTC, EPC], F32); rPOSM = Res()
        BGU = k.sb("BGU", [128, EPC, 32], F32); rBGU = Res()
        BDb1 = k.sb("BDb1", [1, D], BF16); rBDb1 = Res()
        XT = k.sb("XT", [128, 16, HSLOT], BF16); rXT = Res()
        AT = k.sb("AT", [128, 16, HSLOT], BF16); rAT = [Res() for _ in range(16)]
        HBs = [k.sb("HB%d" % i, [128, 8, D], BF16) for i in range(2)]; rHBs = [Res(), Res()]
        S = [k.sb("S%d" % i, [128, 8, CAP], BF16) for i in range(2)]; rS = [Res(), Res()]
        YST = [k.sb("YST%d" % i, [128, 256], BF16) for i in range(4)]; rYST = [Res() for _ in range(4)]
        OUT = [k.sb("OUT%d" % i, [128, D], F32) for i in range(2)]; rOUT = [Res(), Res()]
        ring = WRing(k, "wm", 3)
        T1 = [k.sb("T1_%d" % i, [128, 512], F32) for i in range(2)]; rT1 = [Res(), Res()]
        T2 = [k.sb("T2_%d" % i, [128, 512], F32) for i in range(2)]; rT2 = [Res(), Res()]
        T3 = [k.sb("T3_%d" % i, [128, 512], F32) for i in range(2)]; rT3 = [Res(), Res()]
        PA = [k.ps("PA%d" % i, [128, 512], F32) for i in range(2)]; rPA = [Res(), Res()]
        PB = [k.ps("PB%d" % i, [128, 512], F32) for i in range(2)]; rPB = [Res(), Res()]
        PD = [k.ps("PD%d" % i, [128, 512], F32) for i in range(2)]; rPD = [Res(), Res()]
        PT = [k.ps("PT%d" % i, [128, 1024], BF16) for i in range(2)]; rPT = [Res(), Res()]
        ld = lambda dst, src, r, key: k.dma("sp", dst, src, writes=[r], key=key)
        ld(IOTA[:], iotad, rIOTA, "c0"); ld(SU[:], sud, rSU, "c1"); ld(ONES[:], onesd, rONES, "c2"); ld(ID[:], identb, rID, "c3")
        ld(GC[:], gC, rGC, "c4"); ld(BGU[:], bguT, rBGU, "c5")
        gcf = GC[:].rearrange("p a b -> p (a b)")
        k.op("dve", lambda e: e.tensor_scalar(out=MKb[:], in0=gcf, scalar1=0.0, scalar2=None, op0=ALU.is_gt), reads=[rGC], writes=[rMKb])
        k.op("dve", lambda e: e.tensor_scalar(out=MKf[:].rearrange("p a b -> p (a b)"), in0=gcf, scalar1=0.0, scalar2=None, op0=ALU.is_gt), reads=[rGC], writes=[rMKf])
        k.op("pe", lambda e: e.matmul(PA[0][:, 0:NTC * EPC], lhsT=SU[:], rhs=MKb[:], start=True, stop=True), reads=[rSU, rMKb], writes=[rPA[0]])
        k.op("pe", lambda e: e.matmul(PA[1][:, 0:NTC * EPC], lhsT=ONES[:], rhs=MKb[:], start=True, stop=True), reads=[rONES, rMKb], writes=[rPA[1]])
        k.op("dve", lambda e: e.tensor_copy(out=W1[:].rearrange("p a b -> p (a b)"), in_=PA[0][:, 0:NTC * EPC]), reads=[rPA[0]], writes=[rW1])
        k.op("dve", lambda e: e.tensor_copy(out=CNT[:].rearrange("p a b -> p (a b)"), in_=PA[1][:, 0:NTC * EPC]), reads=[rPA[1]], writes=[rCNT])
        pre4 = PRE[:].rearrange("p (j t) e -> p j t e", t=8)
        cnt4 = CNT[:].rearrange("p (j t) e -> p j t e", t=8)
        k.op("dve", lambda e: e.memset(PRE[:], 0.0), writes=[rPRE])
        for i in range(1, 8):
            k.op("dve", lambda e, i=i: e.tensor_tensor(out=pre4[:, :, i, :], in0=pre4[:, :, i - 1, :], in1=cnt4[:, :, i - 1, :], op=ALU.add), reads=[rPRE, rCNT], writes=[rPRE])
        k.op("dve", lambda e: e.tensor_tensor(out=POSM[:], in0=W1[:], in1=PRE[:], op=ALU.add), reads=[rW1, rPRE], writes=[rPOSM])
        k.op("dve", lambda e: e.scalar_tensor_tensor(out=POSM[:], in0=POSM[:], scalar=1.0, in1=MKf[:], op0=ALU.add, op1=ALU.mult), reads=[rPOSM, rMKf], writes=[rPOSM])
        k.op("dve", lambda e: e.tensor_scalar(out=POSM[:], in0=POSM[:], scalar1=-1.0, scalar2=None, op0=ALU.add), reads=[rPOSM], writes=[rPOSM])
        pieces = []
        for ex, _half in [(a, b) for a in range(EPC) for b in range(2)]:
            for fc in range(16):
                pieces.append([(0, 128, w_gu[ex][:, fc * 128:(fc + 1) * 128]), (128, 256, w_gu[ex][:, D + fc * 128:D + (fc + 1) * 128])])
            for dp in range(8):
                pieces.append([(0, 256, w_dn[ex][:, dp * 256:(dp + 1) * 256])])
        LA = 2
        loaded = []
        pi = [0]

        def get_piece():
            while len(loaded) < min(len(pieces), pi[0] + 1 + LA):
                loaded.append(ring.load(pieces[len(loaded)]))
            r = loaded[pi[0]]
            pi[0] += 1
            return r

        cnt = 0
        dcnt = 0
        scnt = 0
        ycnt = 0
        NS = HSLOT // 512
        for ex, half in [(a, b) for a in range(EPC) for b in range(2)]:
            for j in range(half * NBLK // 2, (half + 1) * NBLK // 2):
                jl = j - half * NBLK // 2
                HB, rHB = HBs[scnt % 2], rHBs[scnt % 2]
                k.dma("sp", HB[:], hbt[j * 1024:(j + 1) * 1024, :].rearrange("(tc p) d -> p tc d", p=128), writes=[rHB], key="hb%d" % (scnt % 2))
                sb_ = scnt % 2
                scnt += 1
                for tc in range(8):
                    k.op("dve", lambda e, sb_=sb_, tc=tc, j=j, ex=ex: e.tensor_scalar(out=S[sb_][:, tc, :], in0=IOTA[:], scalar1=POSM[:, j * 8 + tc, ex:ex + 1], scalar2=None, op0=ALU.is_equal),
                         reads=[rIOTA, rPOSM], writes=[rS[sb_]])
                for dcp in range(8):
                    b = dcnt % 2
                    dcnt += 1
                    for dci in range(2):
                        dc = dcp * 2 + dci
                        for tc in range(8):
                            k.op("pe", lambda e, b=b, dci=dci, dc=dc, tc=tc, sb_=sb_, HB=HB: e.matmul(PD[b][:, dci * CAP:(dci + 1) * CAP], lhsT=HB[:, tc, dc * 128:(dc + 1) * 128], rhs=S[sb_][:, tc, :], start=(tc == 0), stop=(tc == 7)),
                                 reads=[rHB, rS[sb_]], writes=[rPD[b]], sig=(tc == 7 and dci == 1))
                    eng = "act" if dcp % 2 == 0 else "dve"
                    if eng == "act":
                        k.op("act", lambda e, b=b, dcp=dcp, jl=jl: e.copy(out=XT[:, 2 * dcp:2 * dcp + 2, jl * CAP:(jl + 1) * CAP], in_=PD[b][:, 0:2 * CAP].rearrange("p (a s) -> p a s", a=2)), reads=[rPD[b]], writes=[rXT])
                    else:
                        k.op("dve", lambda e, b=b, dcp=dcp, jl=jl: e.tensor_copy(out=XT[:, 2 * dcp:2 * dcp + 2, jl * CAP:(jl + 1) * CAP], in_=PD[b][:, 0:2 * CAP].rearrange("p (a s) -> p a s", a=2)), reads=[rPD[b]], writes=[rXT])
            k.dma("pool", BDb1[:], b_dn[ex:ex + 1, :], writes=[rBDb1], key="bd")
            for fc in range(16):
                wb, rwb = get_piece()
                for ns in range(NS):
                    b = cnt % 2
                    cnt += 1
                    for kc in range(16):
                        k.op("pe", lambda e, b=b, kc=kc, wb=wb, ns=ns: e.matmul(PA[b][:], lhsT=wb[:, kc, 0:128], rhs=XT[:, kc, ns * 512:(ns + 1) * 512], start=(kc == 0), stop=(kc == 15)),
                             reads=[rwb, rXT], writes=[rPA[b]], sig=(kc == 15))
                    for kc in range(16):
                        k.op("pe", lambda e, b=b, kc=kc, wb=wb, ns=ns: e.matmul(PB[b][:], lhsT=wb[:, kc, 128:256], rhs=XT[:, kc, ns * 512:(ns + 1) * 512], start=(kc == 0), stop=(kc == 15)),
                             reads=[rwb, rXT], writes=[rPB[b]], sig=(kc == 15))
                    k.op("dve", lambda e, b=b, ex=ex, fc=fc: e.tensor_scalar(out=T1[b][:], in0=PA[b][:], scalar1=BGU[:, ex, fc:fc + 1], scalar2=7.0, op0=ALU.add, op1=ALU.min),
                         reads=[rPA[b], rBGU], writes=[rT1[b]])
                    k.op("act", lambda e, b=b: e.activation(out=T2[b][:], in_=T1[b][:], func=AF.Sigmoid, scale=1.702), reads=[rT1[b]], writes=[rT2[b]])
                    k.op("dve", lambda e, b=b, ex=ex, fc=fc: e.tensor_scalar(out=T3[b][:], in0=PB[b][:], scalar1=BGU[:, ex, 16 + fc:17 + fc], scalar2=7.0, op0=ALU.add, op1=ALU.min),
                         reads=[rPB[b], rBGU], writes=[rT3[b]])
                    k.op("dve", lambda e, b=b: e.tensor_scalar(out=T3[b][:], in0=T3[b][:], scalar1=-7.0, scalar2=1.0, op0=ALU.max, op1=ALU.add), reads=[rT3[b]], writes=[rT3[b]])
                    k.op("pool", lambda e, b=b: e.tensor_tensor(out=T1[b][:], in0=T1[b][:], in1=T2[b][:], op=ALU.mult), reads=[rT1[b], rT2[b]], writes=[rT1[b]])
                    k.op("pool", lambda e, b=b, fc=fc, ns=ns: e.tensor_tensor(out=AT[:, fc, ns * 512:(ns + 1) * 512], in0=T1[b][:], in1=T3[b][:], op=ALU.mult),
                         reads=[rT1[b], rT3[b]], writes=[rAT[fc]])
            for dp in range(8):
                wb, rwb = get_piece()
                for sc in range(HSLOT // 128):
                    b = dcnt % 2
                    dcnt += 1
                    k.op("pe", lambda e, b=b, dp=dp, ex=ex: e.matmul(PD[b][:, 0:256], lhsT=ONES[0:1, :], rhs=BDb1[:, dp * 256:(dp + 1) * 256], start=True, stop=False),
                         reads=[rONES, rBDb1], writes=[rPD[b]], sig=False)
                    for fc in range(16):
                        k.op("pe", lambda e, b=b, sc=sc, fc=fc, wb=wb: e.matmul(PD[b][:, 0:256], lhsT=AT[:, fc, sc * 128:(sc + 1) * 128], rhs=wb[:, fc, :], start=False, stop=(fc == 15)),
                             reads=[rAT[fc], rwb], writes=[rPD[b]], sig=(fc == 15))
                    yb_ = ycnt % 4
                    ycnt += 1
                    if yb_ % 2 == 0:
                        k.op("act", lambda e, b=b, yb_=yb_: e.copy(out=YST[yb_][:], in_=PD[b][:, 0:256]), reads=[rPD[b]], writes=[rYST[yb_]])
                    else:
                        k.op("dve", lambda e, b=b, yb_=yb_: e.tensor_copy(out=YST[yb_][:], in_=PD[b][:, 0:256]), reads=[rPD[b]], writes=[rYST[yb_]])
                    k.dma("sp", ysp[ex, half * HSLOT + sc * 128:half * HSLOT + (sc + 1) * 128, dp * 256:(dp + 1) * 256], YST[yb_][:], reads=[rYST[yb_]], key="ys%d" % yb_)
        xtf = XT[:].rearrange("p a b -> p (a b)")
        atf = AT[:].rearrange("p a b -> p (a b)")
        hbf = HBs[0][:].rearrange("p a b -> p (a b)")
        Ybuf = []
        for src in (xtf, hbf):
            Ybuf.append((src[:, 0:EPC * D].rearrange("p (e d) -> p e d", e=EPC), src[:, EPC * D:2 * EPC * D].rearrange("p (e d) -> p e d", e=EPC)))
        SGbuf = []
        for o in (0, 2 * EPC * 1024):
            SGbuf.append((atf[:, o:o + EPC * 1024].rearrange("p (e t) -> p e t", e=EPC), atf[:, o + EPC * 1024:o + 2 * EPC * 1024].rearrange("p (e t) -> p e t", e=EPC)))
        rYb = [Res(), Res()]; rSGb = [Res(), Res()]
        banks = [(PA[0], rPA[0]), (PA[1], rPA[1]), (PB[0], rPB[0]), (PB[1], rPB[1]), (PD[0], rPD[0]), (PD[1], rPD[1])]
        bcnt = 0
        ocnt = 0
        for j in range(NBLK):
            par = j % 2
            Y1, Y2 = Ybuf[par]
            SG1, SG2 = SGbuf[par]
            rY12, rSG = rYb[par], rSGb[par]
            for ex in range(EPC):
                if j < 2 and ex == 0:
                    wr = [rY12] + rYST + ([rXT] if par == 0 else rHBs)
                else:
                    wr = [rY12]
                k.dma("sp", Y1[:, ex, :], ysp[ex, j * CAP:j * CAP + 128, :], writes=wr, key="y1%d" % par)
                k.dma("sp", Y2[:, ex, :], ysp[ex, j * CAP + 128:(j + 1) * CAP, :], writes=[rY12], key="y1%d" % par)
            for ex in range(EPC):
                sb_ = scnt % 2
                scnt += 1
                for tc in range(8):
                    k.op("dve", lambda e, sb_=sb_, tc=tc, j=j, ex=ex: e.tensor_scalar(out=S[sb_][:, tc, :], in0=IOTA[:], scalar1=POSM[:, j * 8 + tc, ex:ex + 1], scalar2=GC[:, j * 8 + tc, ex:ex + 1], op0=ALU.is_equal, op1=ALU.mult),
                         reads=[rIOTA, rPOSM, rGC], writes=[rS[sb_]])
                for tc in range(8):
                    k.op("pe", lambda e, sb_=sb_, tc=tc: e.transpose(out=PT[0][:, tc * 128:(tc + 1) * 128], in_=S[sb_][:, tc, 0:128], identity=ID[:]), reads=[rS[sb_], rID], writes=[rPT[0]], sig=(tc == 7))
                for tc in range(8):
                    k.op("pe", lambda e, sb_=sb_, tc=tc: e.transpose(out=PT[1][:, tc * 128:(tc + 1) * 128], in_=S[sb_][:, tc, 128:CAP], identity=ID[:]), reads=[rS[sb_], rID], writes=[rPT[1]], sig=(tc == 7))
                k.op("act", lambda e, ex=ex, SG1=SG1: e.copy(out=SG1[:, ex, :], in_=PT[0][:]), reads=[rPT[0]], writes=[rSG] + (rAT if j < 2 else []))
                k.op("dve", lambda e, ex=ex, SG2=SG2: e.tensor_copy(out=SG2[:, ex, :], in_=PT[1][:]), reads=[rPT[1]], writes=[rSG])
            for tc in range(8):
                ob = ocnt % 2
                ocnt += 1
                for dg in range(4):
                    pb_, rpb_ = banks[bcnt % 6]
                    bcnt += 1
                    n = 0
                    for ex in range(EPC):
                        k.op("pe", lambda e, pb_=pb_, ex=ex, tc=tc, dg=dg, n=n, SG1=SG1, Y1=Y1: e.matmul(pb_[:], lhsT=SG1[:, ex, tc * 128:(tc + 1) * 128], rhs=Y1[:, ex, dg * 512:(dg + 1) * 512], start=(n == 0), stop=False),
                             reads=[rSG, rY12], writes=[rpb_], sig=False)
                        n += 1
                        k.op("pe", lambda e, pb_=pb_, ex=ex, tc=tc, dg=dg, n=n, SG2=SG2, Y2=Y2: e.matmul(pb_[:], lhsT=SG2[:, ex, tc * 128:(tc + 1) * 128], rhs=Y2[:, ex, dg * 512:(dg + 1) * 512], start=False, stop=(n == 2 * EPC - 1)),
                             reads=[rSG, rY12], writes=[rpb_], sig=(n == 2 * EPC - 1))
                        n += 1
                    if dg % 2 == 0:
                        k.op("act", lambda e, pb_=pb_, ob=ob, dg=dg: e.copy(out=OUT[ob][:, dg * 512:(dg + 1) * 512], in_=pb_[:]), reads=[rpb_], writes=[rOUT[ob]])
                    else:
                        k.op("dve", lambda e, pb_=pb_, ob=ob, dg=dg: e.tensor_copy(out=OUT[ob][:, dg * 512:(dg + 1) * 512], in_=pb_[:]), reads=[rpb_], writes=[rOUT[ob]])
                k.dma("sp", y_out[j * 1024 + tc * 128:j * 1024 + (tc + 1) * 128, :], OUT[ob][:], reads=[rOUT[ob]], key="oy%d" % ob, is_out=True)
        k.finish()
    return nc


def build_post():
    nc = bass.Bass("TRN2", target_bir_lowering=False)
    dt_in = lambda name, shape, dt=F32: nc.dram_tensor(name, list(shape), dt, kind="ExternalInput").ap()
    h1 = dt_in("h1", [TPC, D])
    parts = dt_in("parts", [NCORES, TPC, D])
    pin = dt_in("p", [TPC, PLE])
    identb = dt_in("identb", [128, 128], BF16)
    ln2g = dt_in("ln2_g", [D]); ln2b = dt_in("ln2_b", [D])
    ln3g = dt_in("ln3_g", [D]); ln3b = dt_in("ln3_b", [D])
    wg = dt_in("w_ple_gate", [D, D])
    bg = dt_in("b_ple_gate", [D])
    wp = dt_in("w_ple_proj", [PLE, D])
    h3 = nc.dram_tensor("h3", [TPC, D], F32, kind="ExternalOutput").ap()
    NJ = TPC // 128
    with ExitStack() as es:
        k = KB(nc, es)
        ID = k.sb("ID", [128, 128], BF16); rID = Res()
        LG = k.sb("LG", [128, D], F32); rLG = Res()
        LB = k.sb("LB", [128, D], F32); rLB = Res()
        BG = k.sb("BG", [128, D], F32); rBG = Res()
        WP = k.sb("WP", [128, 2, D], BF16); rWP = Res()
        H = k.sb("H", [128, NJ, D], F32); rH = [Res() for _ in range(NJ)]
        PBUF = [k.sb("PBUF%d" % i, [128, D], F32) for i in range(3)]; rPBUF = [Res() for _ in range(3)]
        HT = k.sb("HT", [128, 16, 512], BF16); rHT = [Res() for _ in range(4)]
        PTT = k.sb("PTT", [128, 2, 512], BF16); rPTT = [Res() for _ in range(4)]
        hb = k.sb("hb", [128, D], BF16); rhb = Res()
        p32 = k.sb("p32", [128, PLE], F32); rp32 = Res()
        pb = k.sb("pb", [128, PLE], BF16); rpb = Res()
        t1 = [k.sb("t1_%d" % i, [128, 256], F32) for i in range(2)]; rt1 = [Res(), Res()]
        ring = WRing(k, "wq", 3)
        lt = ln_tmp(k, "lt")
        PT = [k.ps("PT%d" % i, [128, 1024], BF16) for i in range(2)]; rPT = [Res(), Res()]
        PP = [k.ps("PP%d" % i, [128, 512], F32) for i in range(2)]; rPP = [Res(), Res()]
        ptc = [0]; ppc = [0]

        def nextPT():
            i = ptc[0] % 2; ptc[0] += 1
            return PT[i], rPT[i]

        def nextPP():
            i = ppc[0] % 2; ppc[0] += 1
            return PP[i], rPP[i]

        k.dma("sp", ID[:], identb, writes=[rID], key="c0")
        k.dma("sp", LG[:], ln2g.partition_broadcast(128), writes=[rLG], key="c1")
        k.dma("sp", LB[:], ln2b.partition_broadcast(128), writes=[rLB], key="c2")
        k.dma("sp", BG[:], bg.partition_broadcast(128), writes=[rBG], key="c3")
        k.dma("pool", WP[:], wp.rearrange("(kc p) n -> p kc n", p=128), writes=[rWP], key="c4")
        pc = 0
        for j in range(NJ):
            k.dma("sp", H[:, j, :], h1[j * 128:(j + 1) * 128, :], writes=[rH[j]], key="h%d" % j)
            k.op("act", lambda e, j=j: e.mul(out=H[:, j, :], in_=H[:, j, :], mul=ALPHA), reads=[rH[j]], writes=[rH[j]])
            for c in range(NCORES):
                b = pc % 3
                pc += 1
                k.dma("sp", PBUF[b][:], parts[c, j * 128:(j + 1) * 128, :], writes=[rPBUF[b]], key="pb%d" % b)
                eng = "dve" if c % 2 == 0 else "pool"
                k.op(eng, lambda e, b=b, j=j: e.tensor_tensor(out=H[:, j, :], in0=H[:, j, :], in1=PBUF[b][:], op=ALU.add), reads=[rH[j], rPBUF[b]], writes=[rH[j]])
            emit_layernorm(k, H[:, j, :], rH[j], LG, rLG, LB, rLB, lt)
        k.dma("sp", LG[:], ln3g.partition_broadcast(128), writes=[rLG], key="c1")
        k.dma("sp", LB[:], ln3b.partition_broadcast(128), writes=[rLB], key="c2")
        for grp in ([0, 1, 2, 3], [4, 5, 6, 7]):
            wb_next, rwb_next = ring.load([(0, 256, wg[:, 0:256])])
            for c, j in enumerate(grp):
                k.op("act", lambda e, j=j: e.copy(out=hb[:], in_=H[:, j, :]), reads=[rH[j]], writes=[rhb])
                for half in range(2):
                    pt, rpt = nextPT()
                    for i in range(8):
                        kc = half * 8 + i
                        k.op("pe", lambda e, pt=pt, kc=kc, i=i: e.transpose(out=pt[:, i * 128:(i + 1) * 128], in_=hb[:, kc * 128:(kc + 1) * 128], identity=ID[:]),
                             reads=[rhb, rID], writes=[rpt], sig=(i == 7))
                    k.op("dve", lambda e, pt=pt, half=half, c=c: e.tensor_copy(out=HT[:, half * 8:(half + 1) * 8, c * 128:(c + 1) * 128], in_=pt[:].rearrange("p (a b) -> p a b", a=8)),
                         reads=[rpt], writes=[rHT[c]])
                k.op("act", lambda e, j=j: e.mul(out=H[:, j, :], in_=H[:, j, :], mul=ALPHA), reads=[rH[j]], writes=[rH[j]])
                k.dma("sp", p32[:], pin[j * 128:(j + 1) * 128, :], writes=[rp32], key="pp")
                k.op("act", lambda e: e.copy(out=pb[:], in_=p32[:]), reads=[rp32], writes=[rpb])
                pt, rpt = nextPT()
                for i in range(2):
                    k.op("pe", lambda e, pt=pt, i=i: e.transpose(out=pt[:, i * 128:(i + 1) * 128], in_=pb[:, i * 128:(i + 1) * 128], identity=ID[:]),
                         reads=[rpb, rID], writes=[rpt], sig=(i == 1))
                k.op("dve", lambda e, pt=pt, c=c: e.tensor_copy(out=PTT[:, :, c * 128:(c + 1) * 128], in_=pt[:, 0:256].rearrange("p (a b) -> p a b", a=2)),
                     reads=[rpt], writes=[rPTT[c]])
            for gi in range(8):
                wb, rwb = wb_next, rwb_next
                if gi < 7:
                    wb_next, rwb_next = ring.load([(0, 256, wg[:, (gi + 1) * 256:(gi + 2) * 256])])
                cols = slice(gi * 256, (gi + 1) * 256)
                for c, j in enumerate(grp):
                    pp, rpp = nextPP()
                    for kc in range(16):
                        k.op("pe", lambda e, pp=pp, kc=kc, c=c, wb=wb: e.matmul(pp[:, 0:256], lhsT=HT[:, kc, c * 128:(c + 1) * 128], rhs=wb[:, kc, :], start=(kc == 0), stop=(kc == 15)),
                             reads=[rHT[c], rwb], writes=[rpp], sig=False)
                    for kc in range(2):
                        k.op("pe", lambda e, pp=pp, kc=kc, c=c, cols=cols: e.matmul(pp[:, 256:512], lhsT=PTT[:, kc, c * 128:(c + 1) * 128], rhs=WP[:, kc, cols], start=(kc == 0), stop=(kc == 1)),
                             reads=[rPTT[c], rWP], writes=[rpp], sig=(kc == 1))
                    b = (gi * 4 + c) % 2
                    k.op("dve", lambda e, pp=pp, b=b, cols=cols: e.tensor_tensor(out=t1[b][:], in0=pp[:, 0:256], in1=BG[:, cols], op=ALU.add), reads=[rpp, rBG], writes=[rt1[b]])
                    k.op("act", lambda e, b=b: e.activation(out=t1[b][:], in_=t1[b][:], func=AF.Sigmoid), reads=[rt1[b]], writes=[rt1[b]])
                    k.op("dve", lambda e, pp=pp, b=b: e.tensor_tensor(out=t1[b][:], in0=t1[b][:], in1=pp[:, 256:512], op=ALU.mult), reads=[rt1[b], rpp], writes=[rt1[b]])
                    k.op("pool", lambda e, b=b, j=j, cols=cols: e.tensor_tensor(out=H[:, j, cols], in0=H[:, j, cols], in1=t1[b][:], op=ALU.add), reads=[rt1[b], rH[j]], writes=[rH[j]])
            for j in grp:
                emit_layernorm(k, H[:, j, :], rH[j], LG, rLG, LB, rLB, lt)
                k.dma("sp", h3[j * 128:(j + 1) * 128, :], H[:, j, :], reads=[rH[j]], key="oh", is_out=True)
        k.finish()
    return nc


def _consts():
    q = np.arange(128)[:, None]
    j = np.arange(128)[None, :]
    prev = np.where(j > q, 0.0, NEG).astype(np.float32)
    cur = np.where(j <= q, 0.0, NEG).astype(np.float32)
    std = np.concatenate([prev, cur], axis=1)
    first = np.concatenate([np.full((128, 128), NEG, np.float32), cur], axis=1)
    half = 8
    invf = np.exp(-np.log(500000.0) * np.arange(half, dtype=np.float32) * (2.0 / 16)).astype(np.float32)
    return dict(
        identb=np.eye(128).astype(ml_dtypes.bfloat16),
        identf=np.eye(128, dtype=np.float32),
        trild=(j <= q).astype(np.float32),
        invfd=np.ascontiguousarray(np.broadcast_to(invf[None, :], (128, 8))),
        mask_std=std, mask_first=first,
        iotad=np.ascontiguousarray(np.broadcast_to(np.arange(CAP, dtype=np.float32)[None, :], (128, CAP))),
        sud=(q < j).astype(ml_dtypes.bfloat16),
        onesd=np.ones((128, 128), ml_dtypes.bfloat16),
    )


def mixer_inputs(layer, h_full, positions, prm):
    cst = _consts()
    maps = []
    pos_flat = positions.reshape(-1)
    for c in range(NCORES):
        t0 = c * TPC
        seq_start = (t0 % SEQ) == 0
        xin = np.zeros((NCH * 128, D), np.float32)
        pos = np.zeros((NCH * 128,), np.int32)
        xin[128:] = h_full[t0:t0 + TPC]
        pos[128:] = pos_flat[t0:t0 + TPC]
        if not seq_start:
            xin[:128] = h_full[t0 - 128:t0]
            pos[:128] = pos_flat[t0 - 128:t0]
        m = dict(
            xin=xin, posi=np.ascontiguousarray(pos.reshape(NCH, 128).T),
            maskd=np.ascontiguousarray(np.stack([cst["mask_first"] if seq_start else cst["mask_std"], cst["mask_std"]], axis=1)),
            identb=cst["identb"], identf=cst["identf"], trild=cst["trild"], invfd=cst["invfd"],
            w_in=prm["w_in"][layer], sinks=prm["sinks"][layer], w_s=prm["w_s"][layer],
            bsT=np.ascontiguousarray(prm["b_s"][layer].T),
            vnorm_g=np.ascontiguousarray(prm["vnorm_g"][layer].reshape(-1)), vnorm_b=np.ascontiguousarray(prm["vnorm_b"][layer].reshape(-1)),
            gnT=np.ascontiguousarray(np.concatenate([prm["gnorm_attn"][layer], prm["gnorm_gmlp"][layer]]).reshape(16, 128).T),
            w_o=prm["w_o"][layer], ln1_g=prm["ln1_g"][layer], ln1_b=prm["ln1_b"][layer],
            w_router=prm["w_router"][layer], b_router=prm["b_router"][layer],
        )
        if layer == 0:
            m["ln_in_g"] = prm["ln_in_g"]
            m["ln_in_b"] = prm["ln_in_b"]
        maps.append(m)
    return maps


_PROGS = {}


def _prog(name, fn):
    if name not in _PROGS:
        _PROGS[name] = fn()
    return _PROGS[name]


def kernel(**inp):
    prm = {k_: np.asarray(v) for k_, v in inp.items()}
    cores = list(range(NCORES))
    cst = _consts()
    h_full = np.ascontiguousarray(prm["x"].reshape(NTOK, D))
    for layer in range(DEPTH):
        nc = _prog("mix%d" % (layer == 0), lambda: build_mixer(layer == 0))
        res = run_bass_kernel_spmd(nc, mixer_inputs(layer, h_full, prm["positions"], prm), core_ids=cores).results
        h1 = [res[c]["h1"] for c in cores]
        hbt = np.concatenate([res[c]["hbt"] for c in cores], axis=0)
        G = np.concatenate([res[c]["gates"] for c in cores], axis=0)
        del res
        nc = _prog("moe2", build_moe2)
        maps = []
        for c in cores:
            es_ = slice(c * EPC, (c + 1) * EPC)
            maps.append(dict(
                hbt=hbt, gC=np.ascontiguousarray(G[:, es_].reshape(NTOK // 128, 128, EPC).transpose(1, 0, 2)),
                iotad=cst["iotad"], sud=cst["sud"], onesd=cst["onesd"], identb=cst["identb"],
                w_gu=prm["w_gu"][layer, es_],
                bguT=np.ascontiguousarray(prm["b_gu"][layer, es_].reshape(EPC, 32, 128).transpose(2, 0, 1)),
                w_down=prm["w_down"][layer, es_], b_down=prm["b_down"][layer, es_]))
        res = run_bass_kernel_spmd(nc, maps, core_ids=cores).results
        yp = [res[c]["ypart"] for c in cores]
        del res, maps
        nc = _prog("post", build_post)
        p_l = prm["p"][layer].reshape(NTOK, PLE)
        maps = []
        for c in cores:
            ts = slice(c * TPC, (c + 1) * TPC)
            maps.append(dict(
                h1=h1[c], parts=np.ascontiguousarray(np.stack([yp[e][ts] for e in cores], axis=0)),
                p=np.ascontiguousarray(p_l[ts]), identb=cst["identb"],
                ln2_g=prm["ln2_g"][layer], ln2_b=prm["ln2_b"][layer], ln3_g=prm["ln3_g"][layer], ln3_b=prm["ln3_b"][layer],
                w_ple_gate=prm["w_ple_gate"][layer], b_ple_gate=prm["b_ple_gate"][layer], w_ple_proj=prm["w_ple_proj"][layer]))
        del yp
        res = run_bass_kernel_spmd(nc, maps, core_ids=cores).results
        h_full = np.concatenate([res[c]["h3"] for c in cores], axis=0)
        del res, maps
    return h_full.reshape(BATCH, SEQ, D).astype(np.float32)
```

```python
import numpy as np
import ml_dtypes
from contextlib import ExitStack
import concourse.bass as bass
import concourse.mybir as mybir
from concourse.bass_utils import run_bass_kernel_spmd

F32 = mybir.dt.float32
BF16 = mybir.dt.bfloat16
I32 = mybir.dt.int32
ALU = mybir.AluOpType
AF = mybir.ActivationFunctionType
AX = mybir.AxisListType

NCORES = 8
D = 2048
DEPTH = 2
SEQ = 2048
BATCH = 4
NTOK = BATCH * SEQ
TPC = NTOK // NCORES
D_ATTN = 1024
D_GMLP = 1024
D_IN = 3328
NE = 32
EPC = NE // NCORES
PLE = 256
ALPHA = (2.0 * DEPTH) ** 0.25
EPS = 1e-5
PI = float(np.pi)
NEG = -30000.0
EMBED_WAIT = True


class Res:
    __slots__ = ("w", "r")

    def __init__(self):
        self.w = None
        self.r = []


class KB:
    def __init__(self, nc, es):
        self.nc = nc
        self.es = es
        self.eng = {"pe": nc.tensor, "act": nc.scalar, "dve": nc.vector, "pool": nc.gpsimd, "sp": nc.sync}
        self.ops = {k: [] for k in self.eng}
        self.sem = {k: es.enter_context(nc.semaphore("s_" + k)) for k in self.eng}
        self.cnt = {k: 0 for k in self.eng}
        self.waited = {k: {} for k in self.eng}
        self.dsem = {}
        self.out_toks = []

    def sb(self, name, shape, dt):
        return self.es.enter_context(self.nc.sbuf_tensor(name, list(shape), dt))

    def ps(self, name, shape, dt):
        return self.es.enter_context(self.nc.psum_tensor(name, list(shape), dt))

    def _deps(self, e, reads, writes):
        toks = []
        for r in reads:
            if r.w is not None:
                toks.append(r.w)
        for w in writes:
            if w.w is not None:
                toks.append(w.w)
            toks.extend(w.r)
        waits = []
        wd = self.waited[e]
        for (skey, sem, val, teng) in toks:
            if teng == e and e == "pe":
                continue
            if wd.get(skey, 0) >= val:
                continue
            wd[skey] = val
            waits.append((sem, val))
        return waits

    def op(self, e, fn, reads=(), writes=(), sig=True):
        waits = self._deps(e, reads, writes)
        if sig:
            self.cnt[e] += 1
            val = self.cnt[e]
        else:
            val = self.cnt[e] + 1
        tok = (e, self.sem[e], val, e)
        for r in reads:
            r.r.append(tok)
        for w in writes:
            w.w = tok
            w.r = []
        self.ops[e].append((waits, fn, (self.sem[e], 1) if sig else None))
        return tok

    def dma(self, e, out, in_, reads=(), writes=(), key="misc", is_out=False):
        waits = self._deps(e, reads, writes)
        if key not in self.dsem:
            self.dsem[key] = [self.es.enter_context(self.nc.semaphore("d_" + str(key))), 0]
        ds = self.dsem[key]
        ds[1] += 16
        tok = ("d_" + str(key), ds[0], ds[1], "dma")
        for r in reads:
            r.r.append(tok)
        for w in writes:
            w.w = tok
            w.r = []
        self.ops[e].append((waits, (lambda eng, o=out, i=in_: eng.dma_start(out=o, in_=i)), (ds[0], 16)))
        if is_out:
            self.out_toks.append(tok)
        return tok

    def finish(self):
        waits = []
        seen = {}
        for (skey, sem, val, teng) in self.out_toks:
            if seen.get(skey, (None, 0))[1] < val:
                seen[skey] = (sem, val)
        for skey, (sem, val) in seen.items():
            waits.append((sem, val))
        self.ops["sp"].append((waits, None, None))
        with self.nc.Block() as block:
            def mk(name):
                def body(eng):
                    for waits, fn, inc in self.ops[name]:
                        if fn is None:
                            for sem, val in waits:
                                eng.wait_ge(sem, val)
                            continue
                        emb = None
                        if EMBED_WAIT and waits:
                            emb = waits[-1]
                            waits = waits[:-1]
                        for sem, val in waits:
                            eng.wait_ge(sem, val)
                        ins = fn(eng)
                        if emb is not None:
                            ins._wait_ge(emb[0], emb[1])
                        if inc is not None:
                            ins.then_inc(inc[0], inc[1])
                return body
            block.tensor(mk("pe"))
            block.scalar(mk("act"))
            block.vector(mk("dve"))
            block.gpsimd(mk("pool"))
            block.sync(mk("sp"))


def bcast_rows(ap, n=128):
    return ap.partition_broadcast(n)


def emit_layernorm(k, X, rX, G, rG, B, rB, tmp):
    st, rst, mv, rmv, rs, rrs = tmp
    for i in range(4):
        k.op("dve", lambda e, i=i: e.bn_stats(out=st[:, i, :], in_=X[:, i * 512:(i + 1) * 512]),
             reads=[rX], writes=[rst])
    k.op("dve", lambda e: e.bn_aggr(out=mv[:], in_=st[:].rearrange("p a b -> p (a b)")), reads=[rst], writes=[rmv])
    k.op("act", lambda e: e.activation(out=rs[:], in_=mv[:, 1:2], func=AF.Sqrt, bias=EPS, scale=1.0),
         reads=[rmv], writes=[rrs])
    k.op("dve", lambda e: e.reciprocal(out=rs[:], in_=rs[:]), reads=[rrs], writes=[rrs])
    k.op("dve", lambda e: e.tensor_scalar(out=X, in0=X, scalar1=mv[:, 0:1], scalar2=rs[:, 0:1],
                                          op0=ALU.subtract, op1=ALU.mult), reads=[rX, rmv, rrs], writes=[rX])
    k.op("pool", lambda e: e.tensor_tensor(out=X, in0=X, in1=G[:], op=ALU.mult), reads=[rX, rG], writes=[rX])
    k.op("pool", lambda e: e.tensor_tensor(out=X, in0=X, in1=B[:], op=ALU.add), reads=[rX, rB], writes=[rX])


def ln_tmp(k, name):
    return (k.sb(name + "_st", [128, 4, 6], F32), Res(), k.sb(name + "_mv", [128, 2], F32), Res(),
            k.sb(name + "_rs", [128, 1], F32), Res())


class WRing:
    def __init__(self, k, name, nslots, kc=16, ncol=256):
        self.k = k
        self.n = nslots
        self.buf = [k.sb("%s%d" % (name, i), [128, kc, ncol], BF16) for i in range(nslots)]
        self.res = [Res() for _ in range(nslots)]
        self.i = 0
        self.name = name

    def load(self, parts):
        s = self.i % self.n
        self.i += 1
        for (c0, c1, src) in parts:
            self.k.dma("pool", self.buf[s][:, :, c0:c1], src.rearrange("(kc p) n -> p kc n", p=128),
                       writes=[self.res[s]], key="%s%d" % (self.name, s))
        return self.buf[s], self.res[s]


NCH = 9
GROUPS = [[0, 1, 2, 3, 4], [5, 6, 7, 8]]


def build_mixer(layer0, stop=None):
    nc = bass.Bass("TRN2", target_bir_lowering=False)
    dt_in = lambda name, shape, dt=F32: nc.dram_tensor(name, list(shape), dt, kind="ExternalInput").ap()
    dt_out = lambda name, shape, dt=F32: nc.dram_tensor(name, list(shape), dt, kind="ExternalOutput").ap()
    xin = dt_in("xin", [NCH * 128, D])
    posi = dt_in("posi", [128, NCH], I32)
    maskd = dt_in("maskd", [128, 2, 256])
    identb = dt_in("identb", [128, 128], BF16)
    identf = dt_in("identf", [128, 128])
    trild = dt_in("trild", [128, 128])
    invfd = dt_in("invfd", [128, 8])
    if layer0:
        lning = dt_in("ln_in_g", [D])
        lninb = dt_in("ln_in_b", [D])
    w_in = dt_in("w_in", [D, D_IN])
    sinksd = dt_in("sinks", [16])
    w_s = dt_in("w_s", [8, 128, 128])
    bsT = dt_in("bsT", [128, 8])
    vng = dt_in("vnorm_g", [1024])
    vnb = dt_in("vnorm_b", [1024])
    gnT = dt_in("gnT", [128, 16])
    w_o = dt_in("w_o", [D, D])
    ln1g = dt_in("ln1_g", [D])
    ln1b = dt_in("ln1_b", [D])
    w_r = dt_in("w_router", [D, NE])
    b_r = dt_in("b_router", [NE])
    h1_out = dt_out("h1", [TPC, D])
    hT_out = dt_out("hT", [16, 128, TPC], BF16)
    hbt_out = dt_out("hbt", [TPC, D], BF16)
    g_out = dt_out("gates", [TPC, NE])

    with ExitStack() as es:
        k = KB(nc, es)
        ID = k.sb("ID", [128, 128], BF16); rID = Res()
        MASK = k.sb("MASK", [128, 2, 256], F32); rMASK = Res()
        TRIL = k.sb("TRIL", [128, 128], F32); rTRIL = Res()
        INVF = k.sb("INVF", [128, 8], F32); rINVF = Res()
        POSI = k.sb("POSI", [128, NCH], I32); rPOSI = Res()
        LG = k.sb("LG", [128, D], F32); rLG = Res()
        LB = k.sb("LB", [128, D], F32); rLB = Res()
        SINK = k.sb("SINK", [128, 16], F32); rSINK = Res()
        VNG = k.sb("VNG", [128, 1024], F32); rVNG = Res()
        VNB = k.sb("VNB", [128, 1024], F32); rVNB = Res()
        BST = k.sb("BST", [128, 8], F32); rBST = Res()
        GNT = k.sb("GNT", [128, 16], F32); rGNT = Res()
        WR = k.sb("WR", [128, 16, NE], F32); rWR = Res()
        BR = k.sb("BR", [128, NE], F32); rBR = Res()
        WST = k.sb("WST", [128, 8, 128], BF16); rWST = Res()
        H = k.sb("H", [128, NCH, D], F32); rH = [Res() for _ in range(NCH)]
        COS = k.sb("COS", [128, NCH, 4, 8], F32); rCOS = Res()
        SIN = k.sb("SIN", [128, NCH, 4, 8], F32); rSIN = Res()
        KT = k.sb("KT", [128, 2, NCH * 128], BF16); rKT = [Res() for _ in range(NCH)]
        V = k.sb("V", [128, NCH, 128], BF16); rV = [Res() for _ in range(NCH)]
        HT = k.sb("HT", [128, 16, 640], BF16); rHT = [Res() for _ in range(5)]
        YT = k.sb("YT", [128, 16, 512], BF16); rYT = [Res() for _ in range(4)]
        SSQ = k.sb("SSQ", [128, NCH, 12], F32); rSSQ = [Res() for _ in range(NCH)]
        ring = WRing(k, "wr", 3)
        lt = ln_tmp(k, "lt")
        hb = k.sb("hb", [128, D], BF16); rhb = Res()
        t8 = [k.sb("t8_%d" % i, [128, 4, 8], F32) for i in range(4)]; rt8 = [Res() for _ in range(4)]
        qb = k.sb("qb", [128, 4, 64], BF16); rqb = Res()
        kd = k.sb("kd", [128, 2, 64], BF16); rkd = Res()
        qT = k.sb("qT", [128, 4, 128], BF16); rqT = Res()
        sm = k.sb("sm", [128, 4, 256], F32); rsm = Res()
        eb = k.sb("eb", [128, 4, 256], BF16); reb = Res()
        eT = k.sb("eT", [128, 2, 4, 128], BF16); reT = Res()
        mx = k.sb("mx", [128, 4], F32); rmx = Res()
        sx = k.sb("sx", [128, 4], F32); rsx = Res()
        es_ = k.sb("es", [128, 4], F32); res_ = Res()
        o32 = k.sb("o32", [128, 256], F32); ro32 = Res()
        yb = k.sb("yb", [128, 256], BF16); ryb = Res()
        g1 = k.sb("g1", [128, 256], F32); rg1 = Res()
        junk, rjunk = g1, rg1
        g2 = k.sb("g2", [128, 256], F32); rg2 = Res()
        g3 = k.sb("g3", [128, 128], F32); rg3 = Res()
        vst = k.sb("vst", [128, 6], F32); rvst = Res()
        vmv = k.sb("vmv", [128, 2], F32); rvmv = Res()
        vrs = k.sb("vrs", [128, 1], F32); rvrs = Res()
        vh = k.sb("vh", [128, 128], BF16); rvh = Res()
        wtmp = k.sb("wtmp", [128, 128], F32); rwtmp = Res()
        wtb = k.sb("wtb", [128, 128], BF16); rwtb = Res()
        rsa = k.sb("rsa", [128, 2], F32); rrsa = Res()
        hloT = k.sb("hloT", [128, 16, 128], BF16); rhloT = Res()
        hlo = k.sb("hlo", [128, D], BF16); rhlo = Res()
        WRH = k.sb("WRH", [128, 16, NE], BF16); rWRH = Res()
        WRL = k.sb("WRL", [128, 16, NE], BF16); rWRL = Res()
        hbT = k.sb("hbT", [128, 16, 128], BF16); rhbT = Res()
        lg = k.sb("lg", [128, NE], F32); rlg = Res()
        m8 = k.sb("m8", [128, 8], F32); rm8 = Res()
        msk = k.sb("msk", [128, NE], F32); rmsk = Res()
        nm = k.sb("nm", [128, 1], F32); rnm = Res()
        gs = k.sb("gs", [128, 1], F32); rgs = Res()
        posf = k.sb("posf", [128, NCH], F32); rposf = Res()
        ang = k.sb("ang", [128, NCH, 8], F32); rang = Res()
        qf = k.sb("qf", [128, NCH, 8], F32); rqf = Res()
        qi = k.sb("qi", [128, NCH, 8], I32); rqi = Res()
        PT = [k.ps("PT%d" % i, [128, 1024], BF16) for i in range(2)]; rPT = [Res(), Res()]
        PP = [k.ps("PP%d" % i, [128, 512], F32) for i in range(2)]; rPP = [Res(), Res()]
        PS = k.ps("PS", [128, 4, 256], F32); rPS = Res()
        PV = k.ps("PV", [128, 512], F32); rPV = Res()
        PR = k.ps("PR", [128, 512], F32); rPR = Res()
        ptc = [0]
        ppc = [0]

        def nextPT():
            i = ptc[0] % 2; ptc[0] += 1
            return PT[i], rPT[i]

        def nextPP():
            i = ppc[0] % 2; ppc[0] += 1
            return PP[i], rPP[i]

        k.op("pool", lambda e: e.memset(SSQ[:], 0.0), writes=rSSQ)
        ld = lambda dst, src, r, key: k.dma("sp", dst, src, writes=[r], key=key)
        ld(ID[:], identb, rID, "c0"); ld(MASK[:], maskd, rMASK, "c2")
        ld(TRIL[:], trild, rTRIL, "c3"); ld(INVF[:], invfd, rINVF, "c4"); ld(POSI[:], posi, rPOSI, "c5")
        ld(SINK[:], sinksd.partition_broadcast(128), rSINK, "c6")
        ld(VNG[:], vng.partition_broadcast(128), rVNG, "c7"); ld(VNB[:], vnb.partition_broadcast(128), rVNB, "c8")
        ld(BST[:], bsT, rBST, "c9"); ld(GNT[:], gnT, rGNT, "c10")
        ld(WR[:], w_r.rearrange("(kc p) n -> p kc n", p=128), rWR, "c11")
        ld(BR[:], b_r.partition_broadcast(128), rBR, "c12")
        k.op("dve", lambda e: e.tensor_copy(out=WRH[:], in_=WR[:]), reads=[rWR], writes=[rWRH])
        k.op("dve", lambda e: e.tensor_tensor(out=WRL[:], in0=WR[:], in1=WRH[:], op=ALU.subtract), reads=[rWR, rWRH], writes=[rWRL])
        if layer0:
            ld(LG[:], lning.partition_broadcast(128), rLG, "c13"); ld(LB[:], lninb.partition_broadcast(128), rLB, "c14")
        for j in range(NCH):
            k.dma("sp", H[:, j, :], xin[j * 128:(j + 1) * 128, :], writes=[rH[j]], key="x%d" % j)
            if layer0:
                emit_layernorm(k, H[:, j, :], rH[j], LG, rLG, LB, rLB, lt)
        if stop == 'ln':
            k.finish()
            return nc
        ld(LG[:], ln1g.partition_broadcast(128), rLG, "c13"); ld(LB[:], ln1b.partition_broadcast(128), rLB, "c14")
        k.op("dve", lambda e: e.tensor_copy(out=posf[:], in_=POSI[:]), reads=[rPOSI], writes=[rposf])
        for which, TAB, rTAB in ((0, SIN, rSIN), (1, COS, rCOS)):
            for j in range(NCH):
                k.op("dve", lambda e, j=j, which=which: e.tensor_scalar(out=ang[:, j, :], in0=INVF[:], scalar1=posf[:, j:j + 1],
                                                           scalar2=(PI / 2 if which else 0.0), op0=ALU.mult, op1=ALU.add),
                     reads=[rINVF, rposf], writes=[rang])
            k.op("dve", lambda e: e.tensor_scalar(out=qf[:], in0=ang[:], scalar1=1.0 / (2 * PI), scalar2=None, op0=ALU.mult),
                 reads=[rang], writes=[rqf])
            k.op("dve", lambda e: e.tensor_copy(out=qi[:], in_=qf[:]), reads=[rqf], writes=[rqi])
            k.op("dve", lambda e: e.tensor_copy(out=qf[:], in_=qi[:]), reads=[rqi], writes=[rqf])
            k.op("dve", lambda e: e.scalar_tensor_tensor(out=ang[:], in0=qf[:], scalar=-2 * PI, in1=ang[:], op0=ALU.mult, op1=ALU.add),
                 reads=[rqf, rang], writes=[rang])
            k.op("dve", lambda e: e.tensor_scalar(out=qf[:], in0=ang[:], scalar1=PI, scalar2=None, op0=ALU.is_gt), reads=[rang], writes=[rqf])
            k.op("dve", lambda e: e.scalar_tensor_tensor(out=ang[:], in0=qf[:], scalar=-2 * PI, in1=ang[:], op0=ALU.mult, op1=ALU.add),
                 reads=[rqf, rang], writes=[rang])
            k.op("dve", lambda e: e.tensor_scalar(out=qf[:], in0=ang[:], scalar1=-PI, scalar2=None, op0=ALU.is_lt), reads=[rang], writes=[rqf])
            k.op("dve", lambda e: e.scalar_tensor_tensor(out=ang[:], in0=qf[:], scalar=2 * PI, in1=ang[:], op0=ALU.mult, op1=ALU.add),
                 reads=[rqf, rang], writes=[rang])
            for hh in range(4):
                k.op("act", lambda e, hh=hh, TAB=TAB: e.activation(out=TAB[:, :, hh, :], in_=ang[:], func=AF.Sin),
                     reads=[rang], writes=[rTAB])
        if stop == 'rope':
            k.finish()
            return nc
        for h in range(8):
            k.dma("sp", wtmp[:], w_s[h], writes=[rwtmp], key="ws")
            k.op("dve", lambda e: e.tensor_tensor(out=wtb[:], in0=wtmp[:], in1=TRIL[:], op=ALU.mult),
                 reads=[rwtmp, rTRIL], writes=[rwtb])
            pt, rpt = nextPT()
            k.op("pe", lambda e, pt=pt: e.transpose(out=pt[:, 0:128], in_=wtb[:], identity=ID[:]),
                 reads=[rwtb, rID], writes=[rpt])
            k.op("act", lambda e, pt=pt, h=h: e.copy(out=WST[:, h, :], in_=pt[:, 0:128]), reads=[rpt], writes=[rWST])

        if stop == 'wst':
            k.finish()
            return nc
        for grp in GROUPS:
            own = [j for j in grp if j >= 1]
            nloc = len(grp)
            loc = {j: i for i, j in enumerate(grp)}
            yloc = {j: i for i, j in enumerate(own)}
            wb_kv, rwb_kv = ring.load([(0, 256, w_in[:, 1024:1280])])
            for j in grp:
                k.op("act", lambda e, j=j: e.copy(out=hb[:], in_=H[:, j, :]), reads=[rH[j]], writes=[rhb])
                for half in range(2):
                    pt, rpt = nextPT()
                    for i in range(8):
                        kc = half * 8 + i
                        k.op("pe", lambda e, pt=pt, kc=kc, i=i: e.transpose(out=pt[:, i * 128:(i + 1) * 128], in_=hb[:, kc * 128:(kc + 1) * 128], identity=ID[:]),
                             reads=[rhb, rID], writes=[rpt], sig=(i == 7))
                    k.op("dve", lambda e, pt=pt, half=half, c=loc[j]: e.tensor_copy(out=HT[:, half * 8:(half + 1) * 8, c * 128:(c + 1) * 128], in_=pt[:].rearrange("p (a b) -> p a b", a=8)),
                         reads=[rpt], writes=[rHT[loc[j]]])
                if j >= 1:
                    k.op("act", lambda e, j=j: e.mul(out=H[:, j, :], in_=H[:, j, :], mul=ALPHA), reads=[rH[j]], writes=[rH[j]])
            if stop == 'ht':
                k.finish()
                return nc
            wb_next, rwb_next = ring.load([(0, 256, w_in[:, 0:256])])
            for j in grp:
                pp, rpp = nextPP()
                c = loc[j]
                for kc in range(16):
                    k.op("pe", lambda e, pp=pp, kc=kc, c=c: e.matmul(pp[:, 0:256], lhsT=HT[:, kc, c * 128:(c + 1) * 128], rhs=wb_kv[:, kc, :], start=(kc == 0), stop=(kc == 15)),
                         reads=[rHT[c], rwb_kv], writes=[rpp], sig=(kc == 15))
                kv = pp[:, 0:128].rearrange("p (g d) -> p g d", g=2)
                cs, sn = COS[:, j, 0:2, :], SIN[:, j, 0:2, :]
                x1, x2 = kv[:, :, 0:8], kv[:, :, 8:16]
                k.op("dve", lambda e, x1=x1, cs=cs: e.tensor_tensor(out=t8[0][:, 0:2, :], in0=x1, in1=cs, op=ALU.mult), reads=[rpp, rCOS], writes=[rt8[0]])
                k.op("dve", lambda e, x2=x2, sn=sn: e.tensor_tensor(out=t8[1][:, 0:2, :], in0=x2, in1=sn, op=ALU.mult), reads=[rpp, rSIN], writes=[rt8[1]])
                k.op("dve", lambda e, x2=x2, cs=cs: e.tensor_tensor(out=t8[2][:, 0:2, :], in0=x2, in1=cs, op=ALU.mult), reads=[rpp, rCOS], writes=[rt8[2]])
                k.op("dve", lambda e, x1=x1, sn=sn: e.tensor_tensor(out=t8[3][:, 0:2, :], in0=x1, in1=sn, op=ALU.mult), reads=[rpp, rSIN], writes=[rt8[3]])
                k.op("dve", lambda e: e.tensor_tensor(out=kd[:, :, 0:8], in0=t8[0][:, 0:2, :], in1=t8[1][:, 0:2, :], op=ALU.subtract), reads=[rt8[0], rt8[1]], writes=[rkd])
                k.op("dve", lambda e: e.tensor_tensor(out=kd[:, :, 8:16], in0=t8[2][:, 0:2, :], in1=t8[3][:, 0:2, :], op=ALU.add), reads=[rt8[2], rt8[3]], writes=[rkd])
                k.op("act", lambda e, kv=kv: e.copy(out=kd[:, :, 16:64], in_=kv[:, :, 16:64]), reads=[rpp], writes=[rkd])
                k.op("act", lambda e, pp=pp, j=j: e.copy(out=V[:, j, :], in_=pp[:, 128:256]), reads=[rpp], writes=[rV[j]])
                pt, rpt = nextPT()
                for g in range(2):
                    k.op("pe", lambda e, pt=pt, g=g: e.transpose(out=pt[0:64, g * 128:(g + 1) * 128], in_=kd[:, g, :], identity=ID[:]),
                         reads=[rkd, rID], writes=[rpt], sig=(g == 1))
                k.op("dve", lambda e, pt=pt, j=j: e.tensor_copy(out=KT[0:64, :, j * 128:(j + 1) * 128], in_=pt[0:64, 0:256].rearrange("p (g t) -> p g t", g=2)),
                     reads=[rpt], writes=[rKT[j]])
            if stop == 'kv':
                k.finish()
                return nc
            for qi_ in range(4):
                wb, rwb = wb_next, rwb_next
                if qi_ < 3:
                    wb_next, rwb_next = ring.load([(0, 256, w_in[:, (qi_ + 1) * 256:(qi_ + 2) * 256])])
                else:
                    wb_next, rwb_next = ring.load([(0, 128, w_in[:, 1280:1408]), (128, 256, w_in[:, 2304:2432])])
                g = qi_ // 2
                for j in own:
                    c = loc[j]
                    pp, rpp = nextPP()
                    for kc in range(16):
                        k.op("pe", lambda e, pp=pp, kc=kc, c=c, wb=wb: e.matmul(pp[:, 0:256], lhsT=HT[:, kc, c * 128:(c + 1) * 128], rhs=wb[:, kc, :], start=(kc == 0), stop=(kc == 15)),
                             reads=[rHT[c], rwb], writes=[rpp], sig=(kc == 15))
                    q4 = pp[:, 0:256].rearrange("p (h d) -> p h d", h=4)
                    cs, sn = COS[:, j, :, :], SIN[:, j, :, :]
                    x1, x2 = q4[:, :, 0:8], q4[:, :, 8:16]
                    k.op("dve", lambda e, x1=x1, cs=cs: e.tensor_tensor(out=t8[0][:], in0=x1, in1=cs, op=ALU.mult), reads=[rpp, rCOS], writes=[rt8[0]])
                    k.op("dve", lambda e, x2=x2, sn=sn: e.tensor_tensor(out=t8[1][:], in0=x2, in1=sn, op=ALU.mult), reads=[rpp, rSIN], writes=[rt8[1]])
                    k.op("dve", lambda e, x2=x2, cs=cs: e.tensor_tensor(out=t8[2][:], in0=x2, in1=cs, op=ALU.mult), reads=[rpp, rCOS], writes=[rt8[2]])
                    k.op("dve", lambda e, x1=x1, sn=sn: e.tensor_tensor(out=t8[3][:], in0=x1, in1=sn, op=ALU.mult), reads=[rpp, rSIN], writes=[rt8[3]])
                    k.op("dve", lambda e: e.tensor_tensor(out=qb[:, :, 0:8], in0=t8[0][:], in1=t8[1][:], op=ALU.subtract), reads=[rt8[0], rt8[1]], writes=[rqb])
                    k.op("dve", lambda e: e.tensor_tensor(out=qb[:, :, 8:16], in0=t8[2][:], in1=t8[3][:], op=ALU.add), reads=[rt8[2], rt8[3]], writes=[rqb])
                    k.op("act", lambda e, q4=q4: e.copy(out=qb[:, :, 16:64], in_=q4[:, :, 16:64]), reads=[rpp], writes=[rqb])
                    pt, rpt = nextPT()
                    for hh in range(4):
                        k.op("pe", lambda e, pt=pt, hh=hh: e.transpose(out=pt[0:64, hh * 128:(hh + 1) * 128], in_=qb[:, hh, :], identity=ID[:]),
                             reads=[rqb, rID], writes=[rpt], sig=(hh == 3))
                    k.op("dve", lambda e, pt=pt: e.tensor_copy(out=qT[0:64, :, :], in_=pt[0:64, 0:512].rearrange("p (a t) -> p a t", a=4)), reads=[rpt], writes=[rqT])
                    for hh in range(4):
                        k.op("pe", lambda e, hh=hh, j=j, g=g: e.matmul(PS[:, hh, :], lhsT=qT[0:64, hh, :], rhs=KT[0:64, g, (j - 1) * 128:(j + 1) * 128], start=True, stop=True),
                             reads=[rqT, rKT[j - 1], rKT[j]], writes=[rPS], sig=(hh == 3))
                    if stop == 'a1':
                        k.finish()
                        return nc
                    mi = 0 if j == 1 else 1
                    k.op("dve", lambda e, mi=mi: e.scalar_tensor_tensor(out=sm[:], in0=PS[:], scalar=0.125, in1=MASK[:, mi, :].unsqueeze(1).to_broadcast([128, 4, 256]), op0=ALU.mult, op1=ALU.add),
                         reads=[rPS, rMASK], writes=[rsm])
                    k.op("dve", lambda e: e.tensor_reduce(out=mx[:], in_=sm[:], axis=AX.X, op=ALU.max), reads=[rsm], writes=[rmx])
                    k.op("dve", lambda e, qi_=qi_: e.tensor_tensor(out=mx[:], in0=mx[:], in1=SINK[:, qi_ * 4:qi_ * 4 + 4], op=ALU.max), reads=[rmx, rSINK], writes=[rmx])
                    k.op("dve", lambda e: e.tensor_tensor(out=sm[:], in0=sm[:], in1=mx[:].unsqueeze(2).to_broadcast([128, 4, 256]), op=ALU.subtract), reads=[rsm, rmx], writes=[rsm])
                    k.op("act", lambda e: e.activation(out=eb[:], in_=sm[:], func=AF.Exp), reads=[rsm], writes=[reb])
                    k.op("dve", lambda e, qi_=qi_: e.tensor_tensor(out=es_[:], in0=SINK[:, qi_ * 4:qi_ * 4 + 4], in1=mx[:], op=ALU.subtract), reads=[rmx, rSINK], writes=[res_])
                    k.op("act", lambda e: e.activation(out=es_[:], in_=es_[:], func=AF.Exp), reads=[res_], writes=[res_])
                    k.op("dve", lambda e: e.tensor_reduce(out=sx[:], in_=eb[:], axis=AX.X, op=ALU.add), reads=[reb], writes=[rsx])
                    k.op("dve", lambda e: e.tensor_tensor(out=sx[:], in0=sx[:], in1=es_[:], op=ALU.add), reads=[rsx, res_], writes=[rsx])
                    k.op("dve", lambda e: e.reciprocal(out=sx[:], in_=sx[:]), reads=[rsx], writes=[rsx])
                    if stop == 'a2':
                        k.finish()
                        return nc
                    pt, rpt = nextPT()
                    for kcx in range(2):
                        for hh in range(4):
                            idx = kcx * 4 + hh
                            k.op("pe", lambda e, pt=pt, kcx=kcx, hh=hh, idx=idx: e.transpose(out=pt[:, idx * 128:(idx + 1) * 128], in_=eb[:, hh, kcx * 128:(kcx + 1) * 128], identity=ID[:]),
                                 reads=[reb, rID], writes=[rpt], sig=(idx == 7))
                    k.op("act", lambda e, pt=pt: e.copy(out=eT[:], in_=pt[:].rearrange("p (a h t) -> p a h t", a=2, h=4)), reads=[rpt], writes=[reT])
                    if stop == 'a3':
                        k.finish()
                        return nc
                    for hh in range(4):
                        for kcx in range(2):
                            k.op("pe", lambda e, hh=hh, kcx=kcx, j=j, g=g: e.matmul(PV[:, hh * 64:(hh + 1) * 64], lhsT=eT[:, kcx, hh, :], rhs=V[:, j - 1 + kcx, g * 64:(g + 1) * 64], start=(kcx == 0), stop=(kcx == 1)),
                                 reads=[reT, rV[j - 1], rV[j]], writes=[rPV], sig=(hh == 3 and kcx == 1))
                    if stop == 'a4':
                        k.finish()
                        return nc
                    k.op("dve", lambda e: e.tensor_tensor(out=o32[:].rearrange("p (h d) -> p h d", h=4), in0=PV[:, 0:256].rearrange("p (h d) -> p h d", h=4), in1=sx[:].unsqueeze(2).to_broadcast([128, 4, 64]), op=ALU.mult),
                         reads=[rPV, rsx], writes=[ro32])
                    if stop == 'a5':
                        k.finish()
                        return nc
                    k.op("act", lambda e, j=j, qi_=qi_: e.activation(out=junk[:], in_=o32[:], func=AF.Square, accum_out=SSQ[:, j, qi_:qi_ + 1]), reads=[ro32], writes=[rjunk, rSSQ[j]])
                    if stop == 'a6':
                        k.finish()
                        return nc
                    k.op("act", lambda e: e.copy(out=yb[:], in_=o32[:]), reads=[ro32], writes=[ryb])
                    pt, rpt = nextPT()
                    for pr in range(2):
                        k.op("pe", lambda e, pt=pt, pr=pr: e.transpose(out=pt[:, pr * 128:(pr + 1) * 128], in_=yb[:, pr * 128:(pr + 1) * 128], identity=ID[:]),
                             reads=[ryb, rID], writes=[rpt], sig=(pr == 1))
                    for pr in range(2):
                        kc = 2 * qi_ + pr
                        k.op("dve", lambda e, pt=pt, pr=pr, kc=kc, yc=yloc[j]: e.tensor_scalar(out=YT[:, kc, yc * 128:(yc + 1) * 128], in0=pt[:, pr * 128:(pr + 1) * 128], scalar1=GNT[:, kc:kc + 1], scalar2=None, op0=ALU.mult),
                             reads=[rpt, rGNT], writes=[rYT[yloc[j]]])
            if stop == 'attn':
                k.finish()
                return nc
            for hd in range(8):
                wb, rwb = wb_next, rwb_next
                if hd < 7:
                    wb_next, rwb_next = ring.load([(0, 128, w_in[:, 1280 + (hd + 1) * 128:1280 + (hd + 2) * 128]),
                                                   (128, 256, w_in[:, 2304 + (hd + 1) * 128:2304 + (hd + 2) * 128])])
                else:
                    wb_next, rwb_next = ring.load([(0, 256, w_o[:, 0:256])])
                for j in own:
                    c = loc[j]
                    pp, rpp = nextPP()
                    for kc in range(16):
                        k.op("pe", lambda e, pp=pp, kc=kc, c=c, wb=wb: e.matmul(pp[:, 0:256], lhsT=HT[:, kc, c * 128:(c + 1) * 128], rhs=wb[:, kc, :], start=(kc == 0), stop=(kc == 15)),
                             reads=[rHT[c], rwb], writes=[rpp], sig=(kc == 15))
                    z = pp[:, 0:256]
                    k.op("act", lambda e, z=z: e.activation(out=g1[:], in_=z, func=AF.Square), reads=[rpp], writes=[rg1])
                    k.op("dve", lambda e: e.tensor_scalar(out=g1[:], in0=g1[:], scalar1=0.044715, scalar2=1.0, op0=ALU.mult, op1=ALU.add), reads=[rg1], writes=[rg1])
                    k.op("dve", lambda e, z=z: e.tensor_tensor(out=g1[:], in0=g1[:], in1=z, op=ALU.mult), reads=[rg1, rpp], writes=[rg1])
                    k.op("act", lambda e: e.activation(out=g1[:], in_=g1[:], func=AF.Sigmoid, scale=1.5957691216057308), reads=[rg1], writes=[rg1])
                    k.op("dve", lambda e, z=z: e.tensor_tensor(out=g2[:], in0=g1[:], in1=z, op=ALU.mult), reads=[rg1, rpp], writes=[rg2])
                    k.op("dve", lambda e: e.bn_stats(out=vst[:], in_=g2[:, 128:256]), reads=[rg2], writes=[rvst])
                    k.op("dve", lambda e: e.bn_aggr(out=vmv[:], in_=vst[:]), reads=[rvst], writes=[rvmv])
                    k.op("act", lambda e: e.activation(out=vrs[:], in_=vmv[:, 1:2], func=AF.Sqrt, bias=EPS, scale=1.0), reads=[rvmv], writes=[rvrs])
                    k.op("dve", lambda e: e.reciprocal(out=vrs[:], in_=vrs[:]), reads=[rvrs], writes=[rvrs])
                    k.op("dve", lambda e: e.tensor_scalar(out=g3[:, 0:128], in0=g2[:, 128:256], scalar1=vmv[:, 0:1], scalar2=vrs[:, 0:1], op0=ALU.subtract, op1=ALU.mult), reads=[rg2, rvmv, rvrs], writes=[rg3])
                    k.op("pool", lambda e, hd=hd: e.tensor_tensor(out=g3[:, 0:128], in0=g3[:, 0:128], in1=VNG[:, hd * 128:(hd + 1) * 128], op=ALU.mult), reads=[rg3, rVNG], writes=[rg3])
                    k.op("pool", lambda e, hd=hd: e.tensor_tensor(out=vh[:], in0=g3[:, 0:128], in1=VNB[:, hd * 128:(hd + 1) * 128], op=ALU.add), reads=[rg3, rVNB], writes=[rvh])
                    k.op("pe", lambda e, hd=hd: e.matmul(PV[:, 256:384], lhsT=WST[:, hd, :], rhs=vh[:], start=True, stop=True), reads=[rWST, rvh], writes=[rPV])
                    k.op("dve", lambda e, hd=hd: e.scalar_tensor_tensor(out=o32[:, 0:128], in0=PV[:, 256:384], scalar=BST[:, hd:hd + 1], in1=g2[:, 0:128], op0=ALU.add, op1=ALU.mult), reads=[rPV, rBST, rg2], writes=[ro32])
                    k.op("act", lambda e, j=j, hd=hd: e.activation(out=junk[:, 0:128], in_=o32[:, 0:128], func=AF.Square, accum_out=SSQ[:, j, 4 + hd:5 + hd]),
                         reads=[ro32], writes=[rjunk, rSSQ[j]])
                    k.op("act", lambda e: e.copy(out=yb[:, 0:128], in_=o32[:, 0:128]), reads=[ro32], writes=[ryb])
                    pt, rpt = nextPT()
                    k.op("pe", lambda e, pt=pt: e.transpose(out=pt[:, 0:128], in_=yb[:, 0:128], identity=ID[:]), reads=[ryb, rID], writes=[rpt])
                    k.op("dve", lambda e, pt=pt, hd=hd, yc=yloc[j]: e.tensor_scalar(out=YT[:, 8 + hd, yc * 128:(yc + 1) * 128], in0=pt[:, 0:128], scalar1=GNT[:, 8 + hd:9 + hd], scalar2=None, op0=ALU.mult),
                         reads=[rpt, rGNT], writes=[rYT[yloc[j]]])
            if stop == 'gmlp':
                k.finish()
                return nc
            for j in own:
                k.op("dve", lambda e, j=j: e.tensor_reduce(out=rsa[:, 0:1], in_=SSQ[:, j, 0:4], axis=AX.X, op=ALU.add), reads=[rSSQ[j]], writes=[rrsa])
                k.op("dve", lambda e, j=j: e.tensor_reduce(out=rsa[:, 1:2], in_=SSQ[:, j, 4:12], axis=AX.X, op=ALU.add), reads=[rSSQ[j]], writes=[rrsa])
                k.op("act", lambda e: e.activation(out=rsa[:], in_=rsa[:], func=AF.Sqrt, bias=EPS, scale=1.0 / 1024.0), reads=[rrsa], writes=[rrsa])
                k.op("dve", lambda e, j=j: e.reciprocal(out=SSQ[:, j, 0:2], in_=rsa[:]), reads=[rrsa], writes=[rSSQ[j]])
            for oi in range(8):
                wb, rwb = wb_next, rwb_next
                if oi < 7:
                    wb_next, rwb_next = ring.load([(0, 256, w_o[:, (oi + 1) * 256:(oi + 2) * 256])])
                for j in own:
                    yc = yloc[j]
                    pp, rpp = nextPP()
                    for half in range(2):
                        for i in range(8):
                            kc = half * 8 + i
                            k.op("pe", lambda e, pp=pp, kc=kc, yc=yc, wb=wb, half=half, i=i: e.matmul(pp[:, half * 256:(half + 1) * 256], lhsT=YT[:, kc, yc * 128:(yc + 1) * 128], rhs=wb[:, kc, :], start=(i == 0), stop=(i == 7)),
                                 reads=[rYT[yc], rwb], writes=[rpp], sig=(kc == 15))
                    hs = H[:, j, oi * 256:(oi + 1) * 256]
                    k.op("dve", lambda e, pp=pp, hs=hs, j=j: e.scalar_tensor_tensor(out=hs, in0=pp[:, 0:256], scalar=SSQ[:, j, 0:1], in1=hs, op0=ALU.mult, op1=ALU.add), reads=[rpp, rSSQ[j], rH[j]], writes=[rH[j]])
                    k.op("dve", lambda e, pp=pp, hs=hs, j=j: e.scalar_tensor_tensor(out=hs, in0=pp[:, 256:512], scalar=SSQ[:, j, 1:2], in1=hs, op0=ALU.mult, op1=ALU.add), reads=[rpp, rSSQ[j], rH[j]], writes=[rH[j]])
            if stop == 'wo':
                k.finish()
                return nc
            for j in own:
                emit_layernorm(k, H[:, j, :], rH[j], LG, rLG, LB, rLB, lt)
                k.dma("sp", h1_out[(j - 1) * 128:j * 128, :], H[:, j, :], reads=[rH[j]], key="oh", is_out=True)
                k.op("act", lambda e, j=j: e.copy(out=hb[:], in_=H[:, j, :]), reads=[rH[j]], writes=[rhb])
                k.op("dve", lambda e, j=j: e.tensor_tensor(out=hlo[:], in0=H[:, j, :], in1=hb[:], op=ALU.subtract), reads=[rH[j], rhb], writes=[rhlo])
                k.dma("sp", hbt_out[(j - 1) * 128:j * 128, :], hb[:], reads=[rhb], key="ob", is_out=True)
                for src, rsrc, dstT, rdstT in ((hb, rhb, hbT, rhbT), (hlo, rhlo, hloT, rhloT)):
                    for half in range(2):
                        pt, rpt = nextPT()
                        for i in range(8):
                            kc = half * 8 + i
                            k.op("pe", lambda e, pt=pt, kc=kc, i=i, src=src: e.transpose(out=pt[:, i * 128:(i + 1) * 128], in_=src[:, kc * 128:(kc + 1) * 128], identity=ID[:]),
                                 reads=[rsrc, rID], writes=[rpt], sig=(i == 7))
                        k.op("dve", lambda e, pt=pt, half=half, dstT=dstT: e.tensor_copy(out=dstT[:, half * 8:(half + 1) * 8, :], in_=pt[:].rearrange("p (a b) -> p a b", a=8)),
                             reads=[rpt], writes=[rdstT])
                k.dma("sp", hT_out[:, :, (j - 1) * 128:j * 128].rearrange("kc p t -> p kc t"), hbT[:], reads=[rhbT], key="ot", is_out=True)
                n = 0
                for (aT, raT, wq, rwq) in ((hbT, rhbT, WRH, rWRH), (hloT, rhloT, WRH, rWRH), (hbT, rhbT, WRL, rWRL)):
                    for kc in range(16):
                        k.op("pe", lambda e, kc=kc, aT=aT, wq=wq, n=n: e.matmul(PR[:, 0:NE], lhsT=aT[:, kc, :], rhs=wq[:, kc, :], start=(n == 0), stop=(n == 47)),
                             reads=[raT, rwq], writes=[rPR], sig=(n == 47))
                        n += 1
                k.op("dve", lambda e: e.tensor_tensor(out=lg[:], in0=PR[:, 0:NE], in1=BR[:], op=ALU.add), reads=[rPR, rBR], writes=[rlg])
                k.op("dve", lambda e: e.max(out=m8[:], in_=lg[:]), reads=[rlg], writes=[rm8])
                k.op("dve", lambda e: e.tensor_scalar(out=msk[:], in0=lg[:], scalar1=m8[:, 3:4], scalar2=None, op0=ALU.is_ge), reads=[rlg, rm8], writes=[rmsk])
                k.op("dve", lambda e: e.tensor_scalar(out=nm[:], in0=m8[:, 0:1], scalar1=-1.0, scalar2=None, op0=ALU.mult), reads=[rm8], writes=[rnm])
                k.op("act", lambda e: e.activation(out=lg[:], in_=lg[:], func=AF.Exp, bias=nm[:, 0:1], scale=1.0), reads=[rlg, rnm], writes=[rlg])
                k.op("dve", lambda e: e.tensor_tensor(out=lg[:], in0=lg[:], in1=msk[:], op=ALU.mult), reads=[rlg, rmsk], writes=[rlg])
                k.op("dve", lambda e: e.tensor_reduce(out=gs[:], in_=lg[:], axis=AX.X, op=ALU.add), reads=[rlg], writes=[rgs])
                k.op("dve", lambda e: e.reciprocal(out=gs[:], in_=gs[:]), reads=[rgs], writes=[rgs])
                k.op("dve", lambda e: e.tensor_scalar(out=lg[:], in0=lg[:], scalar1=gs[:, 0:1], scalar2=None, op0=ALU.mult), reads=[rlg, rgs], writes=[rlg])
                k.dma("sp", g_out[(j - 1) * 128:j * 128, :], lg[:], reads=[rlg], key="og", is_out=True)
        k.finish()
    return nc


TG = 1024


def build_moe():
    nc = bass.Bass("TRN2", target_bir_lowering=False)
    dt_in = lambda name, shape, dt=F32: nc.dram_tensor(name, list(shape), dt, kind="ExternalInput").ap()
    xT = dt_in("xT", [128, 16, NTOK], BF16)
    gT = dt_in("gT", [EPC, NTOK])
    w_gu = dt_in("w_gu", [EPC, D, 2 * D])
    bguT = dt_in("bguT", [128, EPC, 32])
    w_dn = dt_in("w_down", [EPC, D, D])
    b_dn = dt_in("b_down", [EPC, D])
    y_out = nc.dram_tensor("ypart", [NTOK, D], F32, kind="ExternalOutput").ap()
    NG = NTOK // TG
    with ExitStack() as es:
        k = KB(nc, es)
        XT = k.sb("XT", [128, 16, TG], BF16); rXT = Res()
        GB = k.sb("GB", [128, EPC, TG], F32); rGB = Res()
        G4 = k.sb("G4", [EPC, TG], F32); rG4 = Res()
        G4b = k.sb("G4b", [EPC, TG], BF16); rG4b = Res()
        BD = k.sb("BD", [EPC, D], F32); rBD = Res()
        BDb = k.sb("BDb", [EPC, D], BF16); rBDb = Res()
        BGU = k.sb("BGU", [128, EPC, 32], F32); rBGU = Res()
        YACC = k.sb("YACC", [128, TG // 128, D], F32); rY = [Res() for _ in range(TG // 128)]
        AT = k.sb("AT", [128, 16, TG], BF16); rAT = [Res() for _ in range(16)]
        ring = WRing(k, "wm", 4)
        T1 = [k.sb("T1_%d" % i, [128, 512], F32) for i in range(2)]; rT1 = [Res(), Res()]
        T2 = [k.sb("T2_%d" % i, [128, 512], F32) for i in range(2)]; rT2 = [Res(), Res()]
        T3 = [k.sb("T3_%d" % i, [128, 512], F32) for i in range(2)]; rT3 = [Res(), Res()]
        PA = [k.ps("PA%d" % i, [128, 512], F32) for i in range(2)]; rPA = [Res(), Res()]
        PB = [k.ps("PB%d" % i, [128, 512], F32) for i in range(2)]; rPB = [Res(), Res()]
        PD = [k.ps("PD%d" % i, [128, 512], F32) for i in range(2)]; rPD = [Res(), Res()]
        k.dma("sp", BGU[:], bguT, writes=[rBGU], key="c0")
        k.dma("sp", BD[:], b_dn, writes=[rBD], key="c1")
        k.op("dve", lambda e: e.tensor_copy(out=BDb[:], in_=BD[:]), reads=[rBD], writes=[rBDb])
        pieces = []
        for tg in range(NG):
            for ex in range(EPC):
                for fc in range(16):
                    pieces.append([(0, 128, w_gu[ex][:, fc * 128:(fc + 1) * 128]), (128, 256, w_gu[ex][:, D + fc * 128:D + (fc + 1) * 128])])
                for dp in range(8):
                    pieces.append([(0, 256, w_dn[ex][:, dp * 256:(dp + 1) * 256])])
        LA = 2
        loaded = []
        pi = [0]

        def get_piece():
            while len(loaded) < min(len(pieces), pi[0] + 1 + LA):
                loaded.append(ring.load(pieces[len(loaded)]))
            r = loaded[pi[0]]
            pi[0] += 1
            return r

        cnt = 0
        dcnt = 0
        for tg in range(NG):
            t0 = tg * TG
            k.dma("sp", XT[:], xT[:, :, t0:t0 + TG], writes=[rXT], key="xt")
            for ex in range(EPC):
                k.dma("sp", GB[:, ex, :], gT[ex, t0:t0 + TG].partition_broadcast(128), writes=[rGB], key="gb")
            k.dma("sp", G4[:], gT[:, t0:t0 + TG], writes=[rG4], key="g4")
            k.op("dve", lambda e: e.tensor_copy(out=G4b[:], in_=G4[:]), reads=[rG4], writes=[rG4b])
            for ex in range(EPC):
                for fc in range(16):
                    wb, rwb = get_piece()
                    for ns in range(2):
                        b = cnt % 2
                        cnt += 1
                        for kc in range(16):
                            k.op("pe", lambda e, b=b, kc=kc, wb=wb, ns=ns: e.matmul(PA[b][:], lhsT=wb[:, kc, 0:128], rhs=XT[:, kc, ns * 512:(ns + 1) * 512], start=(kc == 0), stop=(kc == 15)),
                                 reads=[rwb, rXT], writes=[rPA[b]], sig=(kc == 15))
                        for kc in range(16):
                            k.op("pe", lambda e, b=b, kc=kc, wb=wb, ns=ns: e.matmul(PB[b][:], lhsT=wb[:, kc, 128:256], rhs=XT[:, kc, ns * 512:(ns + 1) * 512], start=(kc == 0), stop=(kc == 15)),
                                 reads=[rwb, rXT], writes=[rPB[b]], sig=(kc == 15))
                        k.op("dve", lambda e, b=b, ex=ex, fc=fc: e.tensor_scalar(out=T1[b][:], in0=PA[b][:], scalar1=BGU[:, ex, fc:fc + 1], scalar2=7.0, op0=ALU.add, op1=ALU.min),
                             reads=[rPA[b], rBGU], writes=[rT1[b]])
                        k.op("act", lambda e, b=b: e.activation(out=T2[b][:], in_=T1[b][:], func=AF.Sigmoid, scale=1.702), reads=[rT1[b]], writes=[rT2[b]])
                        k.op("dve", lambda e, b=b, ex=ex, fc=fc: e.tensor_scalar(out=T3[b][:], in0=PB[b][:], scalar1=BGU[:, ex, 16 + fc:17 + fc], scalar2=7.0, op0=ALU.add, op1=ALU.min),
                             reads=[rPB[b], rBGU], writes=[rT3[b]])
                        k.op("dve", lambda e, b=b: e.tensor_scalar(out=T3[b][:], in0=T3[b][:], scalar1=-7.0, scalar2=1.0, op0=ALU.max, op1=ALU.add), reads=[rT3[b]], writes=[rT3[b]])
                        k.op("pool", lambda e, b=b: e.tensor_tensor(out=T1[b][:], in0=T1[b][:], in1=T2[b][:], op=ALU.mult), reads=[rT1[b], rT2[b]], writes=[rT1[b]])
                        k.op("pool", lambda e, b=b: e.tensor_tensor(out=T1[b][:], in0=T1[b][:], in1=T3[b][:], op=ALU.mult), reads=[rT1[b], rT3[b]], writes=[rT1[b]])
                        k.op("pool", lambda e, b=b, ex=ex, fc=fc, ns=ns: e.tensor_tensor(out=AT[:, fc, ns * 512:(ns + 1) * 512], in0=T1[b][:], in1=GB[:, ex, ns * 512:(ns + 1) * 512], op=ALU.mult),
                             reads=[rT1[b], rGB], writes=[rAT[fc]])
                for dp in range(8):
                    wb, rwb = get_piece()
                    for tc in range(TG // 128):
                        b = dcnt % 2
                        dcnt += 1
                        if ex == 0:
                            k.op("pe", lambda e, b=b, tc=tc, dp=dp: e.matmul(PD[b][:, 0:256], lhsT=G4b[:, tc * 128:(tc + 1) * 128], rhs=BDb[:, dp * 256:(dp + 1) * 256], start=True, stop=False),
                                 reads=[rG4b, rBDb], writes=[rPD[b]], sig=False)
                        for fc in range(16):
                            k.op("pe", lambda e, b=b, tc=tc, fc=fc, wb=wb, ex=ex: e.matmul(PD[b][:, 0:256], lhsT=AT[:, fc, tc * 128:(tc + 1) * 128], rhs=wb[:, fc, :], start=(fc == 0 and ex != 0), stop=(fc == 15)),
                                 reads=[rAT[fc], rwb], writes=[rPD[b]], sig=(fc == 15))
                        ys = YACC[:, tc, dp * 256:(dp + 1) * 256]
                        if ex == 0:
                            k.op("act", lambda e, b=b, ys=ys: e.copy(out=ys, in_=PD[b][:, 0:256]), reads=[rPD[b]], writes=[rY[tc]])
                        else:
                            k.op("dve", lambda e, b=b, ys=ys: e.tensor_tensor(out=ys, in0=ys, in1=PD[b][:, 0:256], op=ALU.add), reads=[rPD[b], rY[tc]], writes=[rY[tc]])
            for tc in range(TG // 128):
                k.dma("sp", y_out[t0 + tc * 128:t0 + (tc + 1) * 128, :], YACC[:, tc, :], reads=[rY[tc]], key="oy", is_out=True)
        k.finish()
    return nc


CAP = 256
NBLK = NTOK // 1024
NSLOT = NBLK * CAP
HSLOT = NSLOT // 2


def build_moe2():
    nc = bass.Bass("TRN2", target_bir_lowering=False)
    dt_in = lambda name, shape, dt=F32: nc.dram_tensor(name, list(shape), dt, kind="ExternalInput").ap()
    hbt = dt_in("hbt", [NTOK, D], BF16)
    gC = dt_in("gC", [128, NTOK // 128, EPC])
    iotad = dt_in("iotad", [128, CAP])
    sud = dt_in("sud", [128, 128], BF16)
    onesd = dt_in("onesd", [128, 128], BF16)
    identb = dt_in("identb", [128, 128], BF16)
    w_gu = dt_in("w_gu", [EPC, D, 2 * D])
    bguT = dt_in("bguT", [128, EPC, 32])
    w_dn = dt_in("w_down", [EPC, D, D])
    b_dn = dt_in("b_down", [EPC, D])
    y_out = nc.dram_tensor("ypart", [NTOK, D], F32, kind="ExternalOutput").ap()
    ysp = nc.dram_tensor("ysp", [EPC, NSLOT, D], BF16, kind="Internal").ap()
    NTC = NTOK // 128
    with ExitStack() as es:
        k = KB(nc, es)
        IOTA = k.sb("IOTA", [128, CAP], F32); rIOTA = Res()
        SU = k.sb("SU", [128, 128], BF16); rSU = Res()
        ONES = k.sb("ONES", [128, 128], BF16); rONES = Res()
        ID = k.sb("ID", [128, 128], BF16); rID = Res()
        GC = k.sb("GC", [128, NTC, EPC], F32); rGC = Res()
        MKb = k.sb("MKb", [128, NTC * EPC], BF16); rMKb = Res()
        MKf = k.sb("MKf", [128, NTC, EPC], F32); rMKf = Res()
        W1 = k.sb("W1", [128, NTC, EPC], F32); rW1 = Res()
        CNT = k.sb("CNT", [128, NTC, EPC], F32); rCNT = Res()
        PRE = k.sb("PRE", [128, NTC, EPC], F32); rPRE = Res()
        POSM = k.sb("POSM", [128, NTC, EPC], F32); rPOSM = Res()
        BGU = k.sb("BGU", [128, EPC, 32], F32); rBGU = Res()
        BDb1 = k.sb("BDb1", [1, D], BF16); rBDb1 = Res()
        XT = k.sb("XT", [128, 16, HSLOT], BF16); rXT = Res()
        AT = k.sb("AT", [128, 16, HSLOT], BF16); rAT = [Res() for _ in range(16)]
        HBs = [k.sb("HB%d" % i, [128, 8, D], BF16) for i in range(2)]; rHBs = [Res(), Res()]
        S = [k.sb("S%d" % i, [128, 8, CAP], BF16) for i in range(2)]; rS = [Res(), Res()]
        YST = [k.sb("YST%d" % i, [128, 512], BF16) for i in range(4)]; rYST = [Res() for _ in range(4)]
        OUT = [k.sb("OUT%d" % i, [128, D], F32) for i in range(1)]; rOUT = [Res()]
        ring = WRing(k, "wm", 2, ncol=512)
        T1 = [k.sb("T1_%d" % i, [128, 512], F32) for i in range(2)]; rT1 = [Res(), Res()]
        T2 = [k.sb("T2_%d" % i, [128, 512], F32) for i in range(2)]; rT2 = [Res(), Res()]
        T3 = [k.sb("T3_%d" % i, [128, 512], F32) for i in range(2)]; rT3 = [Res(), Res()]
        PA = [k.ps("PA%d" % i, [128, 512], F32) for i in range(2)]; rPA = [Res(), Res()]
        PB = [k.ps("PB%d" % i, [128, 512], F32) for i in range(2)]; rPB = [Res(), Res()]
        PD = [k.ps("PD%d" % i, [128, 512], F32) for i in range(2)]; rPD = [Res(), Res()]
        PT = [k.ps("PT%d" % i, [128, 1024], BF16) for i in range(2)]; rPT = [Res(), Res()]
        ld = lambda dst, src, r, key: k.dma("sp", dst, src, writes=[r], key=key)
        ld(IOTA[:], iotad, rIOTA, "c0"); ld(SU[:], sud, rSU, "c1"); ld(ONES[:], onesd, rONES, "c2"); ld(ID[:], identb, rID, "c3")
        ld(GC[:], gC, rGC, "c4"); ld(BGU[:], bguT, rBGU, "c5")
        gcf = GC[:].rearrange("p a b -> p (a b)")
        k.op("dve", lambda e: e.tensor_scalar(out=MKb[:], in0=gcf, scalar1=0.0, scalar2=None, op0=ALU.is_gt), reads=[rGC], writes=[rMKb])
        k.op("dve", lambda e: e.tensor_scalar(out=MKf[:].rearrange("p a b -> p (a b)"), in0=gcf, scalar1=0.0, scalar2=None, op0=ALU.is_gt), reads=[rGC], writes=[rMKf])
        k.op("pe", lambda e: e.matmul(PA[0][:, 0:NTC * EPC], lhsT=SU[:], rhs=MKb[:], start=True, stop=True), reads=[rSU, rMKb], writes=[rPA[0]])
        k.op("pe", lambda e: e.matmul(PA[1][:, 0:NTC * EPC], lhsT=ONES[:], rhs=MKb[:], start=True, stop=True), reads=[rONES, rMKb], writes=[rPA[1]])
        k.op("dve", lambda e: e.tensor_copy(out=W1[:].rearrange("p a b -> p (a b)"), in_=PA[0][:, 0:NTC * EPC]), reads=[rPA[0]], writes=[rW1])
        k.op("dve", lambda e: e.tensor_copy(out=CNT[:].rearrange("p a b -> p (a b)"), in_=PA[1][:, 0:NTC * EPC]), reads=[rPA[1]], writes=[rCNT])
        pre4 = PRE[:].rearrange("p (j t) e -> p j t e", t=8)
        cnt4 = CNT[:].rearrange("p (j t) e -> p j t e", t=8)
        k.op("dve", lambda e: e.memset(PRE[:], 0.0), writes=[rPRE])
        for i in range(1, 8):
            k.op("dve", lambda e, i=i: e.tensor_tensor(out=pre4[:, :, i, :], in0=pre4[:, :, i - 1, :], in1=cnt4[:, :, i - 1, :], op=ALU.add), reads=[rPRE, rCNT], writes=[rPRE])
        k.op("dve", lambda e: e.tensor_tensor(out=POSM[:], in0=W1[:], in1=PRE[:], op=ALU.add), reads=[rW1, rPRE], writes=[rPOSM])
        k.op("dve", lambda e: e.scalar_tensor_tensor(out=POSM[:], in0=POSM[:], scalar=1.0, in1=MKf[:], op0=ALU.add, op1=ALU.mult), reads=[rPOSM, rMKf], writes=[rPOSM])
        k.op("dve", lambda e: e.tensor_scalar(out=POSM[:], in0=POSM[:], scalar1=-1.0, scalar2=None, op0=ALU.add), reads=[rPOSM], writes=[rPOSM])
        pieces = []
        for ex, _half in [(a, b) for a in range(EPC) for b in range(2)]:
            for fp in range(8):
                pieces.append([(0, 256, w_gu[ex][:, fp * 256:(fp + 1) * 256]), (256, 512, w_gu[ex][:, D + fp * 256:D + (fp + 1) * 256])])
            for dp in range(4):
                pieces.append([(0, 512, w_dn[ex][:, dp * 512:(dp + 1) * 512])])
        LA = 1
        loaded = []
        pi = [0]

        def get_piece():
            while len(loaded) < min(len(pieces), pi[0] + 1 + LA):
                loaded.append(ring.load(pieces[len(loaded)]))
            r = loaded[pi[0]]
            pi[0] += 1
            return r

        cnt = 0
        dcnt = 0
        scnt = 0
        ycnt = 0
        NS = HSLOT // 512
        for ex, half in [(a, b) for a in range(EPC) for b in range(2)]:
            for j in range(half * NBLK // 2, (half + 1) * NBLK // 2):
                jl = j - half * NBLK // 2
                HB, rHB = HBs[scnt % 2], rHBs[scnt % 2]
                k.dma("sp", HB[:], hbt[j * 1024:(j + 1) * 1024, :].rearrange("(tc p) d -> p tc d", p=128), writes=[rHB], key="hb%d" % (scnt % 2))
                sb_ = scnt % 2
                scnt += 1
                for tc in range(8):
                    k.op("dve", lambda e, sb_=sb_, tc=tc, j=j, ex=ex: e.tensor_scalar(out=S[sb_][:, tc, :], in0=IOTA[:], scalar1=POSM[:, j * 8 + tc, ex:ex + 1], scalar2=None, op0=ALU.is_equal),
                         reads=[rIOTA, rPOSM], writes=[rS[sb_]])
                for dcp in range(8):
                    b = dcnt % 2
                    dcnt += 1
                    for dci in range(2):
                        dc = dcp * 2 + dci
                        for tc in range(8):
                            k.op("pe", lambda e, b=b, dci=dci, dc=dc, tc=tc, sb_=sb_, HB=HB: e.matmul(PD[b][:, dci * CAP:(dci + 1) * CAP], lhsT=HB[:, tc, dc * 128:(dc + 1) * 128], rhs=S[sb_][:, tc, :], start=(tc == 0), stop=(tc == 7)),
                                 reads=[rHB, rS[sb_]], writes=[rPD[b]], sig=(tc == 7 and dci == 1))
                    eng = "act" if dcp % 2 == 0 else "dve"
                    if eng == "act":
                        k.op("act", lambda e, b=b, dcp=dcp, jl=jl: e.copy(out=XT[:, 2 * dcp:2 * dcp + 2, jl * CAP:(jl + 1) * CAP], in_=PD[b][:, 0:2 * CAP].rearrange("p (a s) -> p a s", a=2)), reads=[rPD[b]], writes=[rXT])
                    else:
                        k.op("dve", lambda e, b=b, dcp=dcp, jl=jl: e.tensor_copy(out=XT[:, 2 * dcp:2 * dcp + 2, jl * CAP:(jl + 1) * CAP], in_=PD[b][:, 0:2 * CAP].rearrange("p (a s) -> p a s", a=2)), reads=[rPD[b]], writes=[rXT])
            k.dma("pool", BDb1[:], b_dn[ex:ex + 1, :], writes=[rBDb1], key="bd")
            for fc in range(16):
                if fc % 2 == 0:
                    wb, rwb = get_piece()
                go = (fc % 2) * 128
                for ns in range(NS):
                    b = cnt % 2
                    cnt += 1
                    for kc in range(16):
                        k.op("pe", lambda e, b=b, kc=kc, wb=wb, ns=ns, go=go: e.matmul(PA[b][:], lhsT=wb[:, kc, go:go + 128], rhs=XT[:, kc, ns * 512:(ns + 1) * 512], start=(kc == 0), stop=(kc == 15)),
                             reads=[rwb, rXT], writes=[rPA[b]], sig=(kc == 15))
                    for kc in range(16):
                        k.op("pe", lambda e, b=b, kc=kc, wb=wb, ns=ns, go=go: e.matmul(PB[b][:], lhsT=wb[:, kc, 256 + go:256 + go + 128], rhs=XT[:, kc, ns * 512:(ns + 1) * 512], start=(kc == 0), stop=(kc == 15)),
                             reads=[rwb, rXT], writes=[rPB[b]], sig=(kc == 15))
                    k.op("dve", lambda e, b=b, ex=ex, fc=fc: e.tensor_scalar(out=T1[b][:], in0=PA[b][:], scalar1=BGU[:, ex, fc:fc + 1], scalar2=7.0, op0=ALU.add, op1=ALU.min),
                         reads=[rPA[b], rBGU], writes=[rT1[b]])
                    k.op("act", lambda e, b=b: e.activation(out=T2[b][:], in_=T1[b][:], func=AF.Sigmoid, scale=1.702), reads=[rT1[b]], writes=[rT2[b]])
                    k.op("dve", lambda e, b=b, ex=ex, fc=fc: e.tensor_scalar(out=T3[b][:], in0=PB[b][:], scalar1=BGU[:, ex, 16 + fc:17 + fc], scalar2=7.0, op0=ALU.add, op1=ALU.min),
                         reads=[rPB[b], rBGU], writes=[rT3[b]])
                    k.op("dve", lambda e, b=b: e.tensor_scalar(out=T3[b][:], in0=T3[b][:], scalar1=-7.0, scalar2=1.0, op0=ALU.max, op1=ALU.add), reads=[rT3[b]], writes=[rT3[b]])
                    k.op("pool", lambda e, b=b: e.tensor_tensor(out=T1[b][:], in0=T1[b][:], in1=T2[b][:], op=ALU.mult), reads=[rT1[b], rT2[b]], writes=[rT1[b]])
                    k.op("pool", lambda e, b=b, fc=fc, ns=ns: e.tensor_tensor(out=AT[:, fc, ns * 512:(ns + 1) * 512], in0=T1[b][:], in1=T3[b][:], op=ALU.mult),
                         reads=[rT1[b], rT3[b]], writes=[rAT[fc]])
            for dp in range(4):
                wb, rwb = get_piece()
                for sc in range(HSLOT // 128):
                    b = dcnt % 2
                    dcnt += 1
                    k.op("pe", lambda e, b=b, dp=dp, ex=ex: e.matmul(PD[b][:], lhsT=ONES[0:1, :], rhs=BDb1[:, dp * 512:(dp + 1) * 512], start=True, stop=False),
                         reads=[rONES, rBDb1], writes=[rPD[b]], sig=False)
                    for fc in range(16):
                        k.op("pe", lambda e, b=b, sc=sc, fc=fc, wb=wb: e.matmul(PD[b][:], lhsT=AT[:, fc, sc * 128:(sc + 1) * 128], rhs=wb[:, fc, :], start=False, stop=(fc == 15)),
                             reads=[rAT[fc], rwb], writes=[rPD[b]], sig=(fc == 15))
                    yb_ = ycnt % 4
                    ycnt += 1
                    if yb_ % 2 == 0:
                        k.op("act", lambda e, b=b, yb_=yb_: e.copy(out=YST[yb_][:], in_=PD[b][:]), reads=[rPD[b]], writes=[rYST[yb_]])
                    else:
                        k.op("dve", lambda e, b=b, yb_=yb_: e.tensor_copy(out=YST[yb_][:], in_=PD[b][:]), reads=[rPD[b]], writes=[rYST[yb_]])
                    k.dma("sp", ysp[ex, half * HSLOT + sc * 128:half * HSLOT + (sc + 1) * 128, dp * 512:(dp + 1) * 512], YST[yb_][:], reads=[rYST[yb_]], key="ys%d" % yb_)
        xtf = XT[:].rearrange("p a b -> p (a b)")
        atf = AT[:].rearrange("p a b -> p (a b)")
        hbf = HBs[0][:].rearrange("p a b -> p (a b)")
        Ybuf = []
        for src in (xtf, hbf):
            Ybuf.append((src[:, 0:EPC * D].rearrange("p (e d) -> p e d", e=EPC), src[:, EPC * D:2 * EPC * D].rearrange("p (e d) -> p e d", e=EPC)))
        SGbuf = []
        for o in (0, 2 * EPC * 1024):
            SGbuf.append((atf[:, o:o + EPC * 1024].rearrange("p (e t) -> p e t", e=EPC), atf[:, o + EPC * 1024:o + 2 * EPC * 1024].rearrange("p (e t) -> p e t", e=EPC)))
        rYb = [Res(), Res()]; rSGb = [Res(), Res()]
        banks = [(PA[0], rPA[0]), (PA[1], rPA[1]), (PB[0], rPB[0]), (PB[1], rPB[1]), (PD[0], rPD[0]), (PD[1], rPD[1])]
        bcnt = 0
        ocnt = 0
        for j in range(NBLK):
            par = j % 2
            Y1, Y2 = Ybuf[par]
            SG1, SG2 = SGbuf[par]
            rY12, rSG = rYb[par], rSGb[par]
            for ex in range(EPC):
                if j < 2 and ex == 0:
                    wr = [rY12] + rYST + ([rXT] if par == 0 else rHBs)
                else:
                    wr = [rY12]
                k.dma("sp", Y1[:, ex, :], ysp[ex, j * CAP:j * CAP + 128, :], writes=wr, key="y1%d" % par)
                k.dma("sp", Y2[:, ex, :], ysp[ex, j * CAP + 128:(j + 1) * CAP, :], writes=[rY12], key="y1%d" % par)
            for ex in range(EPC):
                sb_ = scnt % 2
                scnt += 1
                for tc in range(8):
                    k.op("dve", lambda e, sb_=sb_, tc=tc, j=j, ex=ex: e.tensor_scalar(out=S[sb_][:, tc, :], in0=IOTA[:], scalar1=POSM[:, j * 8 + tc, ex:ex + 1], scalar2=GC[:, j * 8 + tc, ex:ex + 1], op0=ALU.is_equal, op1=ALU.mult),
                         reads=[rIOTA, rPOSM, rGC], writes=[rS[sb_]])
                for tc in range(8):
                    k.op("pe", lambda e, sb_=sb_, tc=tc: e.transpose(out=PT[0][:, tc * 128:(tc + 1) * 128], in_=S[sb_][:, tc, 0:128], identity=ID[:]), reads=[rS[sb_], rID], writes=[rPT[0]], sig=(tc == 7))
                for tc in range(8):
                    k.op("pe", lambda e, sb_=sb_, tc=tc: e.transpose(out=PT[1][:, tc * 128:(tc + 1) * 128], in_=S[sb_][:, tc, 128:CAP], identity=ID[:]), reads=[rS[sb_], rID], writes=[rPT[1]], sig=(tc == 7))
                k.op("act", lambda e, ex=ex, SG1=SG1: e.copy(out=SG1[:, ex, :], in_=PT[0][:]), reads=[rPT[0]], writes=[rSG] + (rAT if j < 2 else []))
                k.op("dve", lambda e, ex=ex, SG2=SG2: e.tensor_copy(out=SG2[:, ex, :], in_=PT[1][:]), reads=[rPT[1]], writes=[rSG])
            for tc in range(8):
                ob = 0
                ocnt += 1
                for dg in range(4):
                    pb_, rpb_ = banks[bcnt % 6]
                    bcnt += 1
                    n = 0
                    for ex in range(EPC):
                        k.op("pe", lambda e, pb_=pb_, ex=ex, tc=tc, dg=dg, n=n, SG1=SG1, Y1=Y1: e.matmul(pb_[:], lhsT=SG1[:, ex, tc * 128:(tc + 1) * 128], rhs=Y1[:, ex, dg * 512:(dg + 1) * 512], start=(n == 0), stop=False),
                             reads=[rSG, rY12], writes=[rpb_], sig=False)
                        n += 1
                        k.op("pe", lambda e, pb_=pb_, ex=ex, tc=tc, dg=dg, n=n, SG2=SG2, Y2=Y2: e.matmul(pb_[:], lhsT=SG2[:, ex, tc * 128:(tc + 1) * 128], rhs=Y2[:, ex, dg * 512:(dg + 1) * 512], start=False, stop=(n == 2 * EPC - 1)),
                             reads=[rSG, rY12], writes=[rpb_], sig=(n == 2 * EPC - 1))
                        n += 1
                    if dg % 2 == 0:
                        k.op("act", lambda e, pb_=pb_, ob=ob, dg=dg: e.copy(out=OUT[ob][:, dg * 512:(dg + 1) * 512], in_=pb_[:]), reads=[rpb_], writes=[rOUT[ob]])
                    else:
                        k.op("dve", lambda e, pb_=pb_, ob=ob, dg=dg: e.tensor_copy(out=OUT[ob][:, dg * 512:(dg + 1) * 512], in_=pb_[:]), reads=[rpb_], writes=[rOUT[ob]])
                k.dma("sp", y_out[j * 1024 + tc * 128:j * 1024 + (tc + 1) * 128, :], OUT[ob][:], reads=[rOUT[ob]], key="oy%d" % ob, is_out=True)
        k.finish()
    return nc


def build_post():
    nc = bass.Bass("TRN2", target_bir_lowering=False)
    dt_in = lambda name, shape, dt=F32: nc.dram_tensor(name, list(shape), dt, kind="ExternalInput").ap()
    h1 = dt_in("h1", [TPC, D])
    parts = dt_in("parts", [NCORES, TPC, D])
    pin = dt_in("p", [TPC, PLE])
    identb = dt_in("identb", [128, 128], BF16)
    ln2g = dt_in("ln2_g", [D]); ln2b = dt_in("ln2_b", [D])
    ln3g = dt_in("ln3_g", [D]); ln3b = dt_in("ln3_b", [D])
    wg = dt_in("w_ple_gate", [D, D])
    bg = dt_in("b_ple_gate", [D])
    wp = dt_in("w_ple_proj", [PLE, D])
    h3 = nc.dram_tensor("h3", [TPC, D], F32, kind="ExternalOutput").ap()
    NJ = TPC // 128
    with ExitStack() as es:
        k = KB(nc, es)
        ID = k.sb("ID", [128, 128], BF16); rID = Res()
        LG = k.sb("LG", [128, D], F32); rLG = Res()
        LB = k.sb("LB", [128, D], F32); rLB = Res()
        BG = k.sb("BG", [128, D], F32); rBG = Res()
        WP = k.sb("WP", [128, 2, D], BF16); rWP = Res()
        H = k.sb("H", [128, NJ, D], F32); rH = [Res() for _ in range(NJ)]
        PBUF = [k.sb("PBUF%d" % i, [128, D], F32) for i in range(3)]; rPBUF = [Res() for _ in range(3)]
        HT = k.sb("HT", [128, 16, 512], BF16); rHT = [Res() for _ in range(4)]
        PTT = k.sb("PTT", [128, 2, 512], BF16); rPTT = [Res() for _ in range(4)]
        hb = k.sb("hb", [128, D], BF16); rhb = Res()
        p32 = k.sb("p32", [128, PLE], F32); rp32 = Res()
        pb = k.sb("pb", [128, PLE], BF16); rpb = Res()
        t1 = [k.sb("t1_%d" % i, [128, 256], F32) for i in range(2)]; rt1 = [Res(), Res()]
        ring = WRing(k, "wq", 3)
        lt = ln_tmp(k, "lt")
        PT = [k.ps("PT%d" % i, [128, 1024], BF16) for i in range(2)]; rPT = [Res(), Res()]
        PP = [k.ps("PP%d" % i, [128, 512], F32) for i in range(2)]; rPP = [Res(), Res()]
        ptc = [0]; ppc = [0]

        def nextPT():
            i = ptc[0] % 2; ptc[0] += 1
            return PT[i], rPT[i]

        def nextPP():
            i = ppc[0] % 2; ppc[0] += 1
            return PP[i], rPP[i]

        k.dma("sp", ID[:], identb, writes=[rID], key="c0")
        k.dma("sp", LG[:], ln2g.partition_broadcast(128), writes=[rLG], key="c1")
        k.dma("sp", LB[:], ln2b.partition_broadcast(128), writes=[rLB], key="c2")
        k.dma("sp", BG[:], bg.partition_broadcast(128), writes=[rBG], key="c3")
        k.dma("pool", WP[:], wp.rearrange("(kc p) n -> p kc n", p=128), writes=[rWP], key="c4")
        pc = 0
        rHa = [Res() for _ in range(NJ)]; rHb = [Res() for _ in range(NJ)]
        for j in range(NJ):
            k.dma("sp", H[:, j, :], h1[j * 128:(j + 1) * 128, :], writes=[rH[j]], key="h%d" % j)
            k.op("act", lambda e, j=j: e.mul(out=H[:, j, :], in_=H[:, j, :], mul=ALPHA), reads=[rH[j]], writes=[rH[j], rHa[j], rHb[j]])
            for c in range(NCORES):
                b = pc % 3
                pc += 1
                k.dma("sp", PBUF[b][:], parts[c, j * 128:(j + 1) * 128, :], writes=[rPBUF[b]], key="pb%d" % b)
                k.op("dve", lambda e, b=b, j=j: e.tensor_tensor(out=H[:, j, 0:1280], in0=H[:, j, 0:1280], in1=PBUF[b][:, 0:1280], op=ALU.add), reads=[rHa[j], rPBUF[b]], writes=[rHa[j]])
                k.op("pool", lambda e, b=b, j=j: e.tensor_tensor(out=H[:, j, 1280:D], in0=H[:, j, 1280:D], in1=PBUF[b][:, 1280:D], op=ALU.add), reads=[rHb[j], rPBUF[b]], writes=[rHb[j]])
            k.op("dve", lambda e, j=j: e.tensor_copy(out=lt[4][:, 0:1], in_=H[:, j, 0:1]), reads=[rHa[j], rHb[j]], writes=[rH[j], lt[5]])
            emit_layernorm(k, H[:, j, :], rH[j], LG, rLG, LB, rLB, lt)
        k.dma("sp", LG[:], ln3g.partition_broadcast(128), writes=[rLG], key="c1")
        k.dma("sp", LB[:], ln3b.partition_broadcast(128), writes=[rLB], key="c2")
        for grp in ([0, 1, 2, 3], [4, 5, 6, 7]):
            wb_next, rwb_next = ring.load([(0, 256, wg[:, 0:256])])
            for c, j in enumerate(grp):
                k.op("act", lambda e, j=j: e.copy(out=hb[:], in_=H[:, j, :]), reads=[rH[j]], writes=[rhb])
                for half in range(2):
                    pt, rpt = nextPT()
                    for i in range(8):
                        kc = half * 8 + i
                        k.op("pe", lambda e, pt=pt, kc=kc, i=i: e.transpose(out=pt[:, i * 128:(i + 1) * 128], in_=hb[:, kc * 128:(kc + 1) * 128], identity=ID[:]),
                             reads=[rhb, rID], writes=[rpt], sig=(i == 7))
                    k.op("dve", lambda e, pt=pt, half=half, c=c: e.tensor_copy(out=HT[:, half * 8:(half + 1) * 8, c * 128:(c + 1) * 128], in_=pt[:].rearrange("p (a b) -> p a b", a=8)),
                         reads=[rpt], writes=[rHT[c]])
                k.op("act", lambda e, j=j: e.mul(out=H[:, j, :], in_=H[:, j, :], mul=ALPHA), reads=[rH[j]], writes=[rH[j]])
                k.dma("sp", p32[:], pin[j * 128:(j + 1) * 128, :], writes=[rp32], key="pp")
                k.op("act", lambda e: e.copy(out=pb[:], in_=p32[:]), reads=[rp32], writes=[rpb])
                pt, rpt = nextPT()
                for i in range(2):
                    k.op("pe", lambda e, pt=pt, i=i: e.transpose(out=pt[:, i * 128:(i + 1) * 128], in_=pb[:, i * 128:(i + 1) * 128], identity=ID[:]),
                         reads=[rpb, rID], writes=[rpt], sig=(i == 1))
                k.op("dve", lambda e, pt=pt, c=c: e.tensor_copy(out=PTT[:, :, c * 128:(c + 1) * 128], in_=pt[:, 0:256].rearrange("p (a b) -> p a b", a=2)),
                     reads=[rpt], writes=[rPTT[c]])
            for gi in range(8):
                wb, rwb = wb_next, rwb_next
                if gi < 7:
                    wb_next, rwb_next = ring.load([(0, 256, wg[:, (gi + 1) * 256:(gi + 2) * 256])])
                cols = slice(gi * 256, (gi + 1) * 256)
                for c, j in enumerate(grp):
                    pp, rpp = nextPP()
                    for kc in range(16):
                        k.op("pe", lambda e, pp=pp, kc=kc, c=c, wb=wb: e.matmul(pp[:, 0:256], lhsT=HT[:, kc, c * 128:(c + 1) * 128], rhs=wb[:, kc, :], start=(kc == 0), stop=(kc == 15)),
                             reads=[rHT[c], rwb], writes=[rpp], sig=False)
                    for kc in range(2):
                        k.op("pe", lambda e, pp=pp, kc=kc, c=c, cols=cols: e.matmul(pp[:, 256:512], lhsT=PTT[:, kc, c * 128:(c + 1) * 128], rhs=WP[:, kc, cols], start=(kc == 0), stop=(kc == 1)),
                             reads=[rPTT[c], rWP], writes=[rpp], sig=(kc == 1))
                    b = (gi * 4 + c) % 2
                    k.op("dve", lambda e, pp=pp, b=b, cols=cols: e.tensor_tensor(out=t1[b][:], in0=pp[:, 0:256], in1=BG[:, cols], op=ALU.add), reads=[rpp, rBG], writes=[rt1[b]])
                    k.op("act", lambda e, b=b: e.activation(out=t1[b][:], in_=t1[b][:], func=AF.Sigmoid), reads=[rt1[b]], writes=[rt1[b]])
                    k.op("dve", lambda e, pp=pp, b=b: e.tensor_tensor(out=t1[b][:], in0=t1[b][:], in1=pp[:, 256:512], op=ALU.mult), reads=[rt1[b], rpp], writes=[rt1[b]])
                    k.op("pool", lambda e, b=b, j=j, cols=cols: e.tensor_tensor(out=H[:, j, cols], in0=H[:, j, cols], in1=t1[b][:], op=ALU.add), reads=[rt1[b], rH[j]], writes=[rH[j]])
            for j in grp:
                emit_layernorm(k, H[:, j, :], rH[j], LG, rLG, LB, rLB, lt)
                k.dma("sp", h3[j * 128:(j + 1) * 128, :], H[:, j, :], reads=[rH[j]], key="oh", is_out=True)
        k.finish()
    return nc


def _consts():
    q = np.arange(128)[:, None]
    j = np.arange(128)[None, :]
    prev = np.where(j > q, 0.0, NEG).astype(np.float32)
    cur = np.where(j <= q, 0.0, NEG).astype(np.float32)
    std = np.concatenate([prev, cur], axis=1)
    first = np.concatenate([np.full((128, 128), NEG, np.float32), cur], axis=1)
    half = 8
    invf = np.exp(-np.log(500000.0) * np.arange(half, dtype=np.float32) * (2.0 / 16)).astype(np.float32)
    return dict(
        identb=np.eye(128).astype(ml_dtypes.bfloat16),
        identf=np.eye(128, dtype=np.float32),
        trild=(j <= q).astype(np.float32),
        invfd=np.ascontiguousarray(np.broadcast_to(invf[None, :], (128, 8))),
        mask_std=std, mask_first=first,
        iotad=np.ascontiguousarray(np.broadcast_to(np.arange(CAP, dtype=np.float32)[None, :], (128, CAP))),
        sud=(q < j).astype(ml_dtypes.bfloat16),
        onesd=np.ones((128, 128), ml_dtypes.bfloat16),
    )


def mixer_inputs(layer, h_full, positions, prm):
    cst = _consts()
    maps = []
    pos_flat = positions.reshape(-1)
    for c in range(NCORES):
        t0 = c * TPC
        seq_start = (t0 % SEQ) == 0
        xin = np.zeros((NCH * 128, D), np.float32)
        pos = np.zeros((NCH * 128,), np.int32)
        xin[128:] = h_full[t0:t0 + TPC]
        pos[128:] = pos_flat[t0:t0 + TPC]
        if not seq_start:
            xin[:128] = h_full[t0 - 128:t0]
            pos[:128] = pos_flat[t0 - 128:t0]
        m = dict(
            xin=xin, posi=np.ascontiguousarray(pos.reshape(NCH, 128).T),
            maskd=np.ascontiguousarray(np.stack([cst["mask_first"] if seq_start else cst["mask_std"], cst["mask_std"]], axis=1)),
            identb=cst["identb"], identf=cst["identf"], trild=cst["trild"], invfd=cst["invfd"],
            w_in=prm["w_in"][layer], sinks=prm["sinks"][layer], w_s=prm["w_s"][layer],
            bsT=np.ascontiguousarray(prm["b_s"][layer].T),
            vnorm_g=np.ascontiguousarray(prm["vnorm_g"][layer].reshape(-1)), vnorm_b=np.ascontiguousarray(prm["vnorm_b"][layer].reshape(-1)),
            gnT=np.ascontiguousarray(np.concatenate([prm["gnorm_attn"][layer], prm["gnorm_gmlp"][layer]]).reshape(16, 128).T),
            w_o=prm["w_o"][layer], ln1_g=prm["ln1_g"][layer], ln1_b=prm["ln1_b"][layer],
            w_router=prm["w_router"][layer], b_router=prm["b_router"][layer],
        )
        if layer == 0:
            m["ln_in_g"] = prm["ln_in_g"]
            m["ln_in_b"] = prm["ln_in_b"]
        maps.append(m)
    return maps


_PROGS = {}


def _prog(name, fn):
    if name not in _PROGS:
        _PROGS[name] = fn()
    return _PROGS[name]


def kernel(**inp):
    prm = {k_: np.asarray(v) for k_, v in inp.items()}
    cores = list(range(NCORES))
    cst = _consts()
    h_full = np.ascontiguousarray(prm["x"].reshape(NTOK, D))
    for layer in range(DEPTH):
        nc = _prog("mix%d" % (layer == 0), lambda: build_mixer(layer == 0))
        res = run_bass_kernel_spmd(nc, mixer_inputs(layer, h_full, prm["positions"], prm), core_ids=cores).results
        h1 = [res[c]["h1"] for c in cores]
        hbt = np.concatenate([res[c]["hbt"] for c in cores], axis=0)
        G = np.concatenate([res[c]["gates"] for c in cores], axis=0)
        del res
        nc = _prog("moe2", build_moe2)
        maps = []
        for c in cores:
            es_ = slice(c * EPC, (c + 1) * EPC)
            maps.append(dict(
                hbt=hbt, gC=np.ascontiguousarray(G[:, es_].reshape(NTOK // 128, 128, EPC).transpose(1, 0, 2)),
                iotad=cst["iotad"], sud=cst["sud"], onesd=cst["onesd"], identb=cst["identb"],
                w_gu=prm["w_gu"][layer, es_],
                bguT=np.ascontiguousarray(prm["b_gu"][layer, es_].reshape(EPC, 32, 128).transpose(2, 0, 1)),
                w_down=prm["w_down"][layer, es_], b_down=prm["b_down"][layer, es_]))
        res = run_bass_kernel_spmd(nc, maps, core_ids=cores).results
        yp = [res[c]["ypart"] for c in cores]
        del res, maps
        nc = _prog("post", build_post)
        p_l = prm["p"][layer].reshape(NTOK, PLE)
        maps = []
        for c in cores:
            ts = slice(c * TPC, (c + 1) * TPC)
            maps.append(dict(
                h1=h1[c], parts=np.ascontiguousarray(np.stack([yp[e][ts] for e in cores], axis=0)),
                p=np.ascontiguousarray(p_l[ts]), identb=cst["identb"],
                ln2_g=prm["ln2_g"][layer], ln2_b=prm["ln2_b"][layer], ln3_g=prm["ln3_g"][layer], ln3_b=prm["ln3_b"][layer],
                w_ple_gate=prm["w_ple_gate"][layer], b_ple_gate=prm["b_ple_gate"][layer], w_ple_proj=prm["w_ple_proj"][layer]))
        del yp
        res = run_bass_kernel_spmd(nc, maps, core_ids=cores).results
        h_full = np.concatenate([res[c]["h3"] for c in cores], axis=0)
        del res, maps
    return h_full.reshape(BATCH, SEQ, D).astype(np.float32)
```
